# Optimizing a Trainium2 kernel written in Bass

```python
import jax
import jax.numpy as jnp
from jax import lax
import numpy as np

D_MODEL = 1024
BATCH = 16
SEQ = 2048
DEPTH = 1

POOL_WINDOWS = (2, 4, 8, 16)
POOL_GROUP = 128
POOL_WIDTH = POOL_GROUP * len(POOL_WINDOWS)
HEAD_DIM = 64
ATTN_GROUPS = ((128, 1), (512, 4), (2048, 16))
HEADS_PER_GROUP = 4
N_HEADS = HEADS_PER_GROUP * len(ATTN_GROUPS)
ATTN_WIDTH = N_HEADS * HEAD_DIM
ATTN_OUT_WIDTH = HEADS_PER_GROUP * HEAD_DIM
ATTN_BLOCK = 64
ROPE_THETA = 500000.0
ROPE_DIM = HEAD_DIM // 4
N_BRANCHES = 2
IN_SPLITS = (POOL_WIDTH, POOL_WIDTH + ATTN_WIDTH, POOL_WIDTH + 2 * ATTN_WIDTH, POOL_WIDTH + 3 * ATTN_WIDTH, POOL_WIDTH + 3 * ATTN_WIDTH + D_MODEL)
IN_WIDTH = POOL_WIDTH + 3 * ATTN_WIDTH + N_BRANCHES * D_MODEL
N_EXPERTS = 256
TOP_K = 8
N_EXPERT_GROUPS = 8
TOPK_GROUPS = 4
EXPERT_FF = 256
SHARED_FF = 256
ROUTED_SCALE = 2.5
MOE_BLOCK = 128
N_MOD = 6
EPS = 1e-6
NEG_BIG = -1e30

kernel_name = 'hybrid_pool_dilattn_moe_block'


def rms_norm(x, g):
    xf = x.astype(jnp.float32)
    y = xf * lax.rsqrt(jnp.mean(xf * xf, axis=-1, keepdims=True) + EPS)
    return (y * g.astype(jnp.float32)).astype(x.dtype)


def centred_mean_minus_self(u, radius):
    S = u.shape[1]
    uf = u.astype(jnp.float32)
    cs = jnp.concatenate([jnp.zeros_like(uf[:, :1]), lax.cumsum(uf, axis=1)], axis=1)
    t = jnp.arange(S)
    lo = jnp.maximum(t - radius, 0)
    hi = jnp.minimum(t + radius, S - 1) + 1
    count = (hi - lo).astype(jnp.float32)[None, :, None]
    return ((cs[:, hi] - cs[:, lo]) / count - uf).astype(u.dtype)


def pool_mixer(u, w_grp, ls):
    B, S, _ = u.shape
    ug = u.reshape(B, S, len(POOL_WINDOWS), POOL_GROUP)
    pooled = jnp.stack([centred_mean_minus_self(ug[:, :, i], w // 2) for i, w in enumerate(POOL_WINDOWS)], axis=2)
    y = jnp.einsum('bsgc,gcd->bsgd', pooled, w_grp)
    return y.reshape(B, S, POOL_WIDTH) * ls


def partial_rope(x, positions):
    half = ROPE_DIM // 2
    inv_freq = ROPE_THETA ** (-jnp.arange(half, dtype=jnp.float32) / half)
    ang = positions.astype(jnp.float32)[..., None] * inv_freq
    cos = jnp.cos(ang)[:, :, None, :]
    sin = jnp.sin(ang)[:, :, None, :]
    xr = x[..., :ROPE_DIM].astype(jnp.float32)
    x1, x2 = xr[..., :half], xr[..., half:]
    rot = jnp.concatenate([x1 * cos - x2 * sin, x2 * cos + x1 * sin], axis=-1).astype(x.dtype)
    return jnp.concatenate([rot, x[..., ROPE_DIM:]], axis=-1)


def to_classes(x, dilation, n_blocks):
    B, S, H, X = x.shape
    L = S // dilation
    xc = jnp.transpose(x.reshape(B, L, dilation, H, X), (0, 2, 3, 1, 4))
    xc = jnp.pad(xc, ((0, 0), (0, 0), (0, 0), (0, n_blocks * ATTN_BLOCK - L), (0, 0)))
    return xc.reshape(B, dilation, H, n_blocks, ATTN_BLOCK, X)


def from_classes(xc, seq):
    B, r, H, nb, Q, X = xc.shape
    L = seq // r
    x = xc.reshape(B, r, H, nb * Q, X)[:, :, :, :L]
    return jnp.transpose(x, (0, 3, 1, 2, 4)).reshape(B, seq, H, X)


def neighbour_blocks(xb):
    xp = jnp.pad(xb, ((0, 0), (0, 0), (0, 0), (1, 1), (0, 0), (0, 0)))
    return jnp.concatenate([xp[:, :, :, :-2], xp[:, :, :, 1:-1], xp[:, :, :, 2:]], axis=4)


def dilated_window_attention(q, k, v, window, dilation):
    B, S, H, D = q.shape
    J = window // (2 * dilation)
    L = S // dilation
    nb = -(-L // ATTN_BLOCK)
    qb = to_classes(q, dilation, nb)
    kb = neighbour_blocks(to_classes(k, dilation, nb))
    vb = neighbour_blocks(to_classes(v, dilation, nb))
    qi = jnp.arange(nb)[:, None] * ATTN_BLOCK + jnp.arange(ATTN_BLOCK)[None, :]
    ki = jnp.arange(nb)[:, None] * ATTN_BLOCK - ATTN_BLOCK + jnp.arange(3 * ATTN_BLOCK)[None, :]
    rel = ki[:, None, :] - qi[:, :, None]
    mask = (jnp.abs(rel) <= J) & (ki[:, None, :] >= 0) & (ki[:, None, :] < L)
    s = jnp.einsum('brhnqd,brhnkd->brhnqk', qb, kb, preferred_element_type=jnp.float32) * (HEAD_DIM ** -0.5)
    s = jnp.where(mask, s, NEG_BIG)
    m = jnp.max(s, axis=-1, keepdims=True)
    p = jnp.exp(s - m)
    l = jnp.sum(p, axis=-1, keepdims=True)
    o = jnp.einsum('brhnqk,brhnkd->brhnqd', p, vb.astype(jnp.float32)) / l
    log_den = m + jnp.log(l)
    return from_classes(o, S), from_classes(log_den, S)[..., 0]


def attention_branch(q, k, v, q_norm_g, k_norm_g, positions):
    B, S, _ = q.shape
    q = partial_rope(rms_norm(q.reshape(B, S, N_HEADS, HEAD_DIM), q_norm_g), positions)
    k = partial_rope(rms_norm(k.reshape(B, S, N_HEADS, HEAD_DIM), k_norm_g), positions)
    v = v.reshape(B, S, N_HEADS, HEAD_DIM)
    outs, dens = [], []
    for g, (window, dilation) in enumerate(ATTN_GROUPS):
        hs = slice(g * HEADS_PER_GROUP, (g + 1) * HEADS_PER_GROUP)
        o, ld = dilated_window_attention(q[:, :, hs], k[:, :, hs], v[:, :, hs], window, dilation)
        outs.append(o)
        dens.append(ld)
    wgt = jax.nn.softmax(jnp.stack(dens, axis=0), axis=0)
    o = jnp.sum(wgt[..., None] * jnp.stack(outs, axis=0), axis=0)
    return o.reshape(B, S, ATTN_OUT_WIDTH).astype(v.dtype)


def route(h, w_router, router_bias):
    T = h.shape[0]
    scores = jax.nn.sigmoid(jnp.einsum('td,de->te', h, w_router, preferred_element_type=jnp.float32))
    sel = (scores + router_bias.astype(jnp.float32)).reshape(T, N_EXPERT_GROUPS, N_EXPERTS // N_EXPERT_GROUPS)
    group_score = jnp.sum(lax.top_k(sel, 2)[0], axis=-1)
    _, top_groups = lax.top_k(group_score, TOPK_GROUPS)
    group_mask = jnp.any(top_groups[:, :, None] == jnp.arange(N_EXPERT_GROUPS)[None, None, :], axis=1)
    masked = jnp.where(group_mask[:, :, None], sel, -jnp.inf).reshape(T, N_EXPERTS)
    _, idx = lax.top_k(masked, TOP_K)
    w = jnp.take_along_axis(scores, idx, axis=1)
    gate = w / jnp.sum(w, axis=-1, keepdims=True) * ROUTED_SCALE
    return idx, gate


def routed_experts(h, idx, gate, w_gate, w_up, w_down):
    T, D = h.shape
    TK = T * TOP_K
    flat_e = idx.reshape(TK)
    flat_tok = jnp.arange(TK, dtype=jnp.int32) // TOP_K
    flat_w = gate.reshape(TK)
    order = jnp.argsort(flat_e)
    sorted_e = flat_e[order]
    counts = jnp.zeros((N_EXPERTS,), jnp.int32).at[flat_e].add(1)
    start = jnp.cumsum(counts) - counts
    padded = (counts + MOE_BLOCK - 1) // MOE_BLOCK * MOE_BLOCK
    padded_end = jnp.cumsum(padded)
    padded_start = padded_end - padded
    dest = padded_start[sorted_e] + (jnp.arange(TK, dtype=jnp.int32) - start[sorted_e])
    n_blocks = -(-TK // MOE_BLOCK) + N_EXPERTS
    R = n_blocks * MOE_BLOCK
    row_tok = jnp.full((R,), T, jnp.int32).at[dest].set(flat_tok[order])
    row_w = jnp.zeros((R,), jnp.float32).at[dest].set(flat_w[order])
    block_expert = jnp.minimum(jnp.searchsorted(padded_end, jnp.arange(n_blocks) * MOE_BLOCK, side='right'), N_EXPERTS - 1)
    h_pad = jnp.concatenate([h, jnp.zeros((1, D), h.dtype)], axis=0)

    def block_ffn(args):
        tok, w, e = args
        xb = h_pad[tok]
        a = xb @ w_gate[e]
        b = xb @ w_up[e]
        return ((jax.nn.silu(a) * b) @ w_down[e]).astype(jnp.float32) * w[:, None]

    y = lax.map(block_ffn, (row_tok.reshape(n_blocks, MOE_BLOCK), row_w.reshape(n_blocks, MOE_BLOCK), block_expert))
    out = jax.ops.segment_sum(y.reshape(R, D), row_tok, num_segments=T + 1)[:T]
    return out.astype(h.dtype)


def setup_inputs(seed: int = 0) -> dict:
    key = jax.random.key(seed)
    ks = jax.random.split(key, 24)
    D = D_MODEL

    def nrm(k, shape, scale):
        return jax.random.normal(k, shape, jnp.float32) * scale

    return {
        'x': nrm(ks[0], (BATCH, SEQ, D), 1.0),
        'c': nrm(ks[1], (BATCH, D), 1.0),
        'positions': jnp.broadcast_to(jnp.arange(SEQ, dtype=jnp.int32)[None, :], (BATCH, SEQ)),
        'w_ada': nrm(ks[2], (DEPTH, D, N_MOD * D), 0.5 * D ** -0.5),
        'b_ada': nrm(ks[3], (DEPTH, N_MOD * D), 0.02),
        'norm1_g': 1.0 + nrm(ks[4], (DEPTH, D), 0.02),
        'w_in': nrm(ks[5], (DEPTH, D, IN_WIDTH), D ** -0.5),
        'pool_w_grp': nrm(ks[6], (DEPTH, len(POOL_WINDOWS), POOL_GROUP, POOL_GROUP), POOL_GROUP ** -0.5),
        'pool_scale': 0.5 + nrm(ks[7], (DEPTH, POOL_WIDTH), 0.1),
        'q_norm_g': 1.0 + nrm(ks[8], (DEPTH, HEAD_DIM), 0.02),
        'k_norm_g': 1.0 + nrm(ks[9], (DEPTH, HEAD_DIM), 0.02),
        'w_pool_up': nrm(ks[10], (DEPTH, POOL_WIDTH, D), POOL_WIDTH ** -0.5),
        'w_attn_up': nrm(ks[11], (DEPTH, ATTN_OUT_WIDTH, D), ATTN_OUT_WIDTH ** -0.5),
        'w_out': nrm(ks[12], (DEPTH, D, D), D ** -0.5),
        'norm2_g': 1.0 + nrm(ks[13], (DEPTH, D), 0.02),
        'w_router': nrm(ks[14], (DEPTH, D, N_EXPERTS), D ** -0.5),
        'router_bias': nrm(ks[15], (DEPTH, N_EXPERTS), 0.01),
        'w_shared_gate': nrm(ks[16], (DEPTH, D, SHARED_FF), D ** -0.5),
        'w_shared_up': nrm(ks[17], (DEPTH, D, SHARED_FF), D ** -0.5),
        'w_shared_down': nrm(ks[18], (DEPTH, SHARED_FF, D), SHARED_FF ** -0.5),
        'w_exp_gate': nrm(ks[19], (DEPTH, N_EXPERTS, D, EXPERT_FF), D ** -0.5),
        'w_exp_up': nrm(ks[20], (DEPTH, N_EXPERTS, D, EXPERT_FF), D ** -0.5),
        'w_exp_down': nrm(ks[21], (DEPTH, N_EXPERTS, EXPERT_FF, D), EXPERT_FF ** -0.5),
    }


def reference(x, c, positions, w_ada, b_ada, norm1_g, w_in, pool_w_grp, pool_scale, q_norm_g, k_norm_g, w_pool_up, w_attn_up, w_out, norm2_g, w_router, router_bias, w_shared_gate, w_shared_up, w_shared_down, w_exp_gate, w_exp_up, w_exp_down):
    B, S, D = x.shape
    c_act = jax.nn.silu(c)
    for layer in range(DEPTH):
        mod = jnp.einsum('bd,de->be', c_act, w_ada[layer]) + b_ada[layer]
        shift1, scale1, gate1, shift2, scale2, gate2 = jnp.split(mod[:, None, :], N_MOD, axis=-1)

        h = rms_norm(x, norm1_g[layer]) * (1.0 + scale1) + shift1
        proj = jnp.einsum('bsd,de->bse', h, w_in[layer])
        u, q, k, v, g_pool, g_attn = jnp.split(proj, IN_SPLITS, axis=-1)
        y_pool = jnp.einsum('bsp,pd->bsd', pool_mixer(u, pool_w_grp[layer], pool_scale[layer]), w_pool_up[layer])
        y_attn = jnp.einsum('bsa,ad->bsd', attention_branch(q, k, v, q_norm_g[layer], k_norm_g[layer], positions), w_attn_up[layer])
        merged = jax.nn.sigmoid(g_pool) * y_pool + jax.nn.sigmoid(g_attn) * y_attn
        x = x + gate1 * jnp.einsum('bsd,de->bse', merged, w_out[layer])

        h2 = (rms_norm(x, norm2_g[layer]) * (1.0 + scale2) + shift2).reshape(B * S, D)
        idx, gate = route(h2, w_router[layer], router_bias[layer])
        routed = routed_experts(h2, idx, gate, w_exp_gate[layer], w_exp_up[layer], w_exp_down[layer])
        shared = (jax.nn.silu(h2 @ w_shared_gate[layer]) * (h2 @ w_shared_up[layer])) @ w_shared_down[layer]
        x = x + gate2 * (routed + shared).reshape(B, S, D)
    return x
```

```python
import contextlib
import math
import numpy as np
import ml_dtypes
import concourse.bass as bass
import concourse.mybir as mybir
from concourse.bass_utils import run_bass_kernel_spmd

F32 = mybir.dt.float32
F32R = mybir.dt.float32r
BF16 = mybir.dt.bfloat16
I32 = mybir.dt.int32
AF = mybir.ActivationFunctionType
ALU = mybir.AluOpType
AX = mybir.AxisListType

D = 1024
S = 2048
NB = 2
T = NB * S
NCORES = 8
EPS = 1e-6
IN_WIDTH = 4864
NE = 256
NBLK = T * 8 // 128 + NE
TWO_PI = 2.0 * math.pi
DENSE_TB = 2048


class KB:
    N_DMA_SEMS = 48

    def __init__(self, nc, stack):
        self.nc = nc
        self.stack = stack
        self.eng = dict(pe=nc.tensor, act=nc.scalar, dve=nc.vector, pool=nc.gpsimd, sp=nc.sync)
        self.csem = {}
        self.ccnt = {}
        for e in ("pe", "act", "dve", "pool"):
            self.csem[e] = stack.enter_context(nc.semaphore("c_" + e))
            self.ccnt[e] = 0
        self.dsem = [stack.enter_context(nc.semaphore("d_%d" % i)) for i in range(self.N_DMA_SEMS)]
        self.dcnt = [0] * self.N_DMA_SEMS
        self.drr = 0
        self.waited = {e: {} for e in self.eng}
        self.res = {}
        self.n_inst = 0

    def sb(self, name, shape, dt=F32, stack=None):
        self.n_inst += 1
        return (stack or self.stack).enter_context(self.nc.sbuf_tensor("%s_%d" % (name, self.n_inst), list(shape), dt))

    def ps(self, name, shape, dt=F32, stack=None):
        return (stack or self.stack).enter_context(self.nc.psum_tensor(name, list(shape), dt))

    def _st(self, key):
        s = self.res.get(key)
        if s is None:
            s = {"w": None, "r": []}
            self.res[key] = s
        return s

    def _need(self, engine, deps):
        e = self.eng[engine]
        for (sem, name, val, src) in deps:
            if src == engine and engine == "pe":
                continue
            if engine == "pool" and name.startswith("ix_"):
                continue
            if self.waited[engine].get(name, 0) >= val:
                continue
            e.wait_ge(sem, val)
            self.waited[engine][name] = val

    def _deps(self, reads, writes):
        deps = []
        for k in reads:
            s = self._st(k)
            if s["w"] is not None:
                deps.append(s["w"])
        for k in writes:
            s = self._st(k)
            if s["w"] is not None:
                deps.append(s["w"])
            deps.extend(s["r"])
        return deps

    def _record(self, tok, reads, writes):
        for k in reads:
            s = self._st(k)
            s["r"] = [r for r in s["r"] if r[1] != tok[1]] + [tok]
        for k in writes:
            s = self._st(k)
            s["w"] = tok
            s["r"] = []

    def op(self, engine, fn, reads=(), writes=(), inc=True):
        self._need(engine, self._deps(reads, writes))
        inst = fn()
        if inc:
            self.ccnt[engine] += 1
            inst.then_inc(self.csem[engine], 1)
            tok = (self.csem[engine], "c_" + engine, self.ccnt[engine], engine)
        else:
            tok = (self.csem[engine], "c_" + engine, self.ccnt[engine] + 1, engine)
        self._record(tok, reads, writes)
        self.n_inst += 1
        return inst

    def dma(self, queue, fn, reads=(), writes=()):
        i = self.drr
        self.drr = (self.drr + 1) % self.N_DMA_SEMS
        deps = self._deps(reads, writes)
        if self.dcnt[i] > 0:
            deps.append((self.dsem[i], "d_%d" % i, self.dcnt[i], "dma"))
        self._need(queue, deps)
        inst = fn()
        self.dcnt[i] += 16
        inst.then_inc(self.dsem[i], 16)
        tok = (self.dsem[i], "d_%d" % i, self.dcnt[i], "dma")
        self._record(tok, reads, writes)
        self.n_inst += 1
        return inst

    def _all(self):
        deps = []
        for i in range(self.N_DMA_SEMS):
            if self.dcnt[i] > 0:
                deps.append((self.dsem[i], "d_%d" % i, self.dcnt[i], "dma"))
        for e in ("pe", "act", "dve", "pool"):
            if self.ccnt[e] > 0:
                deps.append((self.csem[e], "c_" + e, self.ccnt[e], "x"))
        return deps

    def drain(self, engine="sp"):
        self._need(engine, self._all())

    def barrier(self):
        deps = self._all()
        for e in ("sp", "act", "dve", "pool", "pe"):
            self._need(e, [d for d in deps])
        self.res = {}


def _consts():
    c = {}
    c["ident"] = np.eye(128, dtype=np.float32)
    c["ident_bf"] = np.eye(128, dtype=np.float32).astype(ml_dtypes.bfloat16)
    bo = np.zeros((128, 128), np.float32)
    bo[:64, :64] = 1.0
    bo[64:, 64:] = 1.0
    c["blockones"] = bo.astype(ml_dtypes.bfloat16)
    rr = np.zeros((128, 128), np.float32)
    for o in (0, 64):
        for i in range(8):
            rr[o + i + 8, o + i] = -1.0
            rr[o + i, o + i + 8] = 1.0
    c["ropeR"] = rr.astype(ml_dtypes.bfloat16)
    invf = np.zeros((128, 1), np.float32)
    half = 8
    inv_freq = (500000.0 ** (-np.arange(half, dtype=np.float32) / half)).astype(np.float32)
    for p in range(128):
        d = p % 64
        if d < 16:
            invf[p, 0] = inv_freq[d % 8]
    c["invf"] = invf
    kk = np.arange(128)[:, None]
    jj = np.arange(256)[None, :]
    c["band"] = ((jj >= kk) & (jj <= kk + 128)).astype(np.float32).astype(ml_dtypes.bfloat16)
    pe = np.ones((128, 4, 16), np.float32)
    for g, R in enumerate((1, 2, 4, 8)):
        for t in range(R):
            pe[:, g, t] = 1.0 / (t + R + 1)
            pe[:, g, 8 + t] = 1.0 / (R + 1 + t)
    c["pooledge"] = pe
    tri = (np.arange(128)[:, None] < np.arange(128)[None, :]).astype(np.float32)
    c["tri"] = tri.astype(ml_dtypes.bfloat16)
    c["ones_bf"] = np.ones((128, 128), ml_dtypes.bfloat16)
    c["ones_f"] = np.ones((128, 128), np.float32)
    ec = np.zeros((128, 2), np.float32)
    ec[:, 0] = EPS
    ec[:, 1] = 64.0 * EPS
    c["epsc"] = ec
    thr = np.zeros((128, 4), np.float32)
    for j in range(4):
        thr[:, j] = 128.0 * (128 * j + np.arange(128))
    c["thr4"] = thr
    return c


CONST_SPECS = [("ident", [128, 128], F32), ("ident_bf", [128, 128], BF16), ("blockones", [128, 128], BF16), ("ropeR", [128, 128], BF16),
               ("invf", [128, 1], F32), ("band", [128, 256], BF16), ("pooledge", [128, 4, 16], F32),
               ("tri", [128, 128], BF16), ("ones_bf", [128, 128], BF16), ("ones_f", [128, 128], F32), ("epsc", [128, 2], F32), ("thr4", [128, 4], F32)]

W_SPECS = [("w_ada", [D, 6 * D]), ("b_ada", [1, 6 * D]), ("norm1_g", [1, D]), ("w_in", [D, IN_WIDTH]),
           ("pool_w_grp", [512, 128]), ("pool_scale", [1, 512]), ("q_norm_g", [1, 64]), ("k_norm_g", [1, 64]),
           ("w_pool_up", [512, D]), ("w_attn_up", [256, D]), ("w_out", [D, D]), ("norm2_g", [1, D]),
           ("w_router", [D, NE]), ("router_bias", [1, NE]), ("w_shared_gate", [D, 256]), ("w_shared_up", [D, 256]),
           ("w_shared_down", [256, D]), ("w_exp_gate", [NE, D, 256]), ("w_exp_up", [NE, D, 256]),
           ("w_exp_down", [NE, 256, D])]


def build_nc(stage=99, dbg=False):
    nc = bass.Bass("TRN2", target_bir_lowering=False)
    dram = {}
    dram["x"] = nc.dram_tensor("x", [T, D], F32, kind="ExternalInput").ap()
    dram["c"] = nc.dram_tensor("c", [NB, D], F32, kind="ExternalInput").ap()
    dram["positions"] = nc.dram_tensor("positions", [NB, S], I32, kind="ExternalInput").ap()
    for name, shape in W_SPECS:
        if stage < 6 and name.startswith("w_exp"):
            continue
        dram[name] = nc.dram_tensor(name, shape, F32, kind="ExternalInput").ap()
    for name, shape, dt in CONST_SPECS:
        dram[name] = nc.dram_tensor("k_" + name, shape, dt, kind="ExternalInput").ap()
    out = nc.dram_tensor("out", [T, D], F32, kind="ExternalOutput").ap()
    HTd = nc.dram_tensor("HTd", [NB, D, S], F32, kind="Internal").ap()
    PMd = nc.dram_tensor("PMd", [NB, 512, S], F32, kind="Internal").ap()
    AOd = nc.dram_tensor("AOd", [NB, 256, S], F32, kind="Internal").ap()
    BCd = nc.dram_tensor("BCd", [4, 128, NB * D], F32, kind="Internal").ap()
    H2d = nc.dram_tensor("H2d", [T, D], F32, kind="Internal").ap()
    Gd = nc.dram_tensor("Gd", [128, T // 128, NE], F32, kind="Internal").ap()
    XGd = Yd = BEXd = None
    if stage >= 7:
        XGd = nc.dram_tensor("XGd", [NBLK * 128, D], F32, kind="Internal").ap()
        Yd = nc.dram_tensor("Yd", [NBLK * 128, D], F32, kind="Internal").ap()
        BEXd = nc.dram_tensor("BEXd", [NBLK], I32, kind="Internal").ap()
    dbg_t = {}
    if dbg:
        dbg_t["hT"] = nc.dram_tensor("dbg_hT", [NB, D, S], F32, kind="ExternalOutput").ap()
        dbg_t["pm"] = nc.dram_tensor("dbg_pm", [NB, 512, S], F32, kind="ExternalOutput").ap()
        dbg_t["ao"] = nc.dram_tensor("dbg_ao", [NB, 256, S], F32, kind="ExternalOutput").ap()
        dbg_t["mod"] = nc.dram_tensor("dbg_mod", [128, 96], F32, kind="ExternalOutput").ap()
        dbg_t["G"] = nc.dram_tensor("dbg_G", [128, T // 128, NE], F32, kind="ExternalOutput").ap()
        if stage >= 7:
            dbg_t["dest"] = nc.dram_tensor("dbg_dest", [128, T // 128 * 8], I32, kind="ExternalOutput").ap()
            dbg_t["gk"] = nc.dram_tensor("dbg_gk", [128, T // 128 * 8], F32, kind="ExternalOutput").ap()
            dbg_t["bex"] = nc.dram_tensor("dbg_bex", [1, NBLK], I32, kind="ExternalOutput").ap()

    with contextlib.ExitStack() as st:
        kb = KB(nc, st)
        ncv, nca, ncp, ncg, ncs = nc.vector, nc.scalar, nc.tensor, nc.gpsimd, nc.sync

        cs = {}
        for name, shape, dt in CONST_SPECS:
            cs[name] = kb.sb("c_" + name, shape, dt)
            kb.dma("sp", lambda n=name: ncs.dma_start(out=cs[n][:], in_=dram[n]), writes=["c_" + name])
        ident = cs["ident"]

        modT = kb.sb("modT", [128, 48, NB])
        gs1 = kb.sb("gs1", [128, 8, NB])
        gs2 = kb.sb("gs2", [128, 8, NB])
        g1T = kb.sb("g1T", [128, 8])
        g2T = kb.sb("g2T", [128, 8])
        lsT = kb.sb("lsT", [128, 4])
        gq = kb.sb("gq", [128, 1])
        gk = kb.sb("gk", [128, 1])

        pA = [kb.ps("pA%d" % i, [128, 1024]) for i in range(2)]
        pB = [kb.ps("pB%d" % i, [128, 512]) for i in range(4)]

        with contextlib.ExitStack() as sa, nc.allow_non_contiguous_dma(reason="tiny transposed vector loads"):
            gate1_b = kb.sb("gate1_b", [128, NB, D], stack=sa)
            gate2_b = kb.sb("gate2_b", [128, NB, D], stack=sa)
            gs2_b = kb.sb("gs2_b", [128, NB, D], stack=sa)
            sh2_b = kb.sb("sh2_b", [128, NB, D], stack=sa)
            cact = kb.sb("cact", [128, 8, NB], stack=sa)
            crep = kb.sb("crep", [128, 8, NB, 128], stack=sa)
            badaT = kb.sb("badaT", [128, 48], stack=sa)
            bada_row = kb.sb("bada_row", [1, 6 * D], stack=sa)
            g2row_b = kb.sb("g2row_b", [128, D], stack=sa)
            wa = [kb.sb("wa%d" % i, [128, 8, 512], stack=sa) for i in range(2)]
            gtmp = kb.sb("gtmp", [128, 64], stack=sa)
            for b_ in range(NB):
                kb.dma("sp", lambda b_=b_: ncs.dma_start(out=cact[:, :, b_], in_=dram["c"][b_, :].rearrange("(k p) -> p k", p=128)), writes=["cact"])
            kb.dma("sp", lambda: ncs.dma_start(out=badaT[:], in_=dram["b_ada"].rearrange("o (j p) -> p (o j)", p=128)), writes=["badaT"])
            kb.dma("sp", lambda: ncs.dma_start(out=bada_row[:], in_=dram["b_ada"]), writes=["bada_row"])
            kb.dma("sp", lambda: ncs.dma_start(out=g1T[:], in_=dram["norm1_g"].rearrange("o (k p) -> p (o k)", p=128)), writes=["g1T"])
            kb.dma("sp", lambda: ncs.dma_start(out=g2T[:], in_=dram["norm2_g"].rearrange("o (k p) -> p (o k)", p=128)), writes=["g2T"])
            kb.dma("sp", lambda: ncs.dma_start(out=lsT[:], in_=dram["pool_scale"].rearrange("o (g p) -> p (o g)", p=128)), writes=["lsT"])
            kb.dma("sp", lambda: ncs.dma_start(out=g2row_b[:], in_=dram["norm2_g"].rearrange("o d -> (o d)").partition_broadcast(128)), writes=["g2row_b"])
            for h2_, (gt, nm) in enumerate(((gq, "q_norm_g"), (gk, "k_norm_g"))):
                for o in (0, 64):
                    kb.dma("sp", lambda gt=gt, nm=nm, o=o: ncs.dma_start(out=gt[o:o + 64, :], in_=dram[nm].rearrange("o d -> d o")),
                           writes=["gq" if gt is gq else "gk"])
            kb.op("dve", lambda: ncv.tensor_scalar(out=gq[:], in0=gq[:], scalar1=8.0, scalar2=None, op0=ALU.mult), reads=["gq"], writes=["gq"])
            kb.op("dve", lambda: ncv.tensor_scalar(out=gk[:], in0=gk[:], scalar1=8.0, scalar2=None, op0=ALU.mult), reads=["gk"], writes=["gk"])
            kb.op("act", lambda: nca.activation(out=cact[:], in_=cact[:], func=AF.Silu), reads=["cact"], writes=["cact"])
            for kc in range(8):
                for b in range(NB):
                    kb.op("dve", lambda kc=kc, b=b: ncv.tensor_copy(out=crep[:, kc, b, :], in_=cact[:, kc, b:b + 1].to_broadcast([128, 128])),
                          reads=["cact"], writes=["crep"])
            pm = pB[0]
            for t in range(12):
                w = wa[t % 2]
                wk = "wa%d" % (t % 2)
                kb.dma("sp" if t % 2 == 0 else "act",
                       lambda t=t, w=w: (ncs if t % 2 == 0 else nca).dma_start(
                           out=w[:], in_=dram["w_ada"][:, t * 512:(t + 1) * 512].rearrange("(k p) n -> p k n", p=128)),
                       writes=[wk])
                for jj in range(4):
                    j = 4 * t + jj
                    for kc in range(8):
                        kb.op("pe", lambda j=j, jj=jj, kc=kc, w=w: ncp.matmul(pm[:, 2 * j:2 * j + 2], w[:, kc, jj * 128:(jj + 1) * 128], cact[:, kc, :],
                                                                            start=(kc == 0), stop=(kc == 7)),
                              reads=[wk, "cact"], writes=["pB0"])
                if t in (4, 5, 10, 11):
                    dst = gate1_b if t in (4, 5) else gate2_b
                    dk = "gate1_b" if t in (4, 5) else "gate2_b"
                    half = t % 2 if t in (4, 5) else (t - 10)
                    for b in range(NB):
                        pg = pB[1 + b]
                        for kc in range(8):
                            kb.op("pe", lambda kc=kc, b=b, w=w, pg=pg: ncp.matmul(pg[:, :], crep[:, kc, b, :], w[:, kc, :], start=(kc == 0), stop=False),
                                  reads=[wk, "crep"], writes=["pB%d" % (1 + b)])
                        kb.op("pe", lambda t=t, pg=pg: ncp.matmul(pg[:, :], cs["ones_f"][0:1, :], bada_row[0:1, t * 512:(t + 1) * 512], start=False, stop=True),
                              reads=["bada_row", "c_ones_f"], writes=["pB%d" % (1 + b)])
                        kb.op("act", lambda b=b, pg=pg, dst=dst, half=half: nca.copy(out=dst[:, b, half * 512:(half + 1) * 512], in_=pg[:, :]),
                              reads=["pB%d" % (1 + b)], writes=[dk])
                if t in (6, 7, 8, 9):
                    dst = sh2_b if t in (6, 7) else gs2_b
                    dk = "sh2_b" if t in (6, 7) else "gs2_b"
                    half = t % 2
                    for b in range(NB):
                        pg = pB[1 + b]
                        for kc in range(8):
                            kb.op("pe", lambda kc=kc, b=b, w=w, pg=pg: ncp.matmul(pg[:, :], crep[:, kc, b, :], w[:, kc, :], start=(kc == 0), stop=False),
                                  reads=[wk, "crep"], writes=["pB%d" % (1 + b)])
                        kb.op("pe", lambda t=t, pg=pg: ncp.matmul(pg[:, :], cs["ones_f"][0:1, :], bada_row[0:1, t * 512:(t + 1) * 512], start=False, stop=True),
                              reads=["bada_row", "c_ones_f"], writes=["pB%d" % (1 + b)])
                        if t in (6, 7):
                            kb.op("act", lambda b=b, pg=pg, dst=dst, half=half: nca.copy(out=dst[:, b, half * 512:(half + 1) * 512], in_=pg[:, :]),
                                  reads=["pB%d" % (1 + b)], writes=[dk])
                        else:
                            kb.op("dve", lambda b=b, pg=pg, half=half: ncv.scalar_tensor_tensor(
                                out=gs2_b[:, b, half * 512:(half + 1) * 512], in0=pg[:, :], scalar=1.0,
                                in1=g2row_b[:, half * 512:(half + 1) * 512], op0=ALU.add, op1=ALU.mult),
                                reads=["pB%d" % (1 + b), "g2row_b"], writes=[dk])
            for b in range(NB):
                kb.op("dve", lambda b=b: ncv.tensor_tensor(out=modT[:, :, b], in0=pm[:, b:96:2], in1=badaT[:, :], op=ALU.add),
                      reads=["pB0", "badaT"], writes=["modT"])
                kb.op("dve", lambda b=b: ncv.scalar_tensor_tensor(out=gs1[:, :, b], in0=modT[:, 8:16, b], scalar=1.0, in1=g1T[:, :], op0=ALU.add, op1=ALU.mult),
                      reads=["modT", "g1T"], writes=["gs1"])
                kb.op("dve", lambda b=b: ncv.scalar_tensor_tensor(out=gs2[:, :, b], in0=modT[:, 32:40, b], scalar=1.0, in1=g2T[:, :], op0=ALU.add, op1=ALU.mult),
                      reads=["modT", "g2T"], writes=["gs2"])
            for i_, (t_, k_) in enumerate(((gate2_b, "gate2_b"), (gs2_b, "gs2_b"), (sh2_b, "sh2_b"), (gate1_b, "gate1_b"))):
                kb.dma("sp", lambda i_=i_, t_=t_: ncs.dma_start(out=BCd[i_], in_=t_[:].rearrange("p b d -> p (b d)")), reads=[k_], writes=["BCd"])
            if dbg:
                kb.dma("sp", lambda: ncs.dma_start(out=dbg_t["mod"], in_=modT[:].rearrange("p j b -> p (j b)")), reads=["modT"], writes=["dbg_mod"])
            kb.barrier()

        if stage >= 1:
            for b in range(NB):
                phase_B(nc, kb, b, dram, cs, dict(modT=modT, gs1=gs1, BCd=BCd, lsT=lsT, gq=gq, gk=gk),
                        pA, pB, HTd, PMd, AOd, out, dbg_t, stage)
        if stage >= 5:
            phase_C(nc, kb, dram, cs, dict(BCd=BCd, H2d=H2d, XGd=XGd, Yd=Yd, BEXd=BEXd, Gd=Gd), pA, pB, out, dbg_t, stage)
        kb.drain("sp")
    return nc


def phase_C(nc, kb, dram, cs, pv, pA, pB, out, dbg_t, stage):
    ncv, nca, ncp, ncg, ncs = nc.vector, nc.scalar, nc.tensor, nc.gpsimd, nc.sync
    ident = cs["ident"]
    BCd = pv["BCd"]
    with contextlib.ExitStack() as s5:
        s5a = contextlib.ExitStack()
        gate2_b = kb.sb("gate2_bc", [128, NB, D], stack=s5)
        NT = T // 128
        dense = stage < 7
        Gall = kb.sb("Gall", [128, NT, NE], stack=(s5a if dense else s5))
        Mall = None if dense else kb.sb("Mall", [128, NT, NE], BF16, stack=s5)
        gs2_b = kb.sb("gs2_bc", [128, NB, D], stack=s5a)
        sh2_b = kb.sb("sh2_bc", [128, NB, D], stack=s5a)
        for i_, (t_, k_) in enumerate(((gate2_b, "gate2_b"), (gs2_b, "gs2_b"), (sh2_b, "sh2_b"))):
            kb.dma("sp", lambda i_=i_, t_=t_: ncs.dma_start(out=t_[:].rearrange("p b d -> p (b d)"), in_=BCd[i_]), reads=["BCd"], writes=[k_])
        wsgu = kb.sb("wsgu", [128, 8, 512], F32R, stack=s5a)
        wsd = kb.sb("wsd", [128, 2, D], F32R, stack=s5a)
        kb.dma("pool", lambda: ncg.dma_start(out=wsgu[:, :, 0:256], in_=dram["w_shared_gate"].rearrange("(k p) n -> p k n", p=128)), writes=["wsgu"])
        kb.dma("pool", lambda: ncg.dma_start(out=wsgu[:, :, 256:512], in_=dram["w_shared_up"].rearrange("(k p) n -> p k n", p=128)), writes=["wsgu"])
        kb.dma("pool", lambda: ncg.dma_start(out=wsd[:], in_=dram["w_shared_down"].rearrange("(k p) n -> p k n", p=128)), writes=["wsd"])
        NT = T // 128
        H2d = pv["H2d"]
        wr = kb.sb("wr", [128, 8, NE], F32R, stack=s5a)
        kb.dma("pool", lambda: ncg.dma_start(out=wr[:], in_=dram["w_router"].rearrange("(k p) n -> p k n", p=128)), writes=["wr"])
        rbias = kb.sb("rbias", [128, NE], stack=s5a)
        kb.dma("sp", lambda: ncs.dma_start(out=rbias[:], in_=dram["router_bias"].rearrange("o d -> (o d)").partition_broadcast(128)), writes=["rbias"])
        sc = kb.sb("sc", [128, NE], stack=s5a)
        sel = kb.sb("sel", [128, NE], stack=s5a)
        msk = kb.sb("msk", [128, NE], stack=s5a)
        selm = kb.sb("selm", [128, NE], stack=s5a)
        wtmp = kb.sb("wtmp", [128, NE], stack=s5a)
        m8g = kb.sb("m8g", [128, 8, 8], stack=s5a)
        gsc = kb.sb("gsc", [128, 8], stack=s5a)
        m8 = kb.sb("m8", [128, 8], stack=s5a)
        gm = kb.sb("gm", [128, 8], stack=s5a)
        pen = kb.sb("pen", [128, 8], stack=s5a)
        wsum = kb.sb("wsum", [128, 1], stack=s5a)
        x1 = [kb.sb("x1t%d" % i, [128, D], stack=s5a) for i in range(2)]
        xn = [kb.sb("xn2%d" % i, [128, D], stack=s5a) for i in range(2)]
        h2 = [kb.sb("h2t%d" % i, [128, D], stack=s5a) for i in range(2)]
        h2T = [kb.sb("h2T%d" % i, [128, 8, 128], F32R, stack=s5a) for i in range(2)]
        junk = kb.sb("junk2", [128, D], stack=s5a)
        ss = kb.sb("ss2", [128, 2], stack=s5a)
        rstd = kb.sb("rstd2", [128, 2], stack=s5a)
        sg = [kb.sb("sg%d" % i, [128, 256], stack=s5a) for i in range(2)]
        act = [kb.sb("actt%d" % i, [128, 256], stack=s5a) for i in range(2)]
        actT = [kb.sb("actT%d" % i, [128, 2, 128], F32R, stack=s5a) for i in range(2)]
        ot = [kb.sb("ot%d" % i, [128, D], stack=s5a) for i in range(2)]
        for tt in range(T // 128):
            i = tt % 2
            b = tt // (S // 128)
            r0 = tt * 128
            kb.dma("sp", lambda i=i, r0=r0: ncs.dma_start(out=x1[i][:], in_=out[r0:r0 + 128, :]), reads=["out"], writes=["x1t%d" % i])
            kb.op("act", lambda i=i: nca.activation(out=junk[:], in_=x1[i][:], func=AF.Square, accum_out=ss[:, i:i + 1]),
                  reads=["x1t%d" % i], writes=["junk2", "ss2%d" % i])
            kb.op("act", lambda i=i: nca.activation(out=rstd[:, i:i + 1], in_=ss[:, i:i + 1], func=AF.Sqrt, scale=1.0 / D, bias=cs["epsc"][:, 0:1]),
                  reads=["ss2%d" % i, "c_epsc"], writes=["rstd2%d" % i])
            kb.op("dve", lambda i=i: ncv.reciprocal(out=rstd[:, i:i + 1], in_=rstd[:, i:i + 1]), reads=["rstd2%d" % i], writes=["rstd2%d" % i])
            kb.op("act", lambda i=i: nca.activation(out=xn[i][:], in_=x1[i][:], func=AF.Identity, scale=rstd[:, i:i + 1]),
                  reads=["x1t%d" % i, "rstd2%d" % i], writes=["xn2%d" % i])
            kb.op("dve", lambda i=i, b=b: ncv.tensor_tensor(out=h2[i][:], in0=xn[i][:], in1=gs2_b[:, b, :], op=ALU.mult),
                  reads=["xn2%d" % i, "gs2_b"], writes=["h2t%d" % i])
            kb.op("pool", lambda i=i, b=b: ncg.tensor_tensor(out=h2[i][:], in0=h2[i][:], in1=sh2_b[:, b, :], op=ALU.add),
                  reads=["h2t%d" % i, "sh2_b"], writes=["h2t%d" % i])
            pa, pak = pA[i], "pA%d" % i
            for kc in range(8):
                kb.op("pe", lambda kc=kc, i=i, pa=pa: ncp.transpose(pa[:, kc * 128:(kc + 1) * 128], h2[i][:, kc * 128:(kc + 1) * 128], ident[:]),
                      reads=["h2t%d" % i, "c_ident"], writes=[pak])
            kb.op("act", lambda i=i, pa=pa: nca.copy(out=h2T[i][:, 0:4, :], in_=pa[:, 0:512].rearrange("p (k t) -> p k t", k=4)), reads=[pak], writes=["h2T%d" % i])
            kb.op("dve", lambda i=i, pa=pa: ncv.tensor_copy(out=h2T[i][:, 4:8, :], in_=pa[:, 512:1024].rearrange("p (k t) -> p k t", k=4)), reads=[pak], writes=["h2T%d" % i])
            kb.dma("sp", lambda i=i, r0=r0: ncs.dma_start(out=H2d[r0:r0 + 128, :], in_=h2[i][:]), reads=["h2t%d" % i], writes=["H2d"])
            pr, prk = pB[2 + i], "pB%d" % (2 + i)
            for kc in range(8):
                kb.op("pe", lambda kc=kc, i=i, pr=pr: ncp.matmul(pr[:, 0:NE], h2T[i][:, kc, :], wr[:, kc, :], start=(kc == 0), stop=(kc == 7)),
                      reads=["h2T%d" % i, "wr"], writes=[prk])
            kb.op("act", lambda pr=pr: nca.activation(out=sc[:], in_=pr[:, 0:NE], func=AF.Sigmoid), reads=[prk], writes=["sc"])
            kb.op("dve", lambda: ncv.tensor_tensor(out=sel[:], in0=sc[:], in1=rbias[:], op=ALU.add), reads=["sc", "rbias"], writes=["sel"])
            for g in range(8):
                kb.op("dve", lambda g=g: ncv.max(out=m8g[:, g, :], in_=sel[:, g * 32:(g + 1) * 32]), reads=["sel"], writes=["m8g"])
            kb.op("dve", lambda: ncv.tensor_tensor(out=gsc[:], in0=m8g[:, :, 0], in1=m8g[:, :, 1], op=ALU.add), reads=["m8g"], writes=["gsc"])
            kb.op("dve", lambda: ncv.max(out=m8[:], in_=gsc[:]), reads=["gsc"], writes=["m8"])
            kb.op("dve", lambda: ncv.tensor_scalar(out=gm[:], in0=gsc[:], scalar1=m8[:, 3:4], scalar2=None, op0=ALU.is_ge), reads=["gsc", "m8"], writes=["gm"])
            kb.op("dve", lambda: ncv.tensor_scalar(out=pen[:], in0=gm[:], scalar1=-1.0, scalar2=1.0e4, op0=ALU.add, op1=ALU.mult), reads=["gm"], writes=["pen"])
            for g in range(8):
                kb.op("dve", lambda g=g: ncv.tensor_scalar(out=msk[:, g * 32:(g + 1) * 32], in0=sel[:, g * 32:(g + 1) * 32], scalar1=gm[:, g:g + 1],
                                                          scalar2=pen[:, g:g + 1], op0=ALU.mult, op1=ALU.add),
                      reads=["sel", "gm", "pen"], writes=["msk"])
            kb.op("dve", lambda: ncv.max(out=m8[:], in_=msk[:]), reads=["msk"], writes=["m8"])
            kb.op("dve", lambda: ncv.tensor_scalar(out=selm[:], in0=msk[:], scalar1=m8[:, 7:8], scalar2=None, op0=ALU.is_ge), reads=["msk", "m8"], writes=["selm"])
            kb.op("dve", lambda: ncv.scalar_tensor_tensor(out=wtmp[:], in0=sc[:], scalar=1.0, in1=selm[:], op0=ALU.mult, op1=ALU.mult, accum_out=wsum[:, 0:1]),
                  reads=["sc", "selm"], writes=["wtmp", "wsum"])
            kb.op("dve", lambda: ncv.reciprocal(out=wsum[:], in_=wsum[:]), reads=["wsum"], writes=["wsum"])
            kb.op("dve", lambda tt=tt: ncv.tensor_scalar(out=Gall[:, tt, :], in0=wtmp[:], scalar1=wsum[:, 0:1], scalar2=2.5, op0=ALU.mult, op1=ALU.mult),
                  reads=["wtmp", "wsum"], writes=["Gall"])
            if Mall is not None:
                kb.op("pool", lambda tt=tt: ncg.tensor_copy(out=Mall[:, tt, :], in_=selm[:]), reads=["selm"], writes=["Mall"])
            pg, pgk = pB[i], "pB%d" % i
            for kc in range(8):
                kb.op("pe", lambda kc=kc, i=i, pg=pg: ncp.matmul(pg[:, :], h2T[i][:, kc, :], wsgu[:, kc, :], start=(kc == 0), stop=(kc == 7)),
                      reads=["h2T%d" % i, "wsgu"], writes=[pgk])
            kb.op("act", lambda i=i, pg=pg: nca.activation(out=sg[i][:], in_=pg[:, 0:256], func=AF.Silu), reads=[pgk], writes=["sg%d" % i])
            kb.op("dve", lambda i=i, pg=pg: ncv.tensor_tensor(out=act[i][:], in0=sg[i][:], in1=pg[:, 256:512], op=ALU.mult), reads=[pgk, "sg%d" % i], writes=["actt%d" % i])
            pt, ptk = pB[2 + i], "pB%d" % (2 + i)
            for j in range(2):
                kb.op("pe", lambda j=j, i=i, pt=pt: ncp.transpose(pt[:, j * 128:(j + 1) * 128], act[i][:, j * 128:(j + 1) * 128], ident[:]),
                      reads=["actt%d" % i, "c_ident"], writes=[ptk])
            kb.op("act", lambda i=i, pt=pt: nca.copy(out=actT[i][:, :, :], in_=pt[:, 0:256].rearrange("p (k t) -> p k t", k=2)), reads=[ptk], writes=["actT%d" % i])
            for hf in range(2):
                for j in range(2):
                    kb.op("pe", lambda j=j, hf=hf, i=i, pa=pa: ncp.matmul(pa[:, hf * 512:(hf + 1) * 512], actT[i][:, j, :], wsd[:, j, hf * 512:(hf + 1) * 512],
                                                                         start=(j == 0), stop=(j == 1)),
                          reads=["actT%d" % i, "wsd"], writes=[pak])
            kb.op("dve", lambda i=i, b=b, pa=pa: ncv.tensor_tensor(out=ot[i][:], in0=pa[:, :], in1=gate2_b[:, b, :], op=ALU.mult),
                  reads=[pak, "gate2_b"], writes=["ot%d" % i])
            kb.op("pool", lambda i=i: ncg.tensor_tensor(out=ot[i][:], in0=ot[i][:], in1=x1[i][:], op=ALU.add),
                  reads=["ot%d" % i, "x1t%d" % i], writes=["ot%d" % i])
            kb.dma("sp", lambda i=i, r0=r0: ncs.dma_start(out=out[r0:r0 + 128, :], in_=ot[i][:]), reads=["ot%d" % i], writes=["out"])
        if dbg_t:
            kb.dma("sp", lambda: ncs.dma_start(out=dbg_t["G"], in_=Gall[:]), reads=["Gall"], writes=["dbg_G"])
        if dense:
            kb.dma("sp", lambda: ncs.dma_start(out=pv["Gd"], in_=Gall[:]), reads=["Gall"], writes=["Gd"])
        kb.barrier()
        s5a.close()
        if stage >= 6:
            if stage >= 7:
                phase_R(nc, kb, dram, cs, pv, pA, pB, out, gate2_b, Gall, Mall, dbg_t)
            else:
                phase_R_dense(nc, kb, dram, cs, pv, pA, pB, out, gate2_b)


def phase_R_dense(nc, kb, dram, cs, pv, pA, pB, out, gate2_b):
    ncv, nca, ncp, ncg, ncs = nc.vector, nc.scalar, nc.tensor, nc.gpsimd, nc.sync
    ident = cs["ident"]
    H2d, Gd = pv["H2d"], pv["Gd"]
    NS = DENSE_TB // 128
    with contextlib.ExitStack() as s4:
        wgu = [kb.sb("wgu%d" % i, [128, 8, 512], F32R, stack=s4) for i in range(2)]
        wdn = [kb.sb("wdn0", [128, 2, D], F32R, stack=s4)] * 2
        Gb = kb.sb("Gb", [128, NS, NE], stack=s4)
        h2T = kb.sb("h2Tb", [128, 8, DENSE_TB], F32R, stack=s4)
        acc = [kb.sb("accd%d" % i, [128, D], stack=s4) for i in range(NS)]
        sg = [kb.sb("sgr%d" % i, [128, 256], stack=s4) for i in range(2)]
        act = [kb.sb("actr%d" % i, [128, 256], stack=s4) for i in range(2)]
        actT = [kb.sb("actTr%d" % i, [128, 2, 128], F32R, stack=s4) for i in range(2)]
        ot = [kb.sb("otr0", [128, D], stack=s4)] * 2
        for tb in range(T // DENSE_TB):
            kb.dma("sp", lambda tb=tb: ncs.dma_start(out=Gb[:], in_=Gd[:, tb * NS:(tb + 1) * NS, :]), reads=["Gd"], writes=["Gb"])
            for sidx in range(NS):
                i = tb * NS + sidx
                j = 0
                kb.dma("sp", lambda i=i, j=j: ncs.dma_start(out=ot[j][:], in_=H2d[i * 128:(i + 1) * 128, :]), reads=["H2d"], writes=["otr%d" % j])
                pa, pak = pA[sidx % 2], "pA%d" % (sidx % 2)
                for kc in range(8):
                    kb.op("pe", lambda kc=kc, j=j, pa=pa: ncp.transpose(pa[:, kc * 128:(kc + 1) * 128], ot[j][:, kc * 128:(kc + 1) * 128], ident[:]),
                          reads=["otr%d" % j, "c_ident"], writes=[pak], inc=(kc == 7))
                kb.op("act", lambda sidx=sidx, pa=pa: nca.copy(out=h2T[:, 0:4, sidx * 128:(sidx + 1) * 128], in_=pa[:, 0:512].rearrange("p (k t) -> p k t", k=4)),
                      reads=[pak], writes=["h2Tb"])
                kb.op("dve", lambda sidx=sidx, pa=pa: ncv.tensor_copy(out=h2T[:, 4:8, sidx * 128:(sidx + 1) * 128], in_=pa[:, 512:1024].rearrange("p (k t) -> p k t", k=4)),
                      reads=[pak], writes=["h2Tb"])
                kb.op("pool", lambda sidx=sidx: ncg.memset(acc[sidx][:], 0.0), writes=["accd%d" % sidx])
            for e in range(NE):
                w = e % 2
                kb.dma("pool", lambda w=w, e=e: ncg.dma_start(out=wgu[w][:, :, 0:256], in_=dram["w_exp_gate"][e].rearrange("(k p) n -> p k n", p=128)), writes=["wgu%d" % w])
                kb.dma("pool", lambda w=w, e=e: ncg.dma_start(out=wgu[w][:, :, 256:512], in_=dram["w_exp_up"][e].rearrange("(k p) n -> p k n", p=128)), writes=["wgu%d" % w])
                kb.dma("pool", lambda w=w, e=e: ncg.dma_start(out=wdn[w][:], in_=dram["w_exp_down"][e].rearrange("(k p) n -> p k n", p=128)), writes=["wdn0"])
                for sidx in range(NS):
                    u = sidx % 2
                    pg, pgk = pB[u], "pB%d" % u
                    for kc in range(8):
                        kb.op("pe", lambda kc=kc, sidx=sidx, w=w, pg=pg: ncp.matmul(pg[:, :], h2T[:, kc, sidx * 128:(sidx + 1) * 128], wgu[w][:, kc, :], start=(kc == 0), stop=(kc == 7)),
                              reads=["h2Tb", "wgu%d" % w], writes=[pgk], inc=(kc == 7))
                    kb.op("act", lambda u=u, pg=pg: nca.activation(out=sg[u][:], in_=pg[:, 0:256], func=AF.Silu), reads=[pgk], writes=["sgr%d" % u])
                    kb.op("dve", lambda u=u, pg=pg: ncv.tensor_tensor(out=act[u][:], in0=sg[u][:], in1=pg[:, 256:512], op=ALU.mult), reads=[pgk, "sgr%d" % u], writes=["actr%d" % u])
                    pt, ptk = pB[2 + u], "pB%d" % (2 + u)
                    for j in range(2):
                        kb.op("pe", lambda j=j, u=u, pt=pt: ncp.transpose(pt[:, j * 128:(j + 1) * 128], act[u][:, j * 128:(j + 1) * 128], ident[:]),
                              reads=["actr%d" % u, "c_ident"], writes=[ptk], inc=(j == 1))
                    kb.op("act", lambda u=u, pt=pt: nca.copy(out=actT[u][:, :, :], in_=pt[:, 0:256].rearrange("p (k t) -> p k t", k=2)), reads=[ptk], writes=["actTr%d" % u])
                    pa, pak = pA[u], "pA%d" % u
                    for hf in range(2):
                        for j in range(2):
                            kb.op("pe", lambda j=j, hf=hf, u=u, w=w, pa=pa: ncp.matmul(pa[:, hf * 512:(hf + 1) * 512], actT[u][:, j, :], wdn[w][:, j, hf * 512:(hf + 1) * 512],
                                                                                   start=(j == 0), stop=(j == 1)),
                                  reads=["actTr%d" % u, "wdn0"], writes=[pak], inc=(hf == 1 and j == 1))
                    kb.op("dve", lambda sidx=sidx, e=e, pa=pa: ncv.scalar_tensor_tensor(out=acc[sidx][:], in0=pa[:, :], scalar=Gb[:, sidx, e:e + 1], in1=acc[sidx][:],
                                                                                      op0=ALU.mult, op1=ALU.add),
                          reads=[pak, "Gb", "accd%d" % sidx], writes=["accd%d" % sidx])
            for sidx in range(NS):
                i = tb * NS + sidx
                j = 0
                b = i // (S // 128)
                kb.dma("sp", lambda i=i, j=j: ncs.dma_start(out=ot[j][:], in_=out[i * 128:(i + 1) * 128, :]), reads=["out"], writes=["otr%d" % j])
                kb.op("pool", lambda sidx=sidx, b=b: ncg.tensor_tensor(out=acc[sidx][:], in0=acc[sidx][:], in1=gate2_b[:, b, :], op=ALU.mult),
                      reads=["accd%d" % sidx, "gate2_b"], writes=["accd%d" % sidx])
                kb.op("pool", lambda sidx=sidx, j=j: ncg.tensor_tensor(out=ot[j][:], in0=ot[j][:], in1=acc[sidx][:], op=ALU.add),
                      reads=["accd%d" % sidx, "otr%d" % j], writes=["otr%d" % j])
                kb.dma("sp", lambda i=i, j=j: ncs.dma_start(out=out[i * 128:(i + 1) * 128, :], in_=ot[j][:]), reads=["otr%d" % j], writes=["out"])
        kb.barrier()


def phase_R(nc, kb, dram, cs, pv, pA, pB, out, gate2_b, Gall, Mall, dbg_t):
    ncv, nca, ncp, ncg, ncs = nc.vector, nc.scalar, nc.tensor, nc.gpsimd, nc.sync
    ident = cs["ident"]
    NT = T // 128
    R = NBLK * 128
    BIGK = 70000.0
    H2d, XGd, Yd, BEXd = pv["H2d"], pv["XGd"], pv["Yd"], pv["BEXd"]
    with contextlib.ExitStack() as sr:
        DESTi = kb.sb("DESTi", [128, NT * 8], I32, stack=sr)
        GK = kb.sb("GK", [128, NT * 8], stack=sr)
        bexrow = kb.sb("bexrow", [1, NBLK], I32, stack=sr)
        with contextlib.ExitStack() as s2:
            RANK = kb.sb("RANK", [128, NT, NE], stack=s2)
            base = kb.sb("base", [128, NE], stack=s2)
            nbk = kb.sb("nbk", [128, NE], stack=s2)
            padded = kb.sb("padded", [128, NE], stack=s2)
            cA = kb.sb("cA", [128, NE], stack=s2)
            cB = kb.sb("cB", [128, NE], stack=s2)
            key = kb.sb("key", [128, NE], stack=s2)
            jk = kb.sb("jk", [128, NE], stack=s2)
            ones256 = kb.sb("ones256", [128, NE], stack=s2)
            m8 = kb.sb("m8r", [128, 8], stack=s2)
            destf = kb.sb("destf", [128, NT * 8], stack=s2)
            bx = kb.sb("bx", [128, 4], stack=s2)
            bxi = kb.sb("bxi", [128, 4], I32, stack=s2)
            kb.op("pool", lambda: ncg.memset(base[:], 0.0), writes=["base"])
            kb.op("pool", lambda: ncg.memset(nbk[:], 0.0), writes=["nbk"])
            kb.op("pool", lambda: ncg.memset(ones256[:], 1.0), writes=["ones256"])
            for i in range(NT):
                pt, ptk = pB[i % 2], "pB%d" % (i % 2)
                kb.op("pe", lambda i=i, pt=pt: ncp.matmul(pt[:, 0:NE], cs["tri"][:], Mall[:, i, :], start=True, stop=True), reads=["Mall", "c_tri"], writes=[ptk])
                kb.op("pe", lambda i=i, pt=pt: ncp.matmul(pt[:, NE:2 * NE], cs["ones_bf"][:], Mall[:, i, :], start=True, stop=True), reads=["Mall", "c_ones_bf"], writes=[ptk])
                kb.op("dve", lambda i=i, pt=pt: ncv.tensor_tensor(out=RANK[:, i, :], in0=pt[:, 0:NE], in1=base[:], op=ALU.add), reads=[ptk, "base"], writes=["RANK"])
                kb.op("dve", lambda pt=pt: ncv.tensor_tensor(out=base[:], in0=base[:], in1=pt[:, NE:2 * NE], op=ALU.add), reads=[ptk, "base"], writes=["base"])
            for k in range(T // 128):
                kb.op("dve", lambda k=k: ncv.scalar_tensor_tensor(out=nbk[:], in0=base[:], scalar=128.0 * k, in1=nbk[:], op0=ALU.is_gt, op1=ALU.add),
                      reads=["base", "nbk"], writes=["nbk"])
            kb.op("dve", lambda: ncv.tensor_scalar(out=padded[:], in0=nbk[:], scalar1=128.0, scalar2=None, op0=ALU.mult), reads=["nbk"], writes=["padded"])
            kb.op("dve", lambda: ncv.tensor_copy(out=cA[:], in_=padded[:]), reads=["padded"], writes=["cA"])
            cur, curk, nxt, nxtk = cA, "cA", cB, "cB"
            sft = 1
            while sft < NE:
                kb.op("dve", lambda cur=cur, nxt=nxt, sft=sft: ncv.tensor_copy(out=nxt[:, 0:sft], in_=cur[:, 0:sft]), reads=[curk], writes=[nxtk])
                kb.op("dve", lambda cur=cur, nxt=nxt, sft=sft: ncv.tensor_tensor(out=nxt[:, sft:NE], in0=cur[:, sft:NE], in1=cur[:, 0:NE - sft], op=ALU.add),
                      reads=[curk], writes=[nxtk])
                cur, curk, nxt, nxtk = nxt, nxtk, cur, curk
                sft *= 2
            pend, pendk = cur, curk
            pstart, pstartk = nxt, nxtk
            kb.op("dve", lambda: ncv.tensor_tensor(out=pstart[:], in0=pend[:], in1=padded[:], op=ALU.subtract), reads=[pendk, "padded"], writes=[pstartk])
            for i in range(NT):
                kb.op("dve", lambda i=i: ncv.tensor_tensor(out=key[:], in0=RANK[:, i, :], in1=pstart[:], op=ALU.add), reads=["RANK", pstartk], writes=["key"])
                kb.op("dve", lambda: ncv.tensor_scalar(out=key[:], in0=key[:], scalar1=-1.0, scalar2=BIGK + 1.0, op0=ALU.mult, op1=ALU.add), reads=["key"], writes=["key"])
                kb.op("dve", lambda i=i: ncv.tensor_tensor(out=key[:], in0=key[:], in1=Mall[:, i, :], op=ALU.mult), reads=["key", "Mall"], writes=["key"])
                kb.op("dve", lambda: ncv.max(out=m8[:], in_=key[:]), reads=["key"], writes=["m8r"])
                kb.op("dve", lambda i=i: ncv.tensor_scalar(out=destf[:, i * 8:(i + 1) * 8], in0=m8[:], scalar1=-1.0, scalar2=BIGK + 1.0, op0=ALU.mult, op1=ALU.add),
                      reads=["m8r"], writes=["destf"])
                for k in range(8):
                    kb.op("dve", lambda i=i, k=k: ncv.scalar_tensor_tensor(out=jk[:], in0=key[:], scalar=m8[:, k:k + 1], in1=Gall[:, i, :], op0=ALU.is_equal, op1=ALU.mult,
                                                                             accum_out=GK[:, i * 8 + k:i * 8 + k + 1]),
                          reads=["key", "m8r", "Gall"], writes=["jk", "GK"])
            kb.op("dve", lambda: ncv.tensor_copy(out=DESTi[:], in_=destf[:]), reads=["destf"], writes=["DESTi"])
            for j in range(4):
                kb.op("dve", lambda j=j: ncv.scalar_tensor_tensor(out=jk[:], in0=pend[:], scalar=cs["thr4"][:, j:j + 1], in1=ones256[:], op0=ALU.is_le, op1=ALU.mult,
                                                                  accum_out=bx[:, j:j + 1]),
                      reads=[pendk, "c_thr4", "ones256"], writes=["jk", "bx"])
            kb.op("dve", lambda: ncv.tensor_scalar(out=bx[:], in0=bx[:], scalar1=float(NE - 1), scalar2=None, op0=ALU.min), reads=["bx"], writes=["bx"])
            kb.op("dve", lambda: ncv.tensor_copy(out=bxi[:], in_=bx[:]), reads=["bx"], writes=["bxi"])
            with nc.allow_non_contiguous_dma(reason="tiny block->expert table transpose"):
                kb.dma("sp", lambda: ncs.dma_start(out=BEXd.rearrange("(j p) -> p j", p=128), in_=bxi[:]), reads=["bxi"], writes=["BEXd"])
            kb.dma("sp", lambda: ncs.dma_start(out=bexrow[:], in_=BEXd.rearrange("(o n) -> o n", o=1)), reads=["BEXd"], writes=["bexrow"])
            if dbg_t:
                kb.dma("sp", lambda: ncs.dma_start(out=dbg_t["dest"], in_=DESTi[:]), reads=["DESTi"], writes=["dbg_dest"])
                kb.dma("sp", lambda: ncs.dma_start(out=dbg_t["gk"], in_=GK[:]), reads=["GK"], writes=["dbg_gk"])
                kb.dma("sp", lambda: ncs.dma_start(out=dbg_t["bex"], in_=bexrow[:]), reads=["bexrow"], writes=["dbg_bex"])
            kb.barrier()
        ssem = kb.stack.enter_context(nc.semaphore("ix_scatter"))
        with contextlib.ExitStack() as s3:
            h2all = kb.sb("h2all", [128, NT, D], stack=s3)
            for i in range(NT):
                kb.dma("sp" if i % 2 == 0 else "act", lambda i=i: (ncs if i % 2 == 0 else nca).dma_start(out=h2all[:, i, :], in_=H2d[i * 128:(i + 1) * 128, :]),
                       reads=["H2d"], writes=["h2all%d" % i])
            nsc = 0
            for i in range(NT):
                kb._need("pool", kb._deps(["h2all%d" % i, "DESTi"], []))
                for k in range(8):
                    ncg.indirect_dma_start(
                        out=XGd[:, :], out_offset=bass.IndirectOffsetOnAxis(ap=DESTi[:, i * 8 + k:i * 8 + k + 1], axis=0),
                        in_=h2all[:, i, :], in_offset=None, bounds_check=R - 1, oob_is_err=False).then_inc(ssem, 16)
                    nsc += 1
            tok = (ssem, "ix_scatter", 16 * nsc, "dma")
            kb._record(tok, ["h2all%d" % i for i in range(NT)] + ["DESTi"], ["XGd"])
            kb.dma("sp", lambda: ncs.dma_start(out=BEXd[0:1], in_=BEXd[0:1]), reads=["XGd"], writes=["relay"])
            kb.barrier()
            kb._record(tok, [], ["XGd"])
        with contextlib.ExitStack() as s4:
            wgu = [kb.sb("wgu%d" % i, [128, 8, 512], F32R, stack=s4) for i in range(2)]
            wdn = [kb.sb("wdn%d" % i, [128, 2, D], F32R, stack=s4) for i in range(2)]
            xg = [kb.sb("xg%d" % i, [128, D], stack=s4) for i in range(2)]
            xgT = [kb.sb("xgT%d" % i, [128, 8, 128], F32R, stack=s4) for i in range(2)]
            sg = [kb.sb("sgr%d" % i, [128, 256], stack=s4) for i in range(2)]
            act = [kb.sb("actr%d" % i, [128, 256], stack=s4) for i in range(2)]
            actT = [kb.sb("actTr%d" % i, [128, 2, 128], F32R, stack=s4) for i in range(2)]
            yt = [kb.sb("yt%d" % i, [128, D], stack=s4) for i in range(2)]
            for blk in range(NBLK):
                i = blk % 2
                kb._need("pool", kb._deps(["bexrow"], ["wgu%d" % i, "wdn%d" % i]))
                e = ncg.value_load(bexrow[0:1, blk:blk + 1], min_val=0, max_val=NE - 1)
                kb.dma("pool", lambda i=i, e=e: ncg.dma_start(out=wgu[i][:, :, 0:256], in_=dram["w_exp_gate"][bass.ds(e, 1), :, :].rearrange("o (k p) n -> p (o k) n", p=128)),
                       reads=["bexrow"], writes=["wgu%d" % i])
                kb.dma("pool", lambda i=i, e=e: ncg.dma_start(out=wgu[i][:, :, 256:512], in_=dram["w_exp_up"][bass.ds(e, 1), :, :].rearrange("o (k p) n -> p (o k) n", p=128)),
                       reads=["bexrow"], writes=["wgu%d" % i])
                kb.dma("pool", lambda i=i, e=e: ncg.dma_start(out=wdn[i][:], in_=dram["w_exp_down"][bass.ds(e, 1), :, :].rearrange("o (k p) n -> p (o k) n", p=128)),
                       reads=["bexrow"], writes=["wdn%d" % i])
                kb.dma("sp", lambda i=i, blk=blk: ncs.dma_start(out=xg[i][:], in_=XGd[blk * 128:(blk + 1) * 128, :]), reads=["XGd"], writes=["xg%d" % i])
                pa, pak = pA[i], "pA%d" % i
                for kc in range(8):
                    kb.op("pe", lambda kc=kc, i=i, pa=pa: ncp.transpose(pa[:, kc * 128:(kc + 1) * 128], xg[i][:, kc * 128:(kc + 1) * 128], ident[:]),
                          reads=["xg%d" % i, "c_ident"], writes=[pak])
                kb.op("act", lambda i=i, pa=pa: nca.copy(out=xgT[i][:, 0:4, :], in_=pa[:, 0:512].rearrange("p (k t) -> p k t", k=4)), reads=[pak], writes=["xgT%d" % i])
                kb.op("dve", lambda i=i, pa=pa: ncv.tensor_copy(out=xgT[i][:, 4:8, :], in_=pa[:, 512:1024].rearrange("p (k t) -> p k t", k=4)), reads=[pak], writes=["xgT%d" % i])
                pg, pgk = pB[i], "pB%d" % i
                for kc in range(8):
                    kb.op("pe", lambda kc=kc, i=i, pg=pg: ncp.matmul(pg[:, :], xgT[i][:, kc, :], wgu[i][:, kc, :], start=(kc == 0), stop=(kc == 7)),
                          reads=["xgT%d" % i, "wgu%d" % i], writes=[pgk])
                kb.op("act", lambda i=i, pg=pg: nca.activation(out=sg[i][:], in_=pg[:, 0:256], func=AF.Silu), reads=[pgk], writes=["sgr%d" % i])
                kb.op("dve", lambda i=i, pg=pg: ncv.tensor_tensor(out=act[i][:], in0=sg[i][:], in1=pg[:, 256:512], op=ALU.mult), reads=[pgk, "sgr%d" % i], writes=["actr%d" % i])
                pt, ptk = pB[2 + i], "pB%d" % (2 + i)
                for j in range(2):
                    kb.op("pe", lambda j=j, i=i, pt=pt: ncp.transpose(pt[:, j * 128:(j + 1) * 128], act[i][:, j * 128:(j + 1) * 128], ident[:]),
                          reads=["actr%d" % i, "c_ident"], writes=[ptk])
                kb.op("act", lambda i=i, pt=pt: nca.copy(out=actT[i][:, :, :], in_=pt[:, 0:256].rearrange("p (k t) -> p k t", k=2)), reads=[ptk], writes=["actTr%d" % i])
                for hf in range(2):
                    for j in range(2):
                        kb.op("pe", lambda j=j, hf=hf, i=i, pa=pa: ncp.matmul(pa[:, hf * 512:(hf + 1) * 512], actT[i][:, j, :], wdn[i][:, j, hf * 512:(hf + 1) * 512],
                                                                             start=(j == 0), stop=(j == 1)),
                              reads=["actTr%d" % i, "wdn%d" % i], writes=[pak])
                kb.op("act", lambda i=i, pa=pa: nca.copy(out=yt[i][:, 0:512], in_=pa[:, 0:512]), reads=[pak], writes=["yt%d" % i])
                kb.op("dve", lambda i=i, pa=pa: ncv.tensor_copy(out=yt[i][:, 512:1024], in_=pa[:, 512:1024]), reads=[pak], writes=["yt%d" % i])
                kb.dma("sp", lambda i=i, blk=blk: ncs.dma_start(out=Yd[blk * 128:(blk + 1) * 128, :], in_=yt[i][:]), reads=["yt%d" % i], writes=["Yd"])
            kb.barrier()
        gsem = [kb.stack.enter_context(nc.semaphore("ix_g%d" % k)) for k in range(8)]
        with contextlib.ExitStack() as s6:
            yk = [kb.sb("yk%d" % i, [128, D], stack=s6) for i in range(8)]
            acc = [kb.sb("accr%d" % i, [128, D], stack=s6) for i in range(2)]
            ot = [kb.sb("otr%d" % i, [128, D], stack=s6) for i in range(2)]
            for i in range(NT):
                j = i % 2
                b = i // (S // 128)
                kb.dma("sp", lambda i=i, j=j: ncs.dma_start(out=ot[j][:], in_=out[i * 128:(i + 1) * 128, :]), reads=["out"], writes=["otr%d" % j])
                for k in range(8):
                    kb._need("pool", kb._deps(["Yd", "DESTi"], ["yk%d" % k]))
                    ncg.indirect_dma_start(
                        out=yk[k][:, :], out_offset=None, in_=Yd[:, :],
                        in_offset=bass.IndirectOffsetOnAxis(ap=DESTi[:, i * 8 + k:i * 8 + k + 1], axis=0), bounds_check=R - 1, oob_is_err=False).then_inc(gsem[k], 16)
                    kb._record((gsem[k], "ix_g%d" % k, 16 * (i + 1), "dma"), ["Yd", "DESTi"], ["yk%d" % k])
                    gcol = GK[:, i * 8 + k:i * 8 + k + 1]
                    if k == 0:
                        kb.op("dve", lambda j=j, k=k, gcol=gcol: ncv.tensor_scalar(out=acc[j][:], in0=yk[k][:], scalar1=gcol, scalar2=None, op0=ALU.mult),
                              reads=["yk%d" % k, "GK"], writes=["accr%d" % j])
                    else:
                        kb.op("dve", lambda j=j, k=k, gcol=gcol: ncv.scalar_tensor_tensor(out=acc[j][:], in0=yk[k][:], scalar=gcol, in1=acc[j][:], op0=ALU.mult, op1=ALU.add),
                              reads=["yk%d" % k, "GK", "accr%d" % j], writes=["accr%d" % j])
                kb.op("dve", lambda j=j, b=b: ncv.tensor_tensor(out=acc[j][:], in0=acc[j][:], in1=gate2_b[:, b, :], op=ALU.mult), reads=["accr%d" % j, "gate2_b"], writes=["accr%d" % j])
                kb.op("dve", lambda j=j: ncv.tensor_tensor(out=ot[j][:], in0=ot[j][:], in1=acc[j][:], op=ALU.add), reads=["accr%d" % j, "otr%d" % j], writes=["otr%d" % j])
                kb.dma("sp", lambda i=i, j=j: ncs.dma_start(out=out[i * 128:(i + 1) * 128, :], in_=ot[j][:]), reads=["otr%d" % j], writes=["out"])
            kb.barrier()


def phase_B(nc, kb, b, dram, cs, pv, pA, pB, HTd, PMd, AOd, out, dbg_t, stage):
    ncv, nca, ncp, ncg, ncs = nc.vector, nc.scalar, nc.tensor, nc.gpsimd, nc.sync
    modT, gs1, lsT, gq, gk = pv["modT"], pv["gs1"], pv["lsT"], pv["gq"], pv["gk"]
    ident = cs["ident"]
    w_in = dram["w_in"]
    GROUPS = ((128, 1), (512, 4), (2048, 16))

    with contextlib.ExitStack() as sq:
        qT = kb.sb("qT", [128, 6, S], BF16, stack=sq)
        kT = kb.sb("kT", [128, 6, 2, S], BF16, stack=sq)
        kb.op("pool", lambda: ncg.memset(kT[:], 0.0), writes=["kT"])
        Vg = [kb.sb("Vg%d" % g, [128, 16, 4, 65], BF16, stack=sq) for g in range(3)]
        for g in range(3):
            kb.op("pool", lambda g=g: ncg.memset(Vg[g][:], 1.0), writes=["Vg%d" % g])
        with contextlib.ExitStack() as s1:
            hT = kb.sb("hT", [128, 8, S], F32R, stack=s1)
            s1b = contextlib.ExitStack()
            xt = [kb.sb("xt%d" % i, [128, D], stack=s1b) for i in range(2)]
            xn = [kb.sb("xn%d" % i, [128, D], stack=s1b) for i in range(2)]
            junk = kb.sb("junk", [128, D], stack=s1b)
            ss = kb.sb("ss", [128, 2], stack=s1b)
            rstd = kb.sb("rstd", [128, 2], stack=s1b)
            for tt in range(16):
                i = tt % 2
                r0 = b * S + tt * 128
                kb.dma("sp", lambda i=i, r0=r0: ncs.dma_start(out=xt[i][:], in_=dram["x"][r0:r0 + 128, :]), writes=["xt%d" % i])
                kb.op("act", lambda i=i: nca.activation(out=junk[:], in_=xt[i][:], func=AF.Square, accum_out=ss[:, i:i + 1]),
                      reads=["xt%d" % i], writes=["junk", "ss%d" % i])
                kb.op("act", lambda i=i: nca.activation(out=rstd[:, i:i + 1], in_=ss[:, i:i + 1], func=AF.Sqrt, scale=1.0 / D, bias=cs["epsc"][:, 0:1]),
                      reads=["ss%d" % i, "c_epsc"], writes=["rstd%d" % i])
                kb.op("dve", lambda i=i: ncv.reciprocal(out=rstd[:, i:i + 1], in_=rstd[:, i:i + 1]),
                      reads=["rstd%d" % i], writes=["rstd%d" % i])
                kb.op("act", lambda i=i: nca.activation(out=xn[i][:], in_=xt[i][:], func=AF.Identity, scale=rstd[:, i:i + 1]),
                      reads=["xt%d" % i, "rstd%d" % i], writes=["xn%d" % i])
                pa = pA[i]
                for kc in range(8):
                    kb.op("pe", lambda kc=kc, i=i, pa=pa: ncp.transpose(pa[:, kc * 128:(kc + 1) * 128], xn[i][:, kc * 128:(kc + 1) * 128], ident[:]),
                          reads=["xn%d" % i, "c_ident"], writes=["pA%d" % i])
                for kc in range(8):
                    dst = hT[:, kc, tt * 128:(tt + 1) * 128]
                    if kc % 2 == 0:
                        kb.op("dve", lambda kc=kc, pa=pa, dst=dst: ncv.tensor_scalar(out=dst, in0=pa[:, kc * 128:(kc + 1) * 128], scalar1=gs1[:, kc, b:b + 1],
                                                                                     scalar2=modT[:, kc, b:b + 1], op0=ALU.mult, op1=ALU.add),
                              reads=["pA%d" % i, "gs1", "modT"], writes=["hT"])
                    else:
                        kb.op("act", lambda kc=kc, pa=pa, dst=dst: nca.activation(out=dst, in_=pa[:, kc * 128:(kc + 1) * 128], func=AF.Identity,
                                                                                  scale=gs1[:, kc, b:b + 1], bias=modT[:, kc, b:b + 1]),
                              reads=["pA%d" % i, "gs1", "modT"], writes=["hT"])
            for kc in range(8):
                kb.dma("pool", lambda kc=kc: ncg.dma_start(out=HTd[b, kc * 128:(kc + 1) * 128, :], in_=hT[:, kc, :]),
                       reads=["hT"], writes=["HTd"])
                if dbg_t:
                    kb.dma("pool", lambda kc=kc: ncg.dma_start(out=dbg_t["hT"][b, kc * 128:(kc + 1) * 128, :], in_=hT[:, kc, :]),
                           reads=["hT"], writes=["dbg_hT"])
            kb.barrier()
            s1b.close()
            if stage >= 2:
                phase_B2a(nc, kb, b, dram, cs, pv, pA, pB, hT, qT, kT, Vg, PMd, dbg_t, stage)
            kb.barrier()
        if stage >= 3:
            phase_B2b(nc, kb, b, cs, pA, pB, qT, kT, Vg, AOd, dbg_t)
        kb.barrier()
    if stage >= 4:
        phase_B2c(nc, kb, b, dram, cs, pv, pA, pB, HTd, PMd, AOd, out)
        kb.barrier()


def phase_B2a(nc, kb, b, dram, cs, pv, pA, pB, hT, qT, kT, Vg, PMd, dbg_t, stage):
    ncv, nca, ncp, ncg, ncs = nc.vector, nc.scalar, nc.tensor, nc.gpsimd, nc.sync
    lsT, gq, gk = pv["lsT"], pv["gq"], pv["gk"]
    w_in = dram["w_in"]
    with contextlib.ExitStack() as s2:
        win = [kb.sb("win%d" % i, [128, 8, 128], F32R, stack=s2) for i in range(2)]
        PADW = 16
        sU = contextlib.ExitStack()
        wgrp = kb.sb("wgrp", [128, 128], F32R, stack=sU)
        ub = kb.sb("ub", [128, S + 2 * PADW], stack=sU)
        a1 = kb.sb("a1", [128, S + 2 * PADW], stack=sU)
        a2 = kb.sb("a2", [128, S + 2 * PADW], stack=sU)
        pooled = kb.sb("pooled", [128, S], F32R, stack=sU)
        pmt = [kb.sb("pmt0", [128, 512], stack=sU)] * 2
        kb.op("pool", lambda: ncg.memset(ub[:], 0.0), writes=["ub"])
        kb.op("pool", lambda: ncg.memset(a1[:], 0.0), writes=["a1"])
        kb.op("pool", lambda: ncg.memset(a2[:], 0.0), writes=["a2"])
        sQ = None

        def open_qk():
            sQ_ = contextlib.ExitStack()
            Ct_ = kb.sb("Ct", [128, S], stack=sQ_)
            St_ = kb.sb("St", [128, S], stack=sQ_)
            with contextlib.ExitStack() as sp_:
                posi = kb.sb("posi", [128, S], I32, stack=sp_)
                kb.dma("sp", lambda: ncs.dma_start(out=posi[:], in_=dram["positions"][b:b + 1, :].rearrange("o d -> (o d)").partition_broadcast(128)), writes=["posi"])
                H = S // 8
                posf = kb.sb("posf", [128, S], stack=sp_)
                kf = kb.sb("kf", [128, H], stack=sp_)
                ki = kb.sb("ki", [128, H], I32, stack=sp_)
                kb.op("dve", lambda: ncv.tensor_copy(out=posf[:], in_=posi[:]), reads=["posi"], writes=["posf"])
                C1 = 6.28125
                C2 = TWO_PI - C1
                for tab0, tk_, off in ((St_, "St", 0.0), (Ct_, "Ct", 0.5 * math.pi)):
                    for hh in range(8):
                        tab = tab0[:, hh * H:(hh + 1) * H]
                        pf = posf[:, hh * H:(hh + 1) * H]
                        kb.op("dve", lambda tab=tab, pf=pf, off=off: ncv.tensor_scalar(out=tab, in0=pf, scalar1=cs["invf"][:, 0:1], scalar2=off, op0=ALU.mult, op1=ALU.add),
                              reads=["posf", "c_invf"], writes=[tk_])
                        kb.op("dve", lambda tab=tab: ncv.tensor_scalar(out=kf[:], in0=tab, scalar1=1.0 / TWO_PI, scalar2=None, op0=ALU.mult),
                              reads=[tk_], writes=["kf"])
                        kb.op("dve", lambda: ncv.tensor_copy(out=ki[:], in_=kf[:]), reads=["kf"], writes=["ki"])
                        kb.op("dve", lambda: ncv.tensor_copy(out=kf[:], in_=ki[:]), reads=["ki"], writes=["kf"])
                        kb.op("dve", lambda tab=tab: ncv.scalar_tensor_tensor(out=tab, in0=kf[:], scalar=-C1, in1=tab, op0=ALU.mult, op1=ALU.add),
                              reads=["kf", tk_], writes=[tk_])
                        kb.op("dve", lambda tab=tab: ncv.scalar_tensor_tensor(out=tab, in0=kf[:], scalar=-C2, in1=tab, op0=ALU.mult, op1=ALU.add),
                              reads=["kf", tk_], writes=[tk_])
                        kb.op("dve", lambda tab=tab: ncv.tensor_scalar(out=kf[:], in0=tab, scalar1=math.pi, scalar2=-TWO_PI, op0=ALU.is_gt, op1=ALU.mult),
                              reads=[tk_], writes=["kf"])
                        kb.op("dve", lambda tab=tab: ncv.tensor_tensor(out=tab, in0=tab, in1=kf[:], op=ALU.add), reads=["kf", tk_], writes=[tk_])
                        kb.op("dve", lambda tab=tab: ncv.tensor_scalar(out=kf[:], in0=tab, scalar1=-math.pi, scalar2=TWO_PI, op0=ALU.is_lt, op1=ALU.mult),
                              reads=[tk_], writes=["kf"])
                        kb.op("dve", lambda tab=tab: ncv.tensor_tensor(out=tab, in0=tab, in1=kf[:], op=ALU.add), reads=["kf", tk_], writes=[tk_])
                        kb.op("act", lambda tab=tab: nca.activation(out=tab, in_=tab, func=AF.Sin), reads=[tk_], writes=[tk_])
                kb.barrier()
            sqt_ = [kb.sb("sqt%d" % i, [128, 512], BF16, stack=sQ_) for i in range(2)]
            qg_ = [kb.sb("qg%d" % i, [128, 512], BF16, stack=sQ_) for i in range(2)]
            rs_ = [kb.sb("rs%d" % i, [128, 512], stack=sQ_) for i in range(2)]
            ta_ = [kb.sb("ta%d" % i, [128, 512], stack=sQ_) for i in range(2)]
            tb_ = [kb.sb("tb%d" % i, [128, 512], stack=sQ_) for i in range(2)]
            return sQ_, Ct_, St_, sqt_, qg_, rs_, ta_, tb_

        pcnt = [0]

        def next_pb():
            p = pcnt[0] % 4
            pcnt[0] += 1
            return pB[p], "pB%d" % p

        def proj_tile(f, wi, n):
            pb, pk = next_pb()
            for kc in range(8):
                kb.op("pe", lambda kc=kc: ncp.matmul(pb[:, :], win[wi][:, kc, :], hT[:, kc, n * 512:(n + 1) * 512], start=(kc == 0), stop=(kc == 7)),
                      reads=["win%d" % wi, "hT"], writes=[pk])
            return pb, pk

        for f in range(16):
            wi = f % 2
            if f == 4 and stage < 2.2:
                break
            if f == 4:
                kb.barrier()
                sU.close()
                sQ, Ct, St, sqt, qg, rs, ta, tb = open_qk()
            kb.dma("pool", lambda f=f, wi=wi: ncg.dma_start(out=win[wi][:], in_=w_in[:, f * 128:(f + 1) * 128].rearrange("(k p) n -> p k n", p=128)),
                   writes=["win%d" % wi])
            if f < 4:
                g = f
                R = (1, 2, 4, 8)[g]
                kb.dma("pool", lambda g=g: ncg.dma_start(out=wgrp[:], in_=dram["pool_w_grp"][g * 128:(g + 1) * 128, :]), writes=["wgrp"])
                for n in range(4):
                    pb, pk = proj_tile(f, wi, n)
                    kb.op("act", lambda n=n, pb=pb: nca.copy(out=ub[:, PADW + n * 512:PADW + (n + 1) * 512], in_=pb[:, :]), reads=[pk], writes=["ub"])
                lo, hi = 0, S + 2 * PADW
                kb.op("dve", lambda: ncv.tensor_tensor(out=a1[:, 0:hi - 1], in0=ub[:, 0:hi - 1], in1=ub[:, 1:hi], op=ALU.add), reads=["ub"], writes=["a1"])
                cur, curk, width = a1, "a1", 2
                other, otherk = a2, "a2"
                while width < R * 2:
                    kb.op("dve", lambda cur=cur, other=other, width=width: ncv.tensor_tensor(
                        out=other[:, 0:hi - 2 * width + 1], in0=cur[:, 0:hi - 2 * width + 1], in1=cur[:, width:hi - width + 1], op=ALU.add),
                        reads=[curk], writes=[otherk])
                    cur, other = other, cur
                    curk, otherk = otherk, curk
                    width *= 2
                kb.op("dve", lambda cur=cur, other=other, R=R: ncv.tensor_tensor(
                    out=other[:, PADW:PADW + S], in0=cur[:, PADW - R:PADW - R + S], in1=ub[:, PADW + R:PADW + R + S], op=ALU.add),
                    reads=[curk, "ub"], writes=[otherk])
                kb.op("dve", lambda other=other, R=R: ncv.scalar_tensor_tensor(
                    out=pooled[:, :], in0=other[:, PADW:PADW + S], scalar=1.0 / (2 * R + 1), in1=ub[:, PADW:PADW + S], op0=ALU.mult, op1=ALU.subtract),
                    reads=[otherk, "ub"], writes=["pooled"])
                kb.op("dve", lambda other=other, R=R, g=g: ncv.tensor_tensor(
                    out=cur[:, PADW:PADW + R], in0=other[:, PADW:PADW + R], in1=cs["pooledge"][:, g, 0:R], op=ALU.mult),
                    reads=[otherk, "c_pooledge"], writes=[curk])
                kb.op("dve", lambda cur=cur, R=R: ncv.tensor_tensor(
                    out=pooled[:, 0:R], in0=cur[:, PADW:PADW + R], in1=ub[:, PADW:PADW + R], op=ALU.subtract),
                    reads=[curk, "ub"], writes=["pooled"])
                for t in range(R):
                    pos = S - 1 - t
                    kb.op("dve", lambda other=other, g=g, t=t, pos=pos: ncv.scalar_tensor_tensor(
                        out=pooled[:, pos:pos + 1], in0=other[:, PADW + pos:PADW + pos + 1], scalar=cs["pooledge"][:, g, 8 + t:9 + t],
                        in1=ub[:, PADW + pos:PADW + pos + 1], op0=ALU.mult, op1=ALU.subtract),
                        reads=[otherk, "ub", "c_pooledge"], writes=["pooled"])
                for n in range(4):
                    pb, pk = next_pb()
                    kb.op("pe", lambda g=g, n=n, pb=pb: ncp.matmul(pb[:, :], wgrp[:, :], pooled[:, n * 512:(n + 1) * 512], start=True, stop=True),
                          reads=["wgrp", "pooled"], writes=[pk])
                    j = 0
                    kb.op("act", lambda g=g, pb=pb, j=j: nca.activation(out=pmt[j][:], in_=pb[:, :], func=AF.Identity, scale=lsT[:, g:g + 1]),
                          reads=[pk, "lsT"], writes=["pmt%d" % j])
                    kb.dma("sp", lambda g=g, n=n, j=j: ncs.dma_start(out=PMd[b, g * 128:(g + 1) * 128, n * 512:(n + 1) * 512], in_=pmt[j][:]),
                           reads=["pmt%d" % j], writes=["PMd"])
                    if dbg_t:
                        kb.dma("sp", lambda g=g, n=n, j=j: ncs.dma_start(out=dbg_t["pm"][b, g * 128:(g + 1) * 128, n * 512:(n + 1) * 512], in_=pmt[j][:]),
                               reads=["pmt%d" % j], writes=["dbg_pm"])
            else:
                isq = f < 10
                tile = (f - 4) if isq else (f - 10)
                g = tile // 2
                r = (1, 4, 16)[g]
                L = S // r
                dstT = qT if isq else kT
                dk = "qT" if isq else "kT"
                gain = gq if isq else gk
                gaink = "gq" if isq else "gk"
                for n in range(4):
                    j = n % 2
                    pb, pk = proj_tile(f, wi, n)
                    kb.op("act", lambda pb=pb, j=j: nca.activation(out=sqt[j][:], in_=pb[:, :], func=AF.Square), reads=[pk], writes=["sqt%d" % j])
                    kb.op("act", lambda pb=pb, j=j, gain=gain: nca.activation(out=qg[j][:], in_=pb[:, :], func=AF.Identity, scale=gain[:, 0:1]),
                          reads=[pk, gaink], writes=["qg%d" % j])
                    ps_, psk = next_pb()
                    kb.op("pe", lambda ps_=ps_, j=j: ncp.matmul(ps_[:, :], cs["blockones"][:], sqt[j][:], start=True, stop=True),
                          reads=["sqt%d" % j, "c_blockones"], writes=[psk])
                    pr_, prk = next_pb()
                    kb.op("pe", lambda pr_=pr_, j=j: ncp.matmul(pr_[:, :], cs["ropeR"][:], qg[j][:], start=True, stop=True),
                          reads=["qg%d" % j, "c_ropeR"], writes=[prk])
                    kb.op("act", lambda ps_=ps_, j=j: nca.activation(out=rs[j][:], in_=ps_[:, :], func=AF.Sqrt, bias=cs["epsc"][:, 1:2]),
                          reads=[psk, "c_epsc"], writes=["rs%d" % j])
                    kb.op("dve", lambda j=j: ncv.reciprocal(out=rs[j][:], in_=rs[j][:]), reads=["rs%d" % j], writes=["rs%d" % j])
                    kb.op("pool", lambda j=j, n=n: ncg.tensor_tensor(out=ta[j][:], in0=qg[j][:], in1=Ct[:, n * 512:(n + 1) * 512], op=ALU.mult),
                          reads=["qg%d" % j, "Ct"], writes=["ta%d" % j])
                    kb.op("dve", lambda pr_=pr_, j=j, n=n: ncv.tensor_tensor(out=tb[j][:], in0=pr_[:, :], in1=St[:, n * 512:(n + 1) * 512], op=ALU.mult),
                          reads=[prk, "St"], writes=["tb%d" % j])
                    kb.op("pool", lambda j=j: ncg.tensor_tensor(out=ta[j][:], in0=ta[j][:], in1=tb[j][:], op=ALU.add),
                          reads=["ta%d" % j, "tb%d" % j], writes=["ta%d" % j])
                    m0 = n * 512 // r
                    mn = 512 // r
                    if not isq:
                        dst = src0 = src1 = None
                    elif r == 1:
                        dst = dstT[:, tile, n * 512:(n + 1) * 512]
                        src0 = ta[j][:, :]
                        src1 = rs[j][:, :]
                    else:
                        dst = dstT[:, tile, :].rearrange("p (rr m) -> p m rr", rr=r)[:, m0:m0 + mn, :]
                        src0 = ta[j][:, :].rearrange("p (m rr) -> p m rr", rr=r)
                        src1 = rs[j][:, :].rearrange("p (m rr) -> p m rr", rr=r)
                    if isq:
                        kb.op("dve", lambda dst=dst, src0=src0, src1=src1: ncv.tensor_tensor(out=dst, in0=src0, in1=src1, op=ALU.mult),
                              reads=["ta%d" % j, "rs%d" % j], writes=[dk])
                    else:
                        for hl in range(2):
                            o = 64 * hl
                            if r == 1:
                                dsth = kT[o:o + 64, tile, hl, n * 512:(n + 1) * 512]
                                s0h, s1h = ta[j][o:o + 64, :], rs[j][o:o + 64, :]
                            else:
                                dsth = kT[o:o + 64, tile, hl, :].rearrange("p (rr m) -> p m rr", rr=r)[:, m0:m0 + mn, :]
                                s0h = ta[j][o:o + 64, :].rearrange("p (m rr) -> p m rr", rr=r)
                                s1h = rs[j][o:o + 64, :].rearrange("p (m rr) -> p m rr", rr=r)
                            kb.op("dve", lambda dsth=dsth, s0h=s0h, s1h=s1h: ncv.tensor_tensor(out=dsth, in0=s0h, in1=s1h, op=ALU.mult),
                                  reads=["ta%d" % j, "rs%d" % j], writes=[dk])
        kb.barrier()
        if sQ is None:
            sU.close()
            return
        sQ.close()
        if stage < 2.3:
            return
        vT = kb.sb("vT", [128, 6, S], BF16, stack=s2)
        for f in range(16, 22):
            wi = f % 2
            tile = f - 16
            g = tile // 2
            r = (1, 4, 16)[g]
            kb.dma("pool", lambda f=f, wi=wi: ncg.dma_start(out=win[wi][:], in_=w_in[:, f * 128:(f + 1) * 128].rearrange("(k p) n -> p k n", p=128)),
                   writes=["win%d" % wi])
            for n in range(4):
                pb, pk = proj_tile(f, wi, n)
                m0 = n * 512 // r
                mn = 512 // r
                if r == 1:
                    dst = vT[:, tile, n * 512:(n + 1) * 512]
                    src = pb[:, :]
                else:
                    dst = vT[:, tile, :].rearrange("p (rr m) -> p m rr", rr=r)[:, m0:m0 + mn, :]
                    src = pb[:, :].rearrange("p (m rr) -> p m rr", rr=r)
                if n % 2 == 0:
                    kb.op("act", lambda dst=dst, src=src: nca.copy(out=dst, in_=src), reads=[pk], writes=["vT"])
                else:
                    kb.op("dve", lambda dst=dst, src=src: ncv.tensor_copy(out=dst, in_=src), reads=[pk], writes=["vT"])
        identb = cs["ident_bf"]
        for g in range(3):
            for ci in range(16):
                pb, pk = next_pb()
                pbb = pb[:, 0:128].bitcast(BF16)
                for hp in range(2):
                    kb.op("pe", lambda g=g, ci=ci, hp=hp, pbb=pbb: ncp.transpose(pbb[:, hp * 128:(hp + 1) * 128], vT[:, 2 * g + hp, ci * 128:(ci + 1) * 128], identb[:]),
                          reads=["vT", "c_ident_bf"], writes=[pk])
                src = pbb[:, :].rearrange("p (h d) -> p h d", d=64)
                if ci % 2 == 0:
                    kb.op("act", lambda g=g, ci=ci, src=src: nca.copy(out=Vg[g][:, ci, :, 0:64], in_=src), reads=[pk], writes=["Vg%d" % g])
                else:
                    kb.op("dve", lambda g=g, ci=ci, src=src: ncv.tensor_copy(out=Vg[g][:, ci, :, 0:64], in_=src), reads=[pk], writes=["Vg%d" % g])


def phase_B2b(nc, kb, b, cs, pA, pB, qT, kT, Vg, AOd, dbg_t):
    ncv, nca, ncp, ncg, ncs = nc.vector, nc.scalar, nc.tensor, nc.gpsimd, nc.sync
    with contextlib.ExitStack() as s3:
        acc = kb.sb("acc", [64, 4, S], stack=s3)
        accd = kb.sb("accd", [64, 4, S], stack=s3)
        kb.op("pool", lambda: ncg.memset(accd[:], 0.0), writes=["accd"])
        PT = [kb.sb("PT%d" % i, [128, 2, 256], BF16, stack=s3) for i in range(3)]
        aot = [kb.sb("aot%d" % i, [64, 512], stack=s3) for i in range(2)]
        kb.op("pool", lambda: ncg.memset(acc[:], 0.0), writes=["acc"])
        it = 0
        for g in range(3):
            r = (1, 4, 16)[g]
            L = S // r
            nch = L // 128
            for rr in range(r):
                for c in range(nch):
                    ci = rr * nch + c
                    j0 = max(0, 128 * c - 64)
                    j1 = min(L, 128 * c + 192)
                    nq = j1 - j0
                    mo = j0 - (128 * c - 64)
                    for hp in range(2):
                        tile = 2 * g + hp
                        pi = it % 4
                        ps_, psk = pB[pi], "pB%d" % pi
                        pt, ptk = PT[it % 3], "PT%d" % (it % 3)
                        po, pok = pA[it % 2], "pA%d" % (it % 2)
                        it += 1
                        for hl in range(2):
                            o = 64 * hl
                            kb.op("pe", lambda o=o, hl=hl, tile=tile, rr=rr, c=c, j0=j0, j1=j1, nq=nq, L=L, ps_=ps_: ncp.matmul(
                                ps_[:, hl * 256:hl * 256 + nq], kT[:, tile, hl, rr * L + 128 * c:rr * L + 128 * c + 128],
                                qT[:, tile, rr * L + j0:rr * L + j1], start=True, stop=True),
                                reads=["qT", "kT"], writes=[psk])
                        kb.op("act", lambda ps_=ps_, pt=pt, nq=nq: nca.activation(
                            out=pt[:, :, 0:nq], in_=ps_[:, :].rearrange("p (h q) -> p h q", h=2)[:, :, 0:nq], func=AF.Exp, scale=0.125),
                            reads=[psk], writes=[ptk])
                        for hl in range(2):
                            kb.op("pool" if hl == 0 else "dve",
                                  lambda hl=hl, pt=pt, nq=nq, mo=mo: (ncg if hl == 0 else ncv).tensor_tensor(
                                      out=pt[:, hl, 0:nq], in0=pt[:, hl, 0:nq], in1=cs["band"][:, mo:mo + nq], op=ALU.mult),
                                  reads=[ptk, "c_band"], writes=[ptk])
                        for hl in range(2):
                            h = 2 * hp + hl
                            kb.op("pe", lambda hl=hl, h=h, g=g, ci=ci, pt=pt, po=po, nq=nq: ncp.matmul(
                                po[0:64, hl * 256:hl * 256 + nq], Vg[g][:, ci, h, 0:64], pt[:, hl, 0:nq], start=True, stop=True),
                                reads=["Vg%d" % g, ptk], writes=[pok])
                            kb.op("pe", lambda hl=hl, pt=pt, po=po, nq=nq: ncp.matmul(
                                po[0:64, 512 + hl * 256:512 + hl * 256 + nq], cs["ones_bf"][:, 0:64], pt[:, hl, 0:nq], start=True, stop=True),
                                reads=["c_ones_bf", ptk], writes=[pok])
                        for hl in range(2):
                            h = 2 * hp + hl
                            ts0 = j0 * r + rr
                            dst = acc[0:64, h, ts0:ts0 + (nq - 1) * r + 1:r]
                            dstd = accd[0:64, h, ts0:ts0 + (nq - 1) * r + 1:r]
                            kb.op("dve", lambda dst=dst, po=po, hl=hl, nq=nq: ncv.tensor_tensor(
                                out=dst, in0=dst, in1=po[0:64, hl * 256:hl * 256 + nq], op=ALU.add),
                                reads=[pok, "acc"], writes=["acc"])
                            kb.op("dve", lambda dstd=dstd, po=po, hl=hl, nq=nq: ncv.tensor_tensor(
                                out=dstd, in0=dstd, in1=po[0:64, 512 + hl * 256:512 + hl * 256 + nq], op=ALU.add),
                                reads=[pok, "accd"], writes=["accd"])
        for h in range(4):
            for n in range(4):
                j = (h * 4 + n) % 2
                kb.op("dve", lambda n=n, h=h, j=j: ncv.reciprocal(out=aot[j][:], in_=accd[0:64, h, n * 512:(n + 1) * 512]), reads=["accd"], writes=["aot%d" % j])
                kb.op("dve", lambda n=n, h=h, j=j: ncv.tensor_tensor(out=aot[j][:], in0=aot[j][:], in1=acc[0:64, h, n * 512:(n + 1) * 512], op=ALU.mult),
                      reads=["aot%d" % j, "acc"], writes=["aot%d" % j])
                kb.dma("sp", lambda h=h, n=n, j=j: ncs.dma_start(out=AOd[b, h * 64:(h + 1) * 64, n * 512:(n + 1) * 512], in_=aot[j][:]),
                       reads=["aot%d" % j], writes=["AOd"])
                if dbg_t:
                    kb.dma("sp", lambda h=h, n=n, j=j: ncs.dma_start(out=dbg_t["ao"][b, h * 64:(h + 1) * 64, n * 512:(n + 1) * 512], in_=aot[j][:]),
                           reads=["aot%d" % j], writes=["dbg_ao"])


def phase_B2c(nc, kb, b, dram, cs, pv, pA, pB, HTd, PMd, AOd, out):
    ncv, nca, ncp, ncg, ncs = nc.vector, nc.scalar, nc.tensor, nc.gpsimd, nc.sync
    w_in = dram["w_in"]
    with contextlib.ExitStack() as s4:
        gate1_b = kb.sb("gate1_b", [128, NB, D], stack=s4)
        kb.dma("sp", lambda: ncs.dma_start(out=gate1_b[:].rearrange("p b d -> p (b d)"), in_=pv["BCd"][3]), reads=["BCd"], writes=["gate1_b"])
        wout = kb.sb("wout", [128, 8, D], F32R, stack=s4)
        wpu = kb.sb("wpu", [128, 4, D], F32R, stack=s4)
        wau = kb.sb("wau", [128, 2, D], F32R, stack=s4)
        hTt = kb.sb("hTt", [128, 8, 512], F32R, stack=s4)
        pmt = kb.sb("pmt_c", [128, 4, 512], F32R, stack=s4)
        aot = kb.sb("aot_c", [128, 2, 512], F32R, stack=s4)
        wgp = [kb.sb("wgp%d" % i, [128, 8, 128], F32R, stack=s4) for i in range(2)]
        wga = [kb.sb("wga%d" % i, [128, 8, 128], F32R, stack=s4) for i in range(2)]
        merged = kb.sb("merged", [128, 8, 512], F32R, stack=s4)
        sgp = kb.sb("sgp", [128, 512], stack=s4)
        sga = kb.sb("sga", [128, 512], stack=s4)
        m1 = kb.sb("m1", [128, 512], stack=s4)
        xt = [kb.sb("xtc%d" % i, [128, D], stack=s4) for i in range(2)]
        x1 = [kb.sb("x1c%d" % i, [128, D], stack=s4) for i in range(2)]
        kb.dma("pool", lambda: ncg.dma_start(out=wout[:], in_=dram["w_out"].rearrange("(k p) n -> p k n", p=128)), writes=["wout"])
        kb.dma("pool", lambda: ncg.dma_start(out=wpu[:], in_=dram["w_pool_up"].rearrange("(k p) n -> p k n", p=128)), writes=["wpu"])
        kb.dma("pool", lambda: ncg.dma_start(out=wau[:], in_=dram["w_attn_up"].rearrange("(k p) n -> p k n", p=128)), writes=["wau"])
        for n in range(4):
            kb.dma("pool", lambda n=n: ncg.dma_start(out=hTt[:], in_=HTd[b, :, n * 512:(n + 1) * 512].rearrange("(k p) t -> p k t", p=128)),
                   reads=["HTd"], writes=["hTt"])
            kb.dma("pool", lambda n=n: ncg.dma_start(out=pmt[:], in_=PMd[b, :, n * 512:(n + 1) * 512].rearrange("(k p) t -> p k t", p=128)),
                   reads=["PMd"], writes=["pmt_c"])
            kb.dma("pool", lambda n=n: ncg.dma_start(out=aot[:], in_=AOd[b, :, n * 512:(n + 1) * 512].rearrange("(k p) t -> p k t", p=128)),
                   reads=["AOd"], writes=["aot_c"])
            for j in range(8):
                wi = j % 2
                kb.dma("pool", lambda j=j, wi=wi: ncg.dma_start(out=wgp[wi][:], in_=w_in[:, 2816 + j * 128:2816 + (j + 1) * 128].rearrange("(k p) n -> p k n", p=128)),
                       writes=["wgp%d" % wi])
                kb.dma("pool", lambda j=j, wi=wi: ncg.dma_start(out=wga[wi][:], in_=w_in[:, 3840 + j * 128:3840 + (j + 1) * 128].rearrange("(k p) n -> p k n", p=128)),
                       writes=["wga%d" % wi])
                for kc in range(8):
                    kb.op("pe", lambda kc=kc, wi=wi: ncp.matmul(pB[0][:, :], wgp[wi][:, kc, :], hTt[:, kc, :], start=(kc == 0), stop=(kc == 7)),
                          reads=["wgp%d" % wi, "hTt"], writes=["pB0"])
                for kc in range(8):
                    kb.op("pe", lambda kc=kc, wi=wi: ncp.matmul(pB[1][:, :], wga[wi][:, kc, :], hTt[:, kc, :], start=(kc == 0), stop=(kc == 7)),
                          reads=["wga%d" % wi, "hTt"], writes=["pB1"])
                for g in range(4):
                    kb.op("pe", lambda g=g, j=j: ncp.matmul(pB[2][:, :], wpu[:, g, j * 128:(j + 1) * 128], pmt[:, g, :], start=(g == 0), stop=(g == 3)),
                          reads=["wpu", "pmt_c"], writes=["pB2"])
                for g in range(2):
                    kb.op("pe", lambda g=g, j=j: ncp.matmul(pB[3][:, :], wau[:, g, j * 128:(j + 1) * 128], aot[:, g, :], start=(g == 0), stop=(g == 1)),
                          reads=["wau", "aot_c"], writes=["pB3"])
                kb.op("act", lambda: nca.activation(out=sgp[:], in_=pB[0][:, :], func=AF.Sigmoid), reads=["pB0"], writes=["sgp"])
                kb.op("act", lambda: nca.activation(out=sga[:], in_=pB[1][:, :], func=AF.Sigmoid), reads=["pB1"], writes=["sga"])
                kb.op("dve", lambda: ncv.tensor_tensor(out=m1[:], in0=sgp[:], in1=pB[2][:, :], op=ALU.mult), reads=["sgp", "pB2"], writes=["m1"])
                kb.op("dve", lambda: ncv.tensor_tensor(out=sga[:], in0=sga[:], in1=pB[3][:, :], op=ALU.mult), reads=["sga", "pB3"], writes=["sga"])
                kb.op("pool", lambda j=j: ncg.tensor_tensor(out=merged[:, j, :], in0=m1[:], in1=sga[:], op=ALU.add), reads=["m1", "sga"], writes=["merged"])
            for s in range(4):
                i = s % 2
                r0 = b * S + n * 512 + s * 128
                kb.dma("sp", lambda i=i, r0=r0: ncs.dma_start(out=xt[i][:], in_=dram["x"][r0:r0 + 128, :]), writes=["xtc%d" % i])
                pa, pak = pA[i], "pA%d" % i
                for hf in range(2):
                    for j in range(8):
                        kb.op("pe", lambda j=j, hf=hf, s=s, pa=pa: ncp.matmul(pa[:, hf * 512:(hf + 1) * 512], merged[:, j, s * 128:(s + 1) * 128],
                                                                             wout[:, j, hf * 512:(hf + 1) * 512], start=(j == 0), stop=(j == 7)),
                              reads=["merged", "wout"], writes=[pak])
                kb.op("dve", lambda i=i, pa=pa: ncv.tensor_tensor(out=x1[i][:], in0=pa[:, :], in1=gate1_b[:, b, :], op=ALU.mult),
                      reads=[pak, "gate1_b"], writes=["x1c%d" % i])
                kb.op("pool", lambda i=i: ncg.tensor_tensor(out=x1[i][:], in0=x1[i][:], in1=xt[i][:], op=ALU.add),
                      reads=["x1c%d" % i, "xtc%d" % i], writes=["x1c%d" % i])
                kb.dma("sp", lambda i=i, r0=r0: ncs.dma_start(out=out[r0:r0 + 128, :], in_=x1[i][:]), reads=["x1c%d" % i], writes=["out"])


_NC_CACHE = {}


def _get_nc(stage=99, dbg=False):
    key = (stage, dbg)
    if key not in _NC_CACHE:
        _NC_CACHE[key] = build_nc(stage, dbg)
    return _NC_CACHE[key]


def _in_maps(inputs, cores, stage=99):
    consts = _consts()
    maps = []
    w = {}
    for name, shape in W_SPECS:
        if stage < 6 and name.startswith("w_exp"):
            continue
        w[name] = np.ascontiguousarray(np.asarray(inputs[name], dtype=np.float32).reshape(shape))
    x = np.asarray(inputs["x"], dtype=np.float32)
    c = np.asarray(inputs["c"], dtype=np.float32)
    pos = np.asarray(inputs["positions"], dtype=np.int32)
    for i in cores:
        m = {"x": np.ascontiguousarray(x[NB * i:NB * (i + 1)].reshape(T, D)),
             "c": np.ascontiguousarray(c[NB * i:NB * (i + 1)]),
             "positions": np.ascontiguousarray(pos[NB * i:NB * (i + 1)])}
        m.update(w)
        for k, v in consts.items():
            m["k_" + k] = v
        maps.append(m)
    return maps


def kernel(**inputs):
    nc = _get_nc(stage=6)
    maps = _in_maps(inputs, list(range(NCORES)), stage=6)
    res = run_bass_kernel_spmd(nc, maps, core_ids=list(range(NCORES)))
    outs = [np.asarray(r["out"]).reshape(NB, S, D) for r in res.results]
    return np.concatenate(outs, axis=0).astype(np.float32)
```

```python
import contextlib
import math
import numpy as np
import ml_dtypes
import concourse.bass as bass
import concourse.mybir as mybir
from concourse.bass_utils import run_bass_kernel_spmd

F32 = mybir.dt.float32
F32R = mybir.dt.float32r
BF16 = mybir.dt.bfloat16
I32 = mybir.dt.int32
AF = mybir.ActivationFunctionType
ALU = mybir.AluOpType
AX = mybir.AxisListType

D = 1024
S = 2048
NB = 2
T = NB * S
NCORES = 8
EPS = 1e-6
IN_WIDTH = 4864
NE = 256
NBLK = T * 8 // 128 + NE
TWO_PI = 2.0 * math.pi
DENSE_TB = 1024


class KB:
    N_DMA_SEMS = 48

    def __init__(self, nc, stack):
        self.nc = nc
        self.stack = stack
        self.eng = dict(pe=nc.tensor, act=nc.scalar, dve=nc.vector, pool=nc.gpsimd, sp=nc.sync)
        self.csem = {}
        self.ccnt = {}
        for e in ("pe", "act", "dve", "pool"):
            self.csem[e] = stack.enter_context(nc.semaphore("c_" + e))
            self.ccnt[e] = 0
        self.dsem = [stack.enter_context(nc.semaphore("d_%d" % i)) for i in range(self.N_DMA_SEMS)]
        self.dcnt = [0] * self.N_DMA_SEMS
        self.drr = 0
        self.waited = {e: {} for e in self.eng}
        self.res = {}
        self.n_inst = 0

    def sb(self, name, shape, dt=F32, stack=None):
        self.n_inst += 1
        return (stack or self.stack).enter_context(self.nc.sbuf_tensor("%s_%d" % (name, self.n_inst), list(shape), dt))

    def ps(self, name, shape, dt=F32, stack=None):
        return (stack or self.stack).enter_context(self.nc.psum_tensor(name, list(shape), dt))

    def _st(self, key):
        s = self.res.get(key)
        if s is None:
            s = {"w": None, "r": []}
            self.res[key] = s
        return s

    def _need(self, engine, deps):
        e = self.eng[engine]
        for (sem, name, val, src) in deps:
            if src == engine and engine == "pe":
                continue
            if engine == "pool" and name.startswith("ix_"):
                continue
            if self.waited[engine].get(name, 0) >= val:
                continue
            e.wait_ge(sem, val)
            self.waited[engine][name] = val

    def _deps(self, reads, writes):
        deps = []
        for k in reads:
            s = self._st(k)
            if s["w"] is not None:
                deps.append(s["w"])
        for k in writes:
            s = self._st(k)
            if s["w"] is not None:
                deps.append(s["w"])
            deps.extend(s["r"])
        return deps

    def _record(self, tok, reads, writes):
        for k in reads:
            s = self._st(k)
            s["r"] = [r for r in s["r"] if r[1] != tok[1]] + [tok]
        for k in writes:
            s = self._st(k)
            s["w"] = tok
            s["r"] = []

    def op(self, engine, fn, reads=(), writes=(), inc=True):
        self._need(engine, self._deps(reads, writes))
        inst = fn()
        if inc:
            self.ccnt[engine] += 1
            inst.then_inc(self.csem[engine], 1)
            tok = (self.csem[engine], "c_" + engine, self.ccnt[engine], engine)
        else:
            tok = (self.csem[engine], "c_" + engine, self.ccnt[engine] + 1, engine)
        self._record(tok, reads, writes)
        self.n_inst += 1
        return inst

    def dma(self, queue, fn, reads=(), writes=()):
        i = self.drr
        self.drr = (self.drr + 1) % self.N_DMA_SEMS
        deps = self._deps(reads, writes)
        if self.dcnt[i] > 0:
            deps.append((self.dsem[i], "d_%d" % i, self.dcnt[i], "dma"))
        self._need(queue, deps)
        inst = fn()
        self.dcnt[i] += 16
        inst.then_inc(self.dsem[i], 16)
        tok = (self.dsem[i], "d_%d" % i, self.dcnt[i], "dma")
        self._record(tok, reads, writes)
        self.n_inst += 1
        return inst

    def _all(self):
        deps = []
        for i in range(self.N_DMA_SEMS):
            if self.dcnt[i] > 0:
                deps.append((self.dsem[i], "d_%d" % i, self.dcnt[i], "dma"))
        for e in ("pe", "act", "dve", "pool"):
            if self.ccnt[e] > 0:
                deps.append((self.csem[e], "c_" + e, self.ccnt[e], "x"))
        return deps

    def drain(self, engine="sp"):
        self._need(engine, self._all())

    def barrier(self):
        deps = self._all()
        for e in ("sp", "act", "dve", "pool", "pe"):
            self._need(e, [d for d in deps])
        self.res = {}


def _consts():
    c = {}
    c["ident"] = np.eye(128, dtype=np.float32)
    c["ident_bf"] = np.eye(128, dtype=np.float32).astype(ml_dtypes.bfloat16)
    bo = np.zeros((128, 128), np.float32)
    bo[:64, :64] = 1.0
    bo[64:, 64:] = 1.0
    c["blockones"] = bo.astype(ml_dtypes.bfloat16)
    rr = np.zeros((128, 128), np.float32)
    for o in (0, 64):
        for i in range(8):
            rr[o + i + 8, o + i] = -1.0
            rr[o + i, o + i + 8] = 1.0
    c["ropeR"] = rr.astype(ml_dtypes.bfloat16)
    invf = np.zeros((128, 1), np.float32)
    half = 8
    inv_freq = (500000.0 ** (-np.arange(half, dtype=np.float32) / half)).astype(np.float32)
    for p in range(128):
        d = p % 64
        if d < 16:
            invf[p, 0] = inv_freq[d % 8]
    c["invf"] = invf
    kk = np.arange(128)[:, None]
    jj = np.arange(256)[None, :]
    c["band"] = ((jj >= kk) & (jj <= kk + 128)).astype(np.float32).astype(ml_dtypes.bfloat16)
    pe = np.ones((128, 4, 16), np.float32)
    for g, R in enumerate((1, 2, 4, 8)):
        for t in range(R):
            pe[:, g, t] = 1.0 / (t + R + 1)
            pe[:, g, 8 + t] = 1.0 / (R + 1 + t)
    c["pooledge"] = pe
    tri = (np.arange(128)[:, None] < np.arange(128)[None, :]).astype(np.float32)
    c["tri"] = tri.astype(ml_dtypes.bfloat16)
    c["ones_bf"] = np.ones((128, 128), ml_dtypes.bfloat16)
    c["ones_f"] = np.ones((128, 128), np.float32)
    ec = np.zeros((128, 2), np.float32)
    ec[:, 0] = EPS
    ec[:, 1] = 64.0 * EPS
    c["epsc"] = ec
    thr = np.zeros((128, 4), np.float32)
    for j in range(4):
        thr[:, j] = 128.0 * (128 * j + np.arange(128))
    c["thr4"] = thr
    return c


CONST_SPECS = [("ident", [128, 128], F32), ("ident_bf", [128, 128], BF16), ("blockones", [128, 128], BF16), ("ropeR", [128, 128], BF16),
               ("invf", [128, 1], F32), ("band", [128, 256], BF16), ("pooledge", [128, 4, 16], F32),
               ("tri", [128, 128], BF16), ("ones_bf", [128, 128], BF16), ("ones_f", [128, 128], F32), ("epsc", [128, 2], F32), ("thr4", [128, 4], F32)]

W_SPECS = [("w_ada", [D, 6 * D]), ("b_ada", [1, 6 * D]), ("norm1_g", [1, D]), ("w_in", [D, IN_WIDTH]),
           ("pool_w_grp", [512, 128]), ("pool_scale", [1, 512]), ("q_norm_g", [1, 64]), ("k_norm_g", [1, 64]),
           ("w_pool_up", [512, D]), ("w_attn_up", [256, D]), ("w_out", [D, D]), ("norm2_g", [1, D]),
           ("w_router", [D, NE]), ("router_bias", [1, NE]), ("w_shared_gate", [D, 256]), ("w_shared_up", [D, 256]),
           ("w_shared_down", [256, D]), ("w_exp_gate", [NE, D, 256]), ("w_exp_up", [NE, D, 256]),
           ("w_exp_down", [NE, 256, D])]


def build_nc(stage=99, dbg=False):
    nc = bass.Bass("TRN2", target_bir_lowering=False)
    dram = {}
    dram["x"] = nc.dram_tensor("x", [T, D], F32, kind="ExternalInput").ap()
    dram["c"] = nc.dram_tensor("c", [NB, D], F32, kind="ExternalInput").ap()
    dram["positions"] = nc.dram_tensor("positions", [NB, S], I32, kind="ExternalInput").ap()
    for name, shape in W_SPECS:
        if stage < 6 and name.startswith("w_exp"):
            continue
        dram[name] = nc.dram_tensor(name, shape, F32, kind="ExternalInput").ap()
    for name, shape, dt in CONST_SPECS:
        dram[name] = nc.dram_tensor("k_" + name, shape, dt, kind="ExternalInput").ap()
    out = nc.dram_tensor("out", [T, D], F32, kind="ExternalOutput").ap()
    HTd = nc.dram_tensor("HTd", [NB, D, S], F32, kind="Internal").ap()
    PMd = nc.dram_tensor("PMd", [NB, 512, S], F32, kind="Internal").ap()
    AOd = nc.dram_tensor("AOd", [NB, 256, S], F32, kind="Internal").ap()
    BCd = nc.dram_tensor("BCd", [4, 128, NB * D], F32, kind="Internal").ap()
    H2d = nc.dram_tensor("H2d", [T, D], F32, kind="Internal").ap()
    Gd = nc.dram_tensor("Gd", [128, T // 128, NE], F32, kind="Internal").ap()
    XGd = Yd = BEXd = None
    if stage >= 7:
        XGd = nc.dram_tensor("XGd", [NBLK * 128, D], F32, kind="Internal").ap()
        Yd = nc.dram_tensor("Yd", [NBLK * 128, D], F32, kind="Internal").ap()
        BEXd = nc.dram_tensor("BEXd", [NBLK], I32, kind="Internal").ap()
    dbg_t = {}
    if dbg:
        dbg_t["hT"] = nc.dram_tensor("dbg_hT", [NB, D, S], F32, kind="ExternalOutput").ap()
        dbg_t["pm"] = nc.dram_tensor("dbg_pm", [NB, 512, S], F32, kind="ExternalOutput").ap()
        dbg_t["ao"] = nc.dram_tensor("dbg_ao", [NB, 256, S], F32, kind="ExternalOutput").ap()
        dbg_t["mod"] = nc.dram_tensor("dbg_mod", [128, 96], F32, kind="ExternalOutput").ap()
        dbg_t["G"] = nc.dram_tensor("dbg_G", [128, T // 128, NE], F32, kind="ExternalOutput").ap()
        if stage >= 7:
            dbg_t["dest"] = nc.dram_tensor("dbg_dest", [128, T // 128 * 8], I32, kind="ExternalOutput").ap()
            dbg_t["gk"] = nc.dram_tensor("dbg_gk", [128, T // 128 * 8], F32, kind="ExternalOutput").ap()
            dbg_t["bex"] = nc.dram_tensor("dbg_bex", [1, NBLK], I32, kind="ExternalOutput").ap()

    with contextlib.ExitStack() as st:
        kb = KB(nc, st)
        ncv, nca, ncp, ncg, ncs = nc.vector, nc.scalar, nc.tensor, nc.gpsimd, nc.sync

        cs = {}
        for name, shape, dt in CONST_SPECS:
            cs[name] = kb.sb("c_" + name, shape, dt)
            kb.dma("sp", lambda n=name: ncs.dma_start(out=cs[n][:], in_=dram[n]), writes=["c_" + name])
        ident = cs["ident"]

        modT = kb.sb("modT", [128, 48, NB])
        gs1 = kb.sb("gs1", [128, 8, NB])
        gs2 = kb.sb("gs2", [128, 8, NB])
        g1T = kb.sb("g1T", [128, 8])
        g2T = kb.sb("g2T", [128, 8])
        lsT = kb.sb("lsT", [128, 4])
        gq = kb.sb("gq", [128, 1])
        gk = kb.sb("gk", [128, 1])

        pA = [kb.ps("pA%d" % i, [128, 1024]) for i in range(2)]
        pB = [kb.ps("pB%d" % i, [128, 512]) for i in range(4)]

        with contextlib.ExitStack() as sa, nc.allow_non_contiguous_dma(reason="tiny transposed vector loads"):
            gate1_b = kb.sb("gate1_b", [128, NB, D], stack=sa)
            gate2_b = kb.sb("gate2_b", [128, NB, D], stack=sa)
            gs2_b = kb.sb("gs2_b", [128, NB, D], stack=sa)
            sh2_b = kb.sb("sh2_b", [128, NB, D], stack=sa)
            cact = kb.sb("cact", [128, 8, NB], stack=sa)
            crep = kb.sb("crep", [128, 8, NB, 128], stack=sa)
            badaT = kb.sb("badaT", [128, 48], stack=sa)
            bada_row = kb.sb("bada_row", [1, 6 * D], stack=sa)
            g2row_b = kb.sb("g2row_b", [128, D], stack=sa)
            wa = [kb.sb("wa%d" % i, [128, 8, 512], stack=sa) for i in range(2)]
            gtmp = kb.sb("gtmp", [128, 64], stack=sa)
            for b_ in range(NB):
                kb.dma("sp", lambda b_=b_: ncs.dma_start(out=cact[:, :, b_], in_=dram["c"][b_, :].rearrange("(k p) -> p k", p=128)), writes=["cact"])
            kb.dma("sp", lambda: ncs.dma_start(out=badaT[:], in_=dram["b_ada"].rearrange("o (j p) -> p (o j)", p=128)), writes=["badaT"])
            kb.dma("sp", lambda: ncs.dma_start(out=bada_row[:], in_=dram["b_ada"]), writes=["bada_row"])
            kb.dma("sp", lambda: ncs.dma_start(out=g1T[:], in_=dram["norm1_g"].rearrange("o (k p) -> p (o k)", p=128)), writes=["g1T"])
            kb.dma("sp", lambda: ncs.dma_start(out=g2T[:], in_=dram["norm2_g"].rearrange("o (k p) -> p (o k)", p=128)), writes=["g2T"])
            kb.dma("sp", lambda: ncs.dma_start(out=lsT[:], in_=dram["pool_scale"].rearrange("o (g p) -> p (o g)", p=128)), writes=["lsT"])
            kb.dma("sp", lambda: ncs.dma_start(out=g2row_b[:], in_=dram["norm2_g"].rearrange("o d -> (o d)").partition_broadcast(128)), writes=["g2row_b"])
            for h2_, (gt, nm) in enumerate(((gq, "q_norm_g"), (gk, "k_norm_g"))):
                for o in (0, 64):
                    kb.dma("sp", lambda gt=gt, nm=nm, o=o: ncs.dma_start(out=gt[o:o + 64, :], in_=dram[nm].rearrange("o d -> d o")),
                           writes=["gq" if gt is gq else "gk"])
            kb.op("dve", lambda: ncv.tensor_scalar(out=gq[:], in0=gq[:], scalar1=8.0, scalar2=None, op0=ALU.mult), reads=["gq"], writes=["gq"])
            kb.op("dve", lambda: ncv.tensor_scalar(out=gk[:], in0=gk[:], scalar1=8.0, scalar2=None, op0=ALU.mult), reads=["gk"], writes=["gk"])
            kb.op("act", lambda: nca.activation(out=cact[:], in_=cact[:], func=AF.Silu), reads=["cact"], writes=["cact"])
            for kc in range(8):
                for b in range(NB):
                    kb.op("dve", lambda kc=kc, b=b: ncv.tensor_copy(out=crep[:, kc, b, :], in_=cact[:, kc, b:b + 1].to_broadcast([128, 128])),
                          reads=["cact"], writes=["crep"])
            pm = pB[0]
            for t in range(12):
                w = wa[t % 2]
                wk = "wa%d" % (t % 2)
                kb.dma("sp" if t % 2 == 0 else "act",
                       lambda t=t, w=w: (ncs if t % 2 == 0 else nca).dma_start(
                           out=w[:], in_=dram["w_ada"][:, t * 512:(t + 1) * 512].rearrange("(k p) n -> p k n", p=128)),
                       writes=[wk])
                for jj in range(4):
                    j = 4 * t + jj
                    for kc in range(8):
                        kb.op("pe", lambda j=j, jj=jj, kc=kc, w=w: ncp.matmul(pm[:, 2 * j:2 * j + 2], w[:, kc, jj * 128:(jj + 1) * 128], cact[:, kc, :],
                                                                            start=(kc == 0), stop=(kc == 7)),
                              reads=[wk, "cact"], writes=["pB0"])
                if t in (4, 5, 10, 11):
                    dst = gate1_b if t in (4, 5) else gate2_b
                    dk = "gate1_b" if t in (4, 5) else "gate2_b"
                    half = t % 2 if t in (4, 5) else (t - 10)
                    for b in range(NB):
                        pg = pB[1 + b]
                        for kc in range(8):
                            kb.op("pe", lambda kc=kc, b=b, w=w, pg=pg: ncp.matmul(pg[:, :], crep[:, kc, b, :], w[:, kc, :], start=(kc == 0), stop=False),
                                  reads=[wk, "crep"], writes=["pB%d" % (1 + b)])
                        kb.op("pe", lambda t=t, pg=pg: ncp.matmul(pg[:, :], cs["ones_f"][0:1, :], bada_row[0:1, t * 512:(t + 1) * 512], start=False, stop=True),
                              reads=["bada_row", "c_ones_f"], writes=["pB%d" % (1 + b)])
                        kb.op("act", lambda b=b, pg=pg, dst=dst, half=half: nca.copy(out=dst[:, b, half * 512:(half + 1) * 512], in_=pg[:, :]),
                              reads=["pB%d" % (1 + b)], writes=[dk])
                if t in (6, 7, 8, 9):
                    dst = sh2_b if t in (6, 7) else gs2_b
                    dk = "sh2_b" if t in (6, 7) else "gs2_b"
                    half = t % 2
                    for b in range(NB):
                        pg = pB[1 + b]
                        for kc in range(8):
                            kb.op("pe", lambda kc=kc, b=b, w=w, pg=pg: ncp.matmul(pg[:, :], crep[:, kc, b, :], w[:, kc, :], start=(kc == 0), stop=False),
                                  reads=[wk, "crep"], writes=["pB%d" % (1 + b)])
                        kb.op("pe", lambda t=t, pg=pg: ncp.matmul(pg[:, :], cs["ones_f"][0:1, :], bada_row[0:1, t * 512:(t + 1) * 512], start=False, stop=True),
                              reads=["bada_row", "c_ones_f"], writes=["pB%d" % (1 + b)])
                        if t in (6, 7):
                            kb.op("act", lambda b=b, pg=pg, dst=dst, half=half: nca.copy(out=dst[:, b, half * 512:(half + 1) * 512], in_=pg[:, :]),
                                  reads=["pB%d" % (1 + b)], writes=[dk])
                        else:
                            kb.op("dve", lambda b=b, pg=pg, half=half: ncv.scalar_tensor_tensor(
                                out=gs2_b[:, b, half * 512:(half + 1) * 512], in0=pg[:, :], scalar=1.0,
                                in1=g2row_b[:, half * 512:(half + 1) * 512], op0=ALU.add, op1=ALU.mult),
                                reads=["pB%d" % (1 + b), "g2row_b"], writes=[dk])
            for b in range(NB):
                kb.op("dve", lambda b=b: ncv.tensor_tensor(out=modT[:, :, b], in0=pm[:, b:96:2], in1=badaT[:, :], op=ALU.add),
                      reads=["pB0", "badaT"], writes=["modT"])
                kb.op("dve", lambda b=b: ncv.scalar_tensor_tensor(out=gs1[:, :, b], in0=modT[:, 8:16, b], scalar=1.0, in1=g1T[:, :], op0=ALU.add, op1=ALU.mult),
                      reads=["modT", "g1T"], writes=["gs1"])
                kb.op("dve", lambda b=b: ncv.scalar_tensor_tensor(out=gs2[:, :, b], in0=modT[:, 32:40, b], scalar=1.0, in1=g2T[:, :], op0=ALU.add, op1=ALU.mult),
                      reads=["modT", "g2T"], writes=["gs2"])
            for i_, (t_, k_) in enumerate(((gate2_b, "gate2_b"), (gs2_b, "gs2_b"), (sh2_b, "sh2_b"), (gate1_b, "gate1_b"))):
                kb.dma("sp", lambda i_=i_, t_=t_: ncs.dma_start(out=BCd[i_], in_=t_[:].rearrange("p b d -> p (b d)")), reads=[k_], writes=["BCd"])
            if dbg:
                kb.dma("sp", lambda: ncs.dma_start(out=dbg_t["mod"], in_=modT[:].rearrange("p j b -> p (j b)")), reads=["modT"], writes=["dbg_mod"])
            kb.barrier()

        if stage >= 1:
            for b in range(NB):
                phase_B(nc, kb, b, dram, cs, dict(modT=modT, gs1=gs1, BCd=BCd, lsT=lsT, gq=gq, gk=gk),
                        pA, pB, HTd, PMd, AOd, out, dbg_t, stage)
        if stage >= 5:
            phase_C(nc, kb, dram, cs, dict(BCd=BCd, H2d=H2d, XGd=XGd, Yd=Yd, BEXd=BEXd, Gd=Gd), pA, pB, out, dbg_t, stage)
        kb.drain("sp")
    return nc


def phase_C(nc, kb, dram, cs, pv, pA, pB, out, dbg_t, stage):
    ncv, nca, ncp, ncg, ncs = nc.vector, nc.scalar, nc.tensor, nc.gpsimd, nc.sync
    ident = cs["ident"]
    BCd = pv["BCd"]
    with contextlib.ExitStack() as s5:
        s5a = contextlib.ExitStack()
        gate2_b = kb.sb("gate2_bc", [128, NB, D], stack=s5)
        NT = T // 128
        dense = stage < 7
        Gall = kb.sb("Gall", [128, NT, NE], stack=(s5a if dense else s5))
        Mall = None if dense else kb.sb("Mall", [128, NT, NE], BF16, stack=s5)
        gs2_b = kb.sb("gs2_bc", [128, NB, D], stack=s5a)
        sh2_b = kb.sb("sh2_bc", [128, NB, D], stack=s5a)
        for i_, (t_, k_) in enumerate(((gate2_b, "gate2_b"), (gs2_b, "gs2_b"), (sh2_b, "sh2_b"))):
            kb.dma("sp", lambda i_=i_, t_=t_: ncs.dma_start(out=t_[:].rearrange("p b d -> p (b d)"), in_=BCd[i_]), reads=["BCd"], writes=[k_])
        wsgu = kb.sb("wsgu", [128, 8, 512], F32R, stack=s5a)
        wsd = kb.sb("wsd", [128, 2, D], F32R, stack=s5a)
        kb.dma("pool", lambda: ncg.dma_start(out=wsgu[:, :, 0:256], in_=dram["w_shared_gate"].rearrange("(k p) n -> p k n", p=128)), writes=["wsgu"])
        kb.dma("pool", lambda: ncg.dma_start(out=wsgu[:, :, 256:512], in_=dram["w_shared_up"].rearrange("(k p) n -> p k n", p=128)), writes=["wsgu"])
        kb.dma("pool", lambda: ncg.dma_start(out=wsd[:], in_=dram["w_shared_down"].rearrange("(k p) n -> p k n", p=128)), writes=["wsd"])
        NT = T // 128
        H2d = pv["H2d"]
        wr = kb.sb("wr", [128, 8, NE], F32R, stack=s5a)
        kb.dma("pool", lambda: ncg.dma_start(out=wr[:], in_=dram["w_router"].rearrange("(k p) n -> p k n", p=128)), writes=["wr"])
        rbias = kb.sb("rbias", [128, NE], stack=s5a)
        kb.dma("sp", lambda: ncs.dma_start(out=rbias[:], in_=dram["router_bias"].rearrange("o d -> (o d)").partition_broadcast(128)), writes=["rbias"])
        sc = kb.sb("sc", [128, NE], stack=s5a)
        sel = kb.sb("sel", [128, NE], stack=s5a)
        msk = kb.sb("msk", [128, NE], stack=s5a)
        selm = kb.sb("selm", [128, NE], stack=s5a)
        wtmp = kb.sb("wtmp", [128, NE], stack=s5a)
        m8g = kb.sb("m8g", [128, 8, 8], stack=s5a)
        gsc = kb.sb("gsc", [128, 8], stack=s5a)
        m8 = kb.sb("m8", [128, 8], stack=s5a)
        gm = kb.sb("gm", [128, 8], stack=s5a)
        pen = kb.sb("pen", [128, 8], stack=s5a)
        wsum = kb.sb("wsum", [128, 1], stack=s5a)
        x1 = [kb.sb("x1t%d" % i, [128, D], stack=s5a) for i in range(2)]
        xn = [kb.sb("xn2%d" % i, [128, D], stack=s5a) for i in range(2)]
        h2 = [kb.sb("h2t%d" % i, [128, D], stack=s5a) for i in range(2)]
        h2T = [kb.sb("h2T%d" % i, [128, 8, 128], F32R, stack=s5a) for i in range(2)]
        junk = kb.sb("junk2", [128, D], stack=s5a)
        ss = kb.sb("ss2", [128, 2], stack=s5a)
        rstd = kb.sb("rstd2", [128, 2], stack=s5a)
        sg = [kb.sb("sg%d" % i, [128, 256], stack=s5a) for i in range(2)]
        act = [kb.sb("actt%d" % i, [128, 256], stack=s5a) for i in range(2)]
        actT = [kb.sb("actT%d" % i, [128, 2, 128], F32R, stack=s5a) for i in range(2)]
        ot = [kb.sb("ot%d" % i, [128, D], stack=s5a) for i in range(2)]
        for tt in range(T // 128):
            i = tt % 2
            b = tt // (S // 128)
            r0 = tt * 128
            kb.dma("sp", lambda i=i, r0=r0: ncs.dma_start(out=x1[i][:], in_=out[r0:r0 + 128, :]), reads=["out"], writes=["x1t%d" % i])
            kb.op("act", lambda i=i: nca.activation(out=junk[:], in_=x1[i][:], func=AF.Square, accum_out=ss[:, i:i + 1]),
                  reads=["x1t%d" % i], writes=["junk2", "ss2%d" % i])
            kb.op("act", lambda i=i: nca.activation(out=rstd[:, i:i + 1], in_=ss[:, i:i + 1], func=AF.Sqrt, scale=1.0 / D, bias=cs["epsc"][:, 0:1]),
                  reads=["ss2%d" % i, "c_epsc"], writes=["rstd2%d" % i])
            kb.op("dve", lambda i=i: ncv.reciprocal(out=rstd[:, i:i + 1], in_=rstd[:, i:i + 1]), reads=["rstd2%d" % i], writes=["rstd2%d" % i])
            kb.op("act", lambda i=i: nca.activation(out=xn[i][:], in_=x1[i][:], func=AF.Identity, scale=rstd[:, i:i + 1]),
                  reads=["x1t%d" % i, "rstd2%d" % i], writes=["xn2%d" % i])
            kb.op("dve", lambda i=i, b=b: ncv.tensor_tensor(out=h2[i][:], in0=xn[i][:], in1=gs2_b[:, b, :], op=ALU.mult),
                  reads=["xn2%d" % i, "gs2_b"], writes=["h2t%d" % i])
            kb.op("pool", lambda i=i, b=b: ncg.tensor_tensor(out=h2[i][:], in0=h2[i][:], in1=sh2_b[:, b, :], op=ALU.add),
                  reads=["h2t%d" % i, "sh2_b"], writes=["h2t%d" % i])
            pa, pak = pA[i], "pA%d" % i
            for kc in range(8):
                kb.op("pe", lambda kc=kc, i=i, pa=pa: ncp.transpose(pa[:, kc * 128:(kc + 1) * 128], h2[i][:, kc * 128:(kc + 1) * 128], ident[:]),
                      reads=["h2t%d" % i, "c_ident"], writes=[pak])
            kb.op("act", lambda i=i, pa=pa: nca.copy(out=h2T[i][:, 0:4, :], in_=pa[:, 0:512].rearrange("p (k t) -> p k t", k=4)), reads=[pak], writes=["h2T%d" % i])
            kb.op("dve", lambda i=i, pa=pa: ncv.tensor_copy(out=h2T[i][:, 4:8, :], in_=pa[:, 512:1024].rearrange("p (k t) -> p k t", k=4)), reads=[pak], writes=["h2T%d" % i])
            kb.dma("sp", lambda i=i, r0=r0: ncs.dma_start(out=H2d[r0:r0 + 128, :], in_=h2[i][:]), reads=["h2t%d" % i], writes=["H2d"])
            pr, prk = pB[2 + i], "pB%d" % (2 + i)
            for kc in range(8):
                kb.op("pe", lambda kc=kc, i=i, pr=pr: ncp.matmul(pr[:, 0:NE], h2T[i][:, kc, :], wr[:, kc, :], start=(kc == 0), stop=(kc == 7)),
                      reads=["h2T%d" % i, "wr"], writes=[prk])
            kb.op("act", lambda pr=pr: nca.activation(out=sc[:], in_=pr[:, 0:NE], func=AF.Sigmoid), reads=[prk], writes=["sc"])
            kb.op("dve", lambda: ncv.tensor_tensor(out=sel[:], in0=sc[:], in1=rbias[:], op=ALU.add), reads=["sc", "rbias"], writes=["sel"])
            for g in range(8):
                kb.op("dve", lambda g=g: ncv.max(out=m8g[:, g, :], in_=sel[:, g * 32:(g + 1) * 32]), reads=["sel"], writes=["m8g"])
            kb.op("dve", lambda: ncv.tensor_tensor(out=gsc[:], in0=m8g[:, :, 0], in1=m8g[:, :, 1], op=ALU.add), reads=["m8g"], writes=["gsc"])
            kb.op("dve", lambda: ncv.max(out=m8[:], in_=gsc[:]), reads=["gsc"], writes=["m8"])
            kb.op("dve", lambda: ncv.tensor_scalar(out=gm[:], in0=gsc[:], scalar1=m8[:, 3:4], scalar2=None, op0=ALU.is_ge), reads=["gsc", "m8"], writes=["gm"])
            kb.op("dve", lambda: ncv.tensor_scalar(out=pen[:], in0=gm[:], scalar1=-1.0, scalar2=1.0e4, op0=ALU.add, op1=ALU.mult), reads=["gm"], writes=["pen"])
            for g in range(8):
                kb.op("dve", lambda g=g: ncv.tensor_scalar(out=msk[:, g * 32:(g + 1) * 32], in0=sel[:, g * 32:(g + 1) * 32], scalar1=gm[:, g:g + 1],
                                                          scalar2=pen[:, g:g + 1], op0=ALU.mult, op1=ALU.add),
                      reads=["sel", "gm", "pen"], writes=["msk"])
            kb.op("dve", lambda: ncv.max(out=m8[:], in_=msk[:]), reads=["msk"], writes=["m8"])
            kb.op("dve", lambda: ncv.tensor_scalar(out=selm[:], in0=msk[:], scalar1=m8[:, 7:8], scalar2=None, op0=ALU.is_ge), reads=["msk", "m8"], writes=["selm"])
            kb.op("dve", lambda: ncv.scalar_tensor_tensor(out=wtmp[:], in0=sc[:], scalar=1.0, in1=selm[:], op0=ALU.mult, op1=ALU.mult, accum_out=wsum[:, 0:1]),
                  reads=["sc", "selm"], writes=["wtmp", "wsum"])
            kb.op("dve", lambda: ncv.reciprocal(out=wsum[:], in_=wsum[:]), reads=["wsum"], writes=["wsum"])
            kb.op("dve", lambda tt=tt: ncv.tensor_scalar(out=Gall[:, tt, :], in0=wtmp[:], scalar1=wsum[:, 0:1], scalar2=2.5, op0=ALU.mult, op1=ALU.mult),
                  reads=["wtmp", "wsum"], writes=["Gall"])
            if Mall is not None:
                kb.op("pool", lambda tt=tt: ncg.tensor_copy(out=Mall[:, tt, :], in_=selm[:]), reads=["selm"], writes=["Mall"])
            pg, pgk = pB[i], "pB%d" % i
            for kc in range(8):
                kb.op("pe", lambda kc=kc, i=i, pg=pg: ncp.matmul(pg[:, :], h2T[i][:, kc, :], wsgu[:, kc, :], start=(kc == 0), stop=(kc == 7)),
                      reads=["h2T%d" % i, "wsgu"], writes=[pgk])
            kb.op("act", lambda i=i, pg=pg: nca.activation(out=sg[i][:], in_=pg[:, 0:256], func=AF.Silu), reads=[pgk], writes=["sg%d" % i])
            kb.op("dve", lambda i=i, pg=pg: ncv.tensor_tensor(out=act[i][:], in0=sg[i][:], in1=pg[:, 256:512], op=ALU.mult), reads=[pgk, "sg%d" % i], writes=["actt%d" % i])
            pt, ptk = pB[2 + i], "pB%d" % (2 + i)
            for j in range(2):
                kb.op("pe", lambda j=j, i=i, pt=pt: ncp.transpose(pt[:, j * 128:(j + 1) * 128], act[i][:, j * 128:(j + 1) * 128], ident[:]),
                      reads=["actt%d" % i, "c_ident"], writes=[ptk])
            kb.op("act", lambda i=i, pt=pt: nca.copy(out=actT[i][:, :, :], in_=pt[:, 0:256].rearrange("p (k t) -> p k t", k=2)), reads=[ptk], writes=["actT%d" % i])
            for hf in range(2):
                for j in range(2):
                    kb.op("pe", lambda j=j, hf=hf, i=i, pa=pa: ncp.matmul(pa[:, hf * 512:(hf + 1) * 512], actT[i][:, j, :], wsd[:, j, hf * 512:(hf + 1) * 512],
                                                                         start=(j == 0), stop=(j == 1)),
                          reads=["actT%d" % i, "wsd"], writes=[pak])
            kb.op("dve", lambda i=i, b=b, pa=pa: ncv.tensor_tensor(out=ot[i][:], in0=pa[:, :], in1=gate2_b[:, b, :], op=ALU.mult),
                  reads=[pak, "gate2_b"], writes=["ot%d" % i])
            kb.op("pool", lambda i=i: ncg.tensor_tensor(out=ot[i][:], in0=ot[i][:], in1=x1[i][:], op=ALU.add),
                  reads=["ot%d" % i, "x1t%d" % i], writes=["ot%d" % i])
            kb.dma("sp", lambda i=i, r0=r0: ncs.dma_start(out=out[r0:r0 + 128, :], in_=ot[i][:]), reads=["ot%d" % i], writes=["out"])
        if dbg_t:
            kb.dma("sp", lambda: ncs.dma_start(out=dbg_t["G"], in_=Gall[:]), reads=["Gall"], writes=["dbg_G"])
        if dense:
            kb.dma("sp", lambda: ncs.dma_start(out=pv["Gd"], in_=Gall[:]), reads=["Gall"], writes=["Gd"])
        kb.barrier()
        s5a.close()
        if stage >= 6:
            if stage >= 7:
                phase_R(nc, kb, dram, cs, pv, pA, pB, out, gate2_b, Gall, Mall, dbg_t)
            else:
                phase_R_dense(nc, kb, dram, cs, pv, pA, pB, out, gate2_b)


def phase_R_dense(nc, kb, dram, cs, pv, pA, pB, out, gate2_b):
    ncv, nca, ncp, ncg, ncs = nc.vector, nc.scalar, nc.tensor, nc.gpsimd, nc.sync
    ident = cs["ident"]
    H2d, Gd = pv["H2d"], pv["Gd"]
    NS = DENSE_TB // 128
    NW = 3
    with contextlib.ExitStack() as s4:
        wgu = [kb.sb("wgu%d" % i, [128, 8, 512], F32R, stack=s4) for i in range(NW)]
        wdn = [kb.sb("wdn%d" % i, [128, 2, D], F32R, stack=s4) for i in range(NW)]
        Gb = kb.sb("Gb", [128, NS, NE], stack=s4)
        h2T = kb.sb("h2Tb", [128, 8, DENSE_TB], F32R, stack=s4)
        acc = [kb.sb("accd%d" % i, [128, D], stack=s4) for i in range(NS)]
        sg = [kb.sb("sgr%d" % i, [128, 256], stack=s4) for i in range(3)]
        act = [kb.sb("actr%d" % i, [128, 256], stack=s4) for i in range(3)]
        actT = [kb.sb("actTr%d" % i, [128, 2, 128], F32R, stack=s4) for i in range(3)]
        ot = [kb.sb("otr%d" % i, [128, D], stack=s4) for i in range(2)]

        def load_w(e):
            w = e % NW
            kb.dma("pool", lambda: ncg.dma_start(out=wgu[w][:, :, 0:256], in_=dram["w_exp_gate"][e].rearrange("(k p) n -> p k n", p=128)), writes=["wgu%d" % w])
            kb.dma("pool", lambda: ncg.dma_start(out=wgu[w][:, :, 256:512], in_=dram["w_exp_up"][e].rearrange("(k p) n -> p k n", p=128)), writes=["wgu%d" % w])
            kb.dma("pool", lambda: ncg.dma_start(out=wdn[w][:], in_=dram["w_exp_down"][e].rearrange("(k p) n -> p k n", p=128)), writes=["wdn%d" % w])

        units = [(e, sidx) for e in range(NE) for sidx in range(NS)]
        U = len(units)

        def gu(n):
            e, sidx = units[n]
            w = e % NW
            pg, pgk = pB[n % 2], "pB%d" % (n % 2)
            for kc in range(8):
                kb.op("pe", lambda kc=kc: ncp.matmul(pg[:, :], h2T[:, kc, sidx * 128:(sidx + 1) * 128], wgu[w][:, kc, :], start=(kc == 0), stop=(kc == 7)),
                      reads=["h2Tb", "wgu%d" % w], writes=[pgk], inc=(kc == 7))

        def mid1(n):
            pg, pgk = pB[n % 2], "pB%d" % (n % 2)
            q = n % 3
            kb.op("act", lambda: nca.activation(out=sg[q][:], in_=pg[:, 0:256], func=AF.Silu), reads=[pgk], writes=["sgr%d" % q])
            kb.op("dve", lambda: ncv.tensor_tensor(out=act[q][:], in0=sg[q][:], in1=pg[:, 256:512], op=ALU.mult), reads=[pgk, "sgr%d" % q], writes=["actr%d" % q])

        def tr(n):
            pt, ptk = pB[2 + n % 2], "pB%d" % (2 + n % 2)
            q = n % 3
            for j in range(2):
                kb.op("pe", lambda j=j: ncp.transpose(pt[:, j * 128:(j + 1) * 128], act[q][:, j * 128:(j + 1) * 128], ident[:]),
                      reads=["actr%d" % q, "c_ident"], writes=[ptk], inc=(j == 1))

        def mid2(n):
            pt, ptk = pB[2 + n % 2], "pB%d" % (2 + n % 2)
            q = n % 3
            kb.op("act", lambda: nca.copy(out=actT[q][:, :, :], in_=pt[:, 0:256].rearrange("p (k t) -> p k t", k=2)), reads=[ptk], writes=["actTr%d" % q])

        def dn(n):
            e, sidx = units[n]
            w = e % NW
            pa, pak = pA[n % 2], "pA%d" % (n % 2)
            q = n % 3
            for hf in range(2):
                for j in range(2):
                    kb.op("pe", lambda j=j, hf=hf: ncp.matmul(pa[:, hf * 512:(hf + 1) * 512], actT[q][:, j, :], wdn[w][:, j, hf * 512:(hf + 1) * 512],
                                                              start=(j == 0), stop=(j == 1)),
                          reads=["actTr%d" % q, "wdn%d" % w], writes=[pak], inc=(hf == 1 and j == 1))

        def fin(n):
            e, sidx = units[n]
            pa, pak = pA[n % 2], "pA%d" % (n % 2)
            kb.op("dve", lambda: ncv.scalar_tensor_tensor(out=acc[sidx][:], in0=pa[:, :], scalar=Gb[:, sidx, e:e + 1], in1=acc[sidx][:], op0=ALU.mult, op1=ALU.add),
                  reads=[pak, "Gb", "accd%d" % sidx], writes=["accd%d" % sidx])

        for tb in range(T // DENSE_TB):
            kb.dma("sp", lambda tb=tb: ncs.dma_start(out=Gb[:], in_=Gd[:, tb * NS:(tb + 1) * NS, :]), reads=["Gd"], writes=["Gb"])
            load_w(0)
            load_w(1)
            for sidx in range(NS):
                i = tb * NS + sidx
                j = sidx % 2
                kb.dma("sp", lambda i=i, j=j: ncs.dma_start(out=ot[j][:], in_=H2d[i * 128:(i + 1) * 128, :]), reads=["H2d"], writes=["otr%d" % j])
                pa, pak = pA[j], "pA%d" % j
                for kc in range(8):
                    kb.op("pe", lambda kc=kc, j=j, pa=pa: ncp.transpose(pa[:, kc * 128:(kc + 1) * 128], ot[j][:, kc * 128:(kc + 1) * 128], ident[:]),
                          reads=["otr%d" % j, "c_ident"], writes=[pak], inc=(kc == 7))
                kb.op("act", lambda sidx=sidx, pa=pa: nca.copy(out=h2T[:, 0:4, sidx * 128:(sidx + 1) * 128], in_=pa[:, 0:512].rearrange("p (k t) -> p k t", k=4)),
                      reads=[pak], writes=["h2Tb"])
                kb.op("dve", lambda sidx=sidx, pa=pa: ncv.tensor_copy(out=h2T[:, 4:8, sidx * 128:(sidx + 1) * 128], in_=pa[:, 512:1024].rearrange("p (k t) -> p k t", k=4)),
                      reads=[pak], writes=["h2Tb"])
                kb.op("pool", lambda sidx=sidx: ncg.memset(acc[sidx][:], 0.0), writes=["accd%d" % sidx])
            gu(0)
            mid1(0)
            for n in range(U):
                if n + 1 < U:
                    gu(n + 1)
                    mid1(n + 1)
                tr(n)
                mid2(n)
                if n >= 1:
                    dn(n - 1)
                    fin(n - 1)
                e, sidx = units[n]
                if sidx == 1 and e + 2 < NE:
                    load_w(e + 2)
            dn(U - 1)
            fin(U - 1)
            for sidx in range(NS):
                i = tb * NS + sidx
                j = sidx % 2
                b = i // (S // 128)
                kb.dma("sp", lambda i=i, j=j: ncs.dma_start(out=ot[j][:], in_=out[i * 128:(i + 1) * 128, :]), reads=["out"], writes=["otr%d" % j])
                kb.op("pool", lambda sidx=sidx, b=b: ncg.tensor_tensor(out=acc[sidx][:], in0=acc[sidx][:], in1=gate2_b[:, b, :], op=ALU.mult),
                      reads=["accd%d" % sidx, "gate2_b"], writes=["accd%d" % sidx])
                kb.op("pool", lambda sidx=sidx, j=j: ncg.tensor_tensor(out=ot[j][:], in0=ot[j][:], in1=acc[sidx][:], op=ALU.add),
                      reads=["accd%d" % sidx, "otr%d" % j], writes=["otr%d" % j])
                kb.dma("sp", lambda i=i, j=j: ncs.dma_start(out=out[i * 128:(i + 1) * 128, :], in_=ot[j][:]), reads=["otr%d" % j], writes=["out"])
        kb.barrier()


def phase_R(nc, kb, dram, cs, pv, pA, pB, out, gate2_b, Gall, Mall, dbg_t):
    ncv, nca, ncp, ncg, ncs = nc.vector, nc.scalar, nc.tensor, nc.gpsimd, nc.sync
    ident = cs["ident"]
    NT = T // 128
    R = NBLK * 128
    BIGK = 70000.0
    H2d, XGd, Yd, BEXd = pv["H2d"], pv["XGd"], pv["Yd"], pv["BEXd"]
    with contextlib.ExitStack() as sr:
        DESTi = kb.sb("DESTi", [128, NT * 8], I32, stack=sr)
        GK = kb.sb("GK", [128, NT * 8], stack=sr)
        bexrow = kb.sb("bexrow", [1, NBLK], I32, stack=sr)
        with contextlib.ExitStack() as s2:
            RANK = kb.sb("RANK", [128, NT, NE], stack=s2)
            base = kb.sb("base", [128, NE], stack=s2)
            nbk = kb.sb("nbk", [128, NE], stack=s2)
            padded = kb.sb("padded", [128, NE], stack=s2)
            cA = kb.sb("cA", [128, NE], stack=s2)
            cB = kb.sb("cB", [128, NE], stack=s2)
            key = kb.sb("key", [128, NE], stack=s2)
            jk = kb.sb("jk", [128, NE], stack=s2)
            ones256 = kb.sb("ones256", [128, NE], stack=s2)
            m8 = kb.sb("m8r", [128, 8], stack=s2)
            destf = kb.sb("destf", [128, NT * 8], stack=s2)
            bx = kb.sb("bx", [128, 4], stack=s2)
            bxi = kb.sb("bxi", [128, 4], I32, stack=s2)
            kb.op("pool", lambda: ncg.memset(base[:], 0.0), writes=["base"])
            kb.op("pool", lambda: ncg.memset(nbk[:], 0.0), writes=["nbk"])
            kb.op("pool", lambda: ncg.memset(ones256[:], 1.0), writes=["ones256"])
            for i in range(NT):
                pt, ptk = pB[i % 2], "pB%d" % (i % 2)
                kb.op("pe", lambda i=i, pt=pt: ncp.matmul(pt[:, 0:NE], cs["tri"][:], Mall[:, i, :], start=True, stop=True), reads=["Mall", "c_tri"], writes=[ptk])
                kb.op("pe", lambda i=i, pt=pt: ncp.matmul(pt[:, NE:2 * NE], cs["ones_bf"][:], Mall[:, i, :], start=True, stop=True), reads=["Mall", "c_ones_bf"], writes=[ptk])
                kb.op("dve", lambda i=i, pt=pt: ncv.tensor_tensor(out=RANK[:, i, :], in0=pt[:, 0:NE], in1=base[:], op=ALU.add), reads=[ptk, "base"], writes=["RANK"])
                kb.op("dve", lambda pt=pt: ncv.tensor_tensor(out=base[:], in0=base[:], in1=pt[:, NE:2 * NE], op=ALU.add), reads=[ptk, "base"], writes=["base"])
            for k in range(T // 128):
                kb.op("dve", lambda k=k: ncv.scalar_tensor_tensor(out=nbk[:], in0=base[:], scalar=128.0 * k, in1=nbk[:], op0=ALU.is_gt, op1=ALU.add),
                      reads=["base", "nbk"], writes=["nbk"])
            kb.op("dve", lambda: ncv.tensor_scalar(out=padded[:], in0=nbk[:], scalar1=128.0, scalar2=None, op0=ALU.mult), reads=["nbk"], writes=["padded"])
            kb.op("dve", lambda: ncv.tensor_copy(out=cA[:], in_=padded[:]), reads=["padded"], writes=["cA"])
            cur, curk, nxt, nxtk = cA, "cA", cB, "cB"
            sft = 1
            while sft < NE:
                kb.op("dve", lambda cur=cur, nxt=nxt, sft=sft: ncv.tensor_copy(out=nxt[:, 0:sft], in_=cur[:, 0:sft]), reads=[curk], writes=[nxtk])
                kb.op("dve", lambda cur=cur, nxt=nxt, sft=sft: ncv.tensor_tensor(out=nxt[:, sft:NE], in0=cur[:, sft:NE], in1=cur[:, 0:NE - sft], op=ALU.add),
                      reads=[curk], writes=[nxtk])
                cur, curk, nxt, nxtk = nxt, nxtk, cur, curk
                sft *= 2
            pend, pendk = cur, curk
            pstart, pstartk = nxt, nxtk
            kb.op("dve", lambda: ncv.tensor_tensor(out=pstart[:], in0=pend[:], in1=padded[:], op=ALU.subtract), reads=[pendk, "padded"], writes=[pstartk])
            for i in range(NT):
                kb.op("dve", lambda i=i: ncv.tensor_tensor(out=key[:], in0=RANK[:, i, :], in1=pstart[:], op=ALU.add), reads=["RANK", pstartk], writes=["key"])
                kb.op("dve", lambda: ncv.tensor_scalar(out=key[:], in0=key[:], scalar1=-1.0, scalar2=BIGK + 1.0, op0=ALU.mult, op1=ALU.add), reads=["key"], writes=["key"])
                kb.op("dve", lambda i=i: ncv.tensor_tensor(out=key[:], in0=key[:], in1=Mall[:, i, :], op=ALU.mult), reads=["key", "Mall"], writes=["key"])
                kb.op("dve", lambda: ncv.max(out=m8[:], in_=key[:]), reads=["key"], writes=["m8r"])
                kb.op("dve", lambda i=i: ncv.tensor_scalar(out=destf[:, i * 8:(i + 1) * 8], in0=m8[:], scalar1=-1.0, scalar2=BIGK + 1.0, op0=ALU.mult, op1=ALU.add),
                      reads=["m8r"], writes=["destf"])
                for k in range(8):
                    kb.op("dve", lambda i=i, k=k: ncv.scalar_tensor_tensor(out=jk[:], in0=key[:], scalar=m8[:, k:k + 1], in1=Gall[:, i, :], op0=ALU.is_equal, op1=ALU.mult,
                                                                             accum_out=GK[:, i * 8 + k:i * 8 + k + 1]),
                          reads=["key", "m8r", "Gall"], writes=["jk", "GK"])
            kb.op("dve", lambda: ncv.tensor_copy(out=DESTi[:], in_=destf[:]), reads=["destf"], writes=["DESTi"])
            for j in range(4):
                kb.op("dve", lambda j=j: ncv.scalar_tensor_tensor(out=jk[:], in0=pend[:], scalar=cs["thr4"][:, j:j + 1], in1=ones256[:], op0=ALU.is_le, op1=ALU.mult,
                                                                  accum_out=bx[:, j:j + 1]),
                      reads=[pendk, "c_thr4", "ones256"], writes=["jk", "bx"])
            kb.op("dve", lambda: ncv.tensor_scalar(out=bx[:], in0=bx[:], scalar1=float(NE - 1), scalar2=None, op0=ALU.min), reads=["bx"], writes=["bx"])
            kb.op("dve", lambda: ncv.tensor_copy(out=bxi[:], in_=bx[:]), reads=["bx"], writes=["bxi"])
            with nc.allow_non_contiguous_dma(reason="tiny block->expert table transpose"):
                kb.dma("sp", lambda: ncs.dma_start(out=BEXd.rearrange("(j p) -> p j", p=128), in_=bxi[:]), reads=["bxi"], writes=["BEXd"])
            kb.dma("sp", lambda: ncs.dma_start(out=bexrow[:], in_=BEXd.rearrange("(o n) -> o n", o=1)), reads=["BEXd"], writes=["bexrow"])
            if dbg_t:
                kb.dma("sp", lambda: ncs.dma_start(out=dbg_t["dest"], in_=DESTi[:]), reads=["DESTi"], writes=["dbg_dest"])
                kb.dma("sp", lambda: ncs.dma_start(out=dbg_t["gk"], in_=GK[:]), reads=["GK"], writes=["dbg_gk"])
                kb.dma("sp", lambda: ncs.dma_start(out=dbg_t["bex"], in_=bexrow[:]), reads=["bexrow"], writes=["dbg_bex"])
            kb.barrier()
        ssem = kb.stack.enter_context(nc.semaphore("ix_scatter"))
        with contextlib.ExitStack() as s3:
            h2all = kb.sb("h2all", [128, NT, D], stack=s3)
            for i in range(NT):
                kb.dma("sp" if i % 2 == 0 else "act", lambda i=i: (ncs if i % 2 == 0 else nca).dma_start(out=h2all[:, i, :], in_=H2d[i * 128:(i + 1) * 128, :]),
                       reads=["H2d"], writes=["h2all%d" % i])
            nsc = 0
            for i in range(NT):
                kb._need("pool", kb._deps(["h2all%d" % i, "DESTi"], []))
                for k in range(8):
                    ncg.indirect_dma_start(
                        out=XGd[:, :], out_offset=bass.IndirectOffsetOnAxis(ap=DESTi[:, i * 8 + k:i * 8 + k + 1], axis=0),
                        in_=h2all[:, i, :], in_offset=None, bounds_check=R - 1, oob_is_err=False).then_inc(ssem, 16)
                    nsc += 1
            tok = (ssem, "ix_scatter", 16 * nsc, "dma")
            kb._record(tok, ["h2all%d" % i for i in range(NT)] + ["DESTi"], ["XGd"])
            kb.dma("sp", lambda: ncs.dma_start(out=BEXd[0:1], in_=BEXd[0:1]), reads=["XGd"], writes=["relay"])
            kb.barrier()
            kb._record(tok, [], ["XGd"])
        with contextlib.ExitStack() as s4:
            wgu = [kb.sb("wgu%d" % i, [128, 8, 512], F32R, stack=s4) for i in range(2)]
            wdn = [kb.sb("wdn%d" % i, [128, 2, D], F32R, stack=s4) for i in range(2)]
            xg = [kb.sb("xg%d" % i, [128, D], stack=s4) for i in range(2)]
            xgT = [kb.sb("xgT%d" % i, [128, 8, 128], F32R, stack=s4) for i in range(2)]
            sg = [kb.sb("sgr%d" % i, [128, 256], stack=s4) for i in range(2)]
            act = [kb.sb("actr%d" % i, [128, 256], stack=s4) for i in range(2)]
            actT = [kb.sb("actTr%d" % i, [128, 2, 128], F32R, stack=s4) for i in range(2)]
            yt = [kb.sb("yt%d" % i, [128, D], stack=s4) for i in range(2)]
            for blk in range(NBLK):
                i = blk % 2
                kb._need("pool", kb._deps(["bexrow"], ["wgu%d" % i, "wdn%d" % i]))
                e = ncg.value_load(bexrow[0:1, blk:blk + 1], min_val=0, max_val=NE - 1)
                kb.dma("pool", lambda i=i, e=e: ncg.dma_start(out=wgu[i][:, :, 0:256], in_=dram["w_exp_gate"][bass.ds(e, 1), :, :].rearrange("o (k p) n -> p (o k) n", p=128)),
                       reads=["bexrow"], writes=["wgu%d" % i])
                kb.dma("pool", lambda i=i, e=e: ncg.dma_start(out=wgu[i][:, :, 256:512], in_=dram["w_exp_up"][bass.ds(e, 1), :, :].rearrange("o (k p) n -> p (o k) n", p=128)),
                       reads=["bexrow"], writes=["wgu%d" % i])
                kb.dma("pool", lambda i=i, e=e: ncg.dma_start(out=wdn[i][:], in_=dram["w_exp_down"][bass.ds(e, 1), :, :].rearrange("o (k p) n -> p (o k) n", p=128)),
                       reads=["bexrow"], writes=["wdn%d" % i])
                kb.dma("sp", lambda i=i, blk=blk: ncs.dma_start(out=xg[i][:], in_=XGd[blk * 128:(blk + 1) * 128, :]), reads=["XGd"], writes=["xg%d" % i])
                pa, pak = pA[i], "pA%d" % i
                for kc in range(8):
                    kb.op("pe", lambda kc=kc, i=i, pa=pa: ncp.transpose(pa[:, kc * 128:(kc + 1) * 128], xg[i][:, kc * 128:(kc + 1) * 128], ident[:]),
                          reads=["xg%d" % i, "c_ident"], writes=[pak])
                kb.op("act", lambda i=i, pa=pa: nca.copy(out=xgT[i][:, 0:4, :], in_=pa[:, 0:512].rearrange("p (k t) -> p k t", k=4)), reads=[pak], writes=["xgT%d" % i])
                kb.op("dve", lambda i=i, pa=pa: ncv.tensor_copy(out=xgT[i][:, 4:8, :], in_=pa[:, 512:1024].rearrange("p (k t) -> p k t", k=4)), reads=[pak], writes=["xgT%d" % i])
                pg, pgk = pB[i], "pB%d" % i
                for kc in range(8):
                    kb.op("pe", lambda kc=kc, i=i, pg=pg: ncp.matmul(pg[:, :], xgT[i][:, kc, :], wgu[i][:, kc, :], start=(kc == 0), stop=(kc == 7)),
                          reads=["xgT%d" % i, "wgu%d" % i], writes=[pgk])
                kb.op("act", lambda i=i, pg=pg: nca.activation(out=sg[i][:], in_=pg[:, 0:256], func=AF.Silu), reads=[pgk], writes=["sgr%d" % i])
                kb.op("dve", lambda i=i, pg=pg: ncv.tensor_tensor(out=act[i][:], in0=sg[i][:], in1=pg[:, 256:512], op=ALU.mult), reads=[pgk, "sgr%d" % i], writes=["actr%d" % i])
                pt, ptk = pB[2 + i], "pB%d" % (2 + i)
                for j in range(2):
                    kb.op("pe", lambda j=j, i=i, pt=pt: ncp.transpose(pt[:, j * 128:(j + 1) * 128], act[i][:, j * 128:(j + 1) * 128], ident[:]),
                          reads=["actr%d" % i, "c_ident"], writes=[ptk])
                kb.op("act", lambda i=i, pt=pt: nca.copy(out=actT[i][:, :, :], in_=pt[:, 0:256].rearrange("p (k t) -> p k t", k=2)), reads=[ptk], writes=["actTr%d" % i])
                for hf in range(2):
                    for j in range(2):
                        kb.op("pe", lambda j=j, hf=hf, i=i, pa=pa: ncp.matmul(pa[:, hf * 512:(hf + 1) * 512], actT[i][:, j, :], wdn[i][:, j, hf * 512:(hf + 1) * 512],
                                                                             start=(j == 0), stop=(j == 1)),
                              reads=["actTr%d" % i, "wdn%d" % i], writes=[pak])
                kb.op("act", lambda i=i, pa=pa: nca.copy(out=yt[i][:, 0:512], in_=pa[:, 0:512]), reads=[pak], writes=["yt%d" % i])
                kb.op("dve", lambda i=i, pa=pa: ncv.tensor_copy(out=yt[i][:, 512:1024], in_=pa[:, 512:1024]), reads=[pak], writes=["yt%d" % i])
                kb.dma("sp", lambda i=i, blk=blk: ncs.dma_start(out=Yd[blk * 128:(blk + 1) * 128, :], in_=yt[i][:]), reads=["yt%d" % i], writes=["Yd"])
            kb.barrier()
        gsem = [kb.stack.enter_context(nc.semaphore("ix_g%d" % k)) for k in range(8)]
        with contextlib.ExitStack() as s6:
            yk = [kb.sb("yk%d" % i, [128, D], stack=s6) for i in range(8)]
            acc = [kb.sb("accr%d" % i, [128, D], stack=s6) for i in range(2)]
            ot = [kb.sb("otr%d" % i, [128, D], stack=s6) for i in range(2)]
            for i in range(NT):
                j = i % 2
                b = i // (S // 128)
                kb.dma("sp", lambda i=i, j=j: ncs.dma_start(out=ot[j][:], in_=out[i * 128:(i + 1) * 128, :]), reads=["out"], writes=["otr%d" % j])
                for k in range(8):
                    kb._need("pool", kb._deps(["Yd", "DESTi"], ["yk%d" % k]))
                    ncg.indirect_dma_start(
                        out=yk[k][:, :], out_offset=None, in_=Yd[:, :],
                        in_offset=bass.IndirectOffsetOnAxis(ap=DESTi[:, i * 8 + k:i * 8 + k + 1], axis=0), bounds_check=R - 1, oob_is_err=False).then_inc(gsem[k], 16)
                    kb._record((gsem[k], "ix_g%d" % k, 16 * (i + 1), "dma"), ["Yd", "DESTi"], ["yk%d" % k])
                    gcol = GK[:, i * 8 + k:i * 8 + k + 1]
                    if k == 0:
                        kb.op("dve", lambda j=j, k=k, gcol=gcol: ncv.tensor_scalar(out=acc[j][:], in0=yk[k][:], scalar1=gcol, scalar2=None, op0=ALU.mult),
                              reads=["yk%d" % k, "GK"], writes=["accr%d" % j])
                    else:
                        kb.op("dve", lambda j=j, k=k, gcol=gcol: ncv.scalar_tensor_tensor(out=acc[j][:], in0=yk[k][:], scalar=gcol, in1=acc[j][:], op0=ALU.mult, op1=ALU.add),
                              reads=["yk%d" % k, "GK", "accr%d" % j], writes=["accr%d" % j])
                kb.op("dve", lambda j=j, b=b: ncv.tensor_tensor(out=acc[j][:], in0=acc[j][:], in1=gate2_b[:, b, :], op=ALU.mult), reads=["accr%d" % j, "gate2_b"], writes=["accr%d" % j])
                kb.op("dve", lambda j=j: ncv.tensor_tensor(out=ot[j][:], in0=ot[j][:], in1=acc[j][:], op=ALU.add), reads=["accr%d" % j, "otr%d" % j], writes=["otr%d" % j])
                kb.dma("sp", lambda i=i, j=j: ncs.dma_start(out=out[i * 128:(i + 1) * 128, :], in_=ot[j][:]), reads=["otr%d" % j], writes=["out"])
            kb.barrier()


def phase_B(nc, kb, b, dram, cs, pv, pA, pB, HTd, PMd, AOd, out, dbg_t, stage):
    ncv, nca, ncp, ncg, ncs = nc.vector, nc.scalar, nc.tensor, nc.gpsimd, nc.sync
    modT, gs1, lsT, gq, gk = pv["modT"], pv["gs1"], pv["lsT"], pv["gq"], pv["gk"]
    ident = cs["ident"]
    w_in = dram["w_in"]
    GROUPS = ((128, 1), (512, 4), (2048, 16))

    with contextlib.ExitStack() as sq:
        qT = kb.sb("qT", [128, 6, S], BF16, stack=sq)
        kT = kb.sb("kT", [128, 6, 2, S], BF16, stack=sq)
        kb.op("pool", lambda: ncg.memset(kT[:], 0.0), writes=["kT"])
        Vg = [kb.sb("Vg%d" % g, [128, 16, 4, 65], BF16, stack=sq) for g in range(3)]
        for g in range(3):
            kb.op("pool", lambda g=g: ncg.memset(Vg[g][:], 1.0), writes=["Vg%d" % g])
        with contextlib.ExitStack() as s1:
            hT = kb.sb("hT", [128, 8, S], F32R, stack=s1)
            s1b = contextlib.ExitStack()
            xt = [kb.sb("xt%d" % i, [128, D], stack=s1b) for i in range(2)]
            xn = [kb.sb("xn%d" % i, [128, D], stack=s1b) for i in range(2)]
            junk = kb.sb("junk", [128, D], stack=s1b)
            ss = kb.sb("ss", [128, 2], stack=s1b)
            rstd = kb.sb("rstd", [128, 2], stack=s1b)
            for tt in range(16):
                i = tt % 2
                r0 = b * S + tt * 128
                kb.dma("sp", lambda i=i, r0=r0: ncs.dma_start(out=xt[i][:], in_=dram["x"][r0:r0 + 128, :]), writes=["xt%d" % i])
                kb.op("act", lambda i=i: nca.activation(out=junk[:], in_=xt[i][:], func=AF.Square, accum_out=ss[:, i:i + 1]),
                      reads=["xt%d" % i], writes=["junk", "ss%d" % i])
                kb.op("act", lambda i=i: nca.activation(out=rstd[:, i:i + 1], in_=ss[:, i:i + 1], func=AF.Sqrt, scale=1.0 / D, bias=cs["epsc"][:, 0:1]),
                      reads=["ss%d" % i, "c_epsc"], writes=["rstd%d" % i])
                kb.op("dve", lambda i=i: ncv.reciprocal(out=rstd[:, i:i + 1], in_=rstd[:, i:i + 1]),
                      reads=["rstd%d" % i], writes=["rstd%d" % i])
                kb.op("act", lambda i=i: nca.activation(out=xn[i][:], in_=xt[i][:], func=AF.Identity, scale=rstd[:, i:i + 1]),
                      reads=["xt%d" % i, "rstd%d" % i], writes=["xn%d" % i])
                pa = pA[i]
                for kc in range(8):
                    kb.op("pe", lambda kc=kc, i=i, pa=pa: ncp.transpose(pa[:, kc * 128:(kc + 1) * 128], xn[i][:, kc * 128:(kc + 1) * 128], ident[:]),
                          reads=["xn%d" % i, "c_ident"], writes=["pA%d" % i])
                for kc in range(8):
                    dst = hT[:, kc, tt * 128:(tt + 1) * 128]
                    if kc % 2 == 0:
                        kb.op("dve", lambda kc=kc, pa=pa, dst=dst: ncv.tensor_scalar(out=dst, in0=pa[:, kc * 128:(kc + 1) * 128], scalar1=gs1[:, kc, b:b + 1],
                                                                                     scalar2=modT[:, kc, b:b + 1], op0=ALU.mult, op1=ALU.add),
                              reads=["pA%d" % i, "gs1", "modT"], writes=["hT"])
                    else:
                        kb.op("act", lambda kc=kc, pa=pa, dst=dst: nca.activation(out=dst, in_=pa[:, kc * 128:(kc + 1) * 128], func=AF.Identity,
                                                                                  scale=gs1[:, kc, b:b + 1], bias=modT[:, kc, b:b + 1]),
                              reads=["pA%d" % i, "gs1", "modT"], writes=["hT"])
            for kc in range(8):
                kb.dma("pool", lambda kc=kc: ncg.dma_start(out=HTd[b, kc * 128:(kc + 1) * 128, :], in_=hT[:, kc, :]),
                       reads=["hT"], writes=["HTd"])
                if dbg_t:
                    kb.dma("pool", lambda kc=kc: ncg.dma_start(out=dbg_t["hT"][b, kc * 128:(kc + 1) * 128, :], in_=hT[:, kc, :]),
                           reads=["hT"], writes=["dbg_hT"])
            kb.barrier()
            s1b.close()
            if stage >= 2:
                phase_B2a(nc, kb, b, dram, cs, pv, pA, pB, hT, qT, kT, Vg, PMd, dbg_t, stage)
            kb.barrier()
        if stage >= 3:
            phase_B2b(nc, kb, b, cs, pA, pB, qT, kT, Vg, AOd, dbg_t)
        kb.barrier()
    if stage >= 4:
        phase_B2c(nc, kb, b, dram, cs, pv, pA, pB, HTd, PMd, AOd, out)
        kb.barrier()


def phase_B2a(nc, kb, b, dram, cs, pv, pA, pB, hT, qT, kT, Vg, PMd, dbg_t, stage):
    ncv, nca, ncp, ncg, ncs = nc.vector, nc.scalar, nc.tensor, nc.gpsimd, nc.sync
    lsT, gq, gk = pv["lsT"], pv["gq"], pv["gk"]
    w_in = dram["w_in"]
    with contextlib.ExitStack() as s2:
        win = [kb.sb("win%d" % i, [128, 8, 128], F32R, stack=s2) for i in range(2)]
        PADW = 16
        sU = contextlib.ExitStack()
        wgrp = kb.sb("wgrp", [128, 128], F32R, stack=sU)
        ub = kb.sb("ub", [128, S + 2 * PADW], stack=sU)
        a1 = kb.sb("a1", [128, S + 2 * PADW], stack=sU)
        a2 = kb.sb("a2", [128, S + 2 * PADW], stack=sU)
        pooled = kb.sb("pooled", [128, S], F32R, stack=sU)
        pmt = [kb.sb("pmt0", [128, 512], stack=sU)] * 2
        kb.op("pool", lambda: ncg.memset(ub[:], 0.0), writes=["ub"])
        kb.op("pool", lambda: ncg.memset(a1[:], 0.0), writes=["a1"])
        kb.op("pool", lambda: ncg.memset(a2[:], 0.0), writes=["a2"])
        sQ = None

        def open_qk():
            sQ_ = contextlib.ExitStack()
            Ct_ = kb.sb("Ct", [128, S], stack=sQ_)
            St_ = kb.sb("St", [128, S], stack=sQ_)
            with contextlib.ExitStack() as sp_:
                posi = kb.sb("posi", [128, S], I32, stack=sp_)
                kb.dma("sp", lambda: ncs.dma_start(out=posi[:], in_=dram["positions"][b:b + 1, :].rearrange("o d -> (o d)").partition_broadcast(128)), writes=["posi"])
                H = S // 8
                posf = kb.sb("posf", [128, S], stack=sp_)
                kf = kb.sb("kf", [128, H], stack=sp_)
                ki = kb.sb("ki", [128, H], I32, stack=sp_)
                kb.op("dve", lambda: ncv.tensor_copy(out=posf[:], in_=posi[:]), reads=["posi"], writes=["posf"])
                C1 = 6.28125
                C2 = TWO_PI - C1
                for tab0, tk_, off in ((St_, "St", 0.0), (Ct_, "Ct", 0.5 * math.pi)):
                    for hh in range(8):
                        tab = tab0[:, hh * H:(hh + 1) * H]
                        pf = posf[:, hh * H:(hh + 1) * H]
                        kb.op("dve", lambda tab=tab, pf=pf, off=off: ncv.tensor_scalar(out=tab, in0=pf, scalar1=cs["invf"][:, 0:1], scalar2=off, op0=ALU.mult, op1=ALU.add),
                              reads=["posf", "c_invf"], writes=[tk_])
                        kb.op("dve", lambda tab=tab: ncv.tensor_scalar(out=kf[:], in0=tab, scalar1=1.0 / TWO_PI, scalar2=None, op0=ALU.mult),
                              reads=[tk_], writes=["kf"])
                        kb.op("dve", lambda: ncv.tensor_copy(out=ki[:], in_=kf[:]), reads=["kf"], writes=["ki"])
                        kb.op("dve", lambda: ncv.tensor_copy(out=kf[:], in_=ki[:]), reads=["ki"], writes=["kf"])
                        kb.op("dve", lambda tab=tab: ncv.scalar_tensor_tensor(out=tab, in0=kf[:], scalar=-C1, in1=tab, op0=ALU.mult, op1=ALU.add),
                              reads=["kf", tk_], writes=[tk_])
                        kb.op("dve", lambda tab=tab: ncv.scalar_tensor_tensor(out=tab, in0=kf[:], scalar=-C2, in1=tab, op0=ALU.mult, op1=ALU.add),
                              reads=["kf", tk_], writes=[tk_])
                        kb.op("dve", lambda tab=tab: ncv.tensor_scalar(out=kf[:], in0=tab, scalar1=math.pi, scalar2=-TWO_PI, op0=ALU.is_gt, op1=ALU.mult),
                              reads=[tk_], writes=["kf"])
                        kb.op("dve", lambda tab=tab: ncv.tensor_tensor(out=tab, in0=tab, in1=kf[:], op=ALU.add), reads=["kf", tk_], writes=[tk_])
                        kb.op("dve", lambda tab=tab: ncv.tensor_scalar(out=kf[:], in0=tab, scalar1=-math.pi, scalar2=TWO_PI, op0=ALU.is_lt, op1=ALU.mult),
                              reads=[tk_], writes=["kf"])
                        kb.op("dve", lambda tab=tab: ncv.tensor_tensor(out=tab, in0=tab, in1=kf[:], op=ALU.add), reads=["kf", tk_], writes=[tk_])
                        kb.op("act", lambda tab=tab: nca.activation(out=tab, in_=tab, func=AF.Sin), reads=[tk_], writes=[tk_])
                kb.barrier()
            sqt_ = [kb.sb("sqt%d" % i, [128, 512], BF16, stack=sQ_) for i in range(2)]
            qg_ = [kb.sb("qg%d" % i, [128, 512], BF16, stack=sQ_) for i in range(2)]
            rs_ = [kb.sb("rs%d" % i, [128, 512], stack=sQ_) for i in range(2)]
            ta_ = [kb.sb("ta%d" % i, [128, 512], stack=sQ_) for i in range(2)]
            tb_ = [kb.sb("tb%d" % i, [128, 512], stack=sQ_) for i in range(2)]
            return sQ_, Ct_, St_, sqt_, qg_, rs_, ta_, tb_

        pcnt = [0]

        def next_pb():
            p = pcnt[0] % 4
            pcnt[0] += 1
            return pB[p], "pB%d" % p

        def proj_tile(f, wi, n):
            pb, pk = next_pb()
            for kc in range(8):
                kb.op("pe", lambda kc=kc: ncp.matmul(pb[:, :], win[wi][:, kc, :], hT[:, kc, n * 512:(n + 1) * 512], start=(kc == 0), stop=(kc == 7)),
                      reads=["win%d" % wi, "hT"], writes=[pk])
            return pb, pk

        for f in range(16):
            wi = f % 2
            if f == 4 and stage < 2.2:
                break
            if f == 4:
                kb.barrier()
                sU.close()
                sQ, Ct, St, sqt, qg, rs, ta, tb = open_qk()
            kb.dma("pool", lambda f=f, wi=wi: ncg.dma_start(out=win[wi][:], in_=w_in[:, f * 128:(f + 1) * 128].rearrange("(k p) n -> p k n", p=128)),
                   writes=["win%d" % wi])
            if f < 4:
                g = f
                R = (1, 2, 4, 8)[g]
                kb.dma("pool", lambda g=g: ncg.dma_start(out=wgrp[:], in_=dram["pool_w_grp"][g * 128:(g + 1) * 128, :]), writes=["wgrp"])
                for n in range(4):
                    pb, pk = proj_tile(f, wi, n)
                    kb.op("act", lambda n=n, pb=pb: nca.copy(out=ub[:, PADW + n * 512:PADW + (n + 1) * 512], in_=pb[:, :]), reads=[pk], writes=["ub"])
                lo, hi = 0, S + 2 * PADW
                kb.op("dve", lambda: ncv.tensor_tensor(out=a1[:, 0:hi - 1], in0=ub[:, 0:hi - 1], in1=ub[:, 1:hi], op=ALU.add), reads=["ub"], writes=["a1"])
                cur, curk, width = a1, "a1", 2
                other, otherk = a2, "a2"
                while width < R * 2:
                    kb.op("dve", lambda cur=cur, other=other, width=width: ncv.tensor_tensor(
                        out=other[:, 0:hi - 2 * width + 1], in0=cur[:, 0:hi - 2 * width + 1], in1=cur[:, width:hi - width + 1], op=ALU.add),
                        reads=[curk], writes=[otherk])
                    cur, other = other, cur
                    curk, otherk = otherk, curk
                    width *= 2
                kb.op("dve", lambda cur=cur, other=other, R=R: ncv.tensor_tensor(
                    out=other[:, PADW:PADW + S], in0=cur[:, PADW - R:PADW - R + S], in1=ub[:, PADW + R:PADW + R + S], op=ALU.add),
                    reads=[curk, "ub"], writes=[otherk])
                kb.op("dve", lambda other=other, R=R: ncv.scalar_tensor_tensor(
                    out=pooled[:, :], in0=other[:, PADW:PADW + S], scalar=1.0 / (2 * R + 1), in1=ub[:, PADW:PADW + S], op0=ALU.mult, op1=ALU.subtract),
                    reads=[otherk, "ub"], writes=["pooled"])
                kb.op("dve", lambda other=other, R=R, g=g: ncv.tensor_tensor(
                    out=cur[:, PADW:PADW + R], in0=other[:, PADW:PADW + R], in1=cs["pooledge"][:, g, 0:R], op=ALU.mult),
                    reads=[otherk, "c_pooledge"], writes=[curk])
                kb.op("dve", lambda cur=cur, R=R: ncv.tensor_tensor(
                    out=pooled[:, 0:R], in0=cur[:, PADW:PADW + R], in1=ub[:, PADW:PADW + R], op=ALU.subtract),
                    reads=[curk, "ub"], writes=["pooled"])
                for t in range(R):
                    pos = S - 1 - t
                    kb.op("dve", lambda other=other, g=g, t=t, pos=pos: ncv.scalar_tensor_tensor(
                        out=pooled[:, pos:pos + 1], in0=other[:, PADW + pos:PADW + pos + 1], scalar=cs["pooledge"][:, g, 8 + t:9 + t],
                        in1=ub[:, PADW + pos:PADW + pos + 1], op0=ALU.mult, op1=ALU.subtract),
                        reads=[otherk, "ub", "c_pooledge"], writes=["pooled"])
                for n in range(4):
                    pb, pk = next_pb()
                    kb.op("pe", lambda g=g, n=n, pb=pb: ncp.matmul(pb[:, :], wgrp[:, :], pooled[:, n * 512:(n + 1) * 512], start=True, stop=True),
                          reads=["wgrp", "pooled"], writes=[pk])
                    j = 0
                    kb.op("act", lambda g=g, pb=pb, j=j: nca.activation(out=pmt[j][:], in_=pb[:, :], func=AF.Identity, scale=lsT[:, g:g + 1]),
                          reads=[pk, "lsT"], writes=["pmt%d" % j])
                    kb.dma("sp", lambda g=g, n=n, j=j: ncs.dma_start(out=PMd[b, g * 128:(g + 1) * 128, n * 512:(n + 1) * 512], in_=pmt[j][:]),
                           reads=["pmt%d" % j], writes=["PMd"])
                    if dbg_t:
                        kb.dma("sp", lambda g=g, n=n, j=j: ncs.dma_start(out=dbg_t["pm"][b, g * 128:(g + 1) * 128, n * 512:(n + 1) * 512], in_=pmt[j][:]),
                               reads=["pmt%d" % j], writes=["dbg_pm"])
            else:
                isq = f < 10
                tile = (f - 4) if isq else (f - 10)
                g = tile // 2
                r = (1, 4, 16)[g]
                L = S // r
                dstT = qT if isq else kT
                dk = "qT" if isq else "kT"
                gain = gq if isq else gk
                gaink = "gq" if isq else "gk"
                for n in range(4):
                    j = n % 2
                    pb, pk = proj_tile(f, wi, n)
                    kb.op("act", lambda pb=pb, j=j: nca.activation(out=sqt[j][:], in_=pb[:, :], func=AF.Square), reads=[pk], writes=["sqt%d" % j])
                    kb.op("act", lambda pb=pb, j=j, gain=gain: nca.activation(out=qg[j][:], in_=pb[:, :], func=AF.Identity, scale=gain[:, 0:1]),
                          reads=[pk, gaink], writes=["qg%d" % j])
                    ps_, psk = next_pb()
                    kb.op("pe", lambda ps_=ps_, j=j: ncp.matmul(ps_[:, :], cs["blockones"][:], sqt[j][:], start=True, stop=True),
                          reads=["sqt%d" % j, "c_blockones"], writes=[psk])
                    pr_, prk = next_pb()
                    kb.op("pe", lambda pr_=pr_, j=j: ncp.matmul(pr_[:, :], cs["ropeR"][:], qg[j][:], start=True, stop=True),
                          reads=["qg%d" % j, "c_ropeR"], writes=[prk])
                    kb.op("act", lambda ps_=ps_, j=j: nca.activation(out=rs[j][:], in_=ps_[:, :], func=AF.Sqrt, bias=cs["epsc"][:, 1:2]),
                          reads=[psk, "c_epsc"], writes=["rs%d" % j])
                    kb.op("dve", lambda j=j: ncv.reciprocal(out=rs[j][:], in_=rs[j][:]), reads=["rs%d" % j], writes=["rs%d" % j])
                    kb.op("pool", lambda j=j, n=n: ncg.tensor_tensor(out=ta[j][:], in0=qg[j][:], in1=Ct[:, n * 512:(n + 1) * 512], op=ALU.mult),
                          reads=["qg%d" % j, "Ct"], writes=["ta%d" % j])
                    kb.op("dve", lambda pr_=pr_, j=j, n=n: ncv.tensor_tensor(out=tb[j][:], in0=pr_[:, :], in1=St[:, n * 512:(n + 1) * 512], op=ALU.mult),
                          reads=[prk, "St"], writes=["tb%d" % j])
                    kb.op("pool", lambda j=j: ncg.tensor_tensor(out=ta[j][:], in0=ta[j][:], in1=tb[j][:], op=ALU.add),
                          reads=["ta%d" % j, "tb%d" % j], writes=["ta%d" % j])
                    m0 = n * 512 // r
                    mn = 512 // r
                    if not isq:
                        dst = src0 = src1 = None
                    elif r == 1:
                        dst = dstT[:, tile, n * 512:(n + 1) * 512]
                        src0 = ta[j][:, :]
                        src1 = rs[j][:, :]
                    else:
                        dst = dstT[:, tile, :].rearrange("p (rr m) -> p m rr", rr=r)[:, m0:m0 + mn, :]
                        src0 = ta[j][:, :].rearrange("p (m rr) -> p m rr", rr=r)
                        src1 = rs[j][:, :].rearrange("p (m rr) -> p m rr", rr=r)
                    if isq:
                        kb.op("dve", lambda dst=dst, src0=src0, src1=src1: ncv.tensor_tensor(out=dst, in0=src0, in1=src1, op=ALU.mult),
                              reads=["ta%d" % j, "rs%d" % j], writes=[dk])
                    else:
                        for hl in range(2):
                            o = 64 * hl
                            if r == 1:
                                dsth = kT[o:o + 64, tile, hl, n * 512:(n + 1) * 512]
                                s0h, s1h = ta[j][o:o + 64, :], rs[j][o:o + 64, :]
                            else:
                                dsth = kT[o:o + 64, tile, hl, :].rearrange("p (rr m) -> p m rr", rr=r)[:, m0:m0 + mn, :]
                                s0h = ta[j][o:o + 64, :].rearrange("p (m rr) -> p m rr", rr=r)
                                s1h = rs[j][o:o + 64, :].rearrange("p (m rr) -> p m rr", rr=r)
                            kb.op("dve", lambda dsth=dsth, s0h=s0h, s1h=s1h: ncv.tensor_tensor(out=dsth, in0=s0h, in1=s1h, op=ALU.mult),
                                  reads=["ta%d" % j, "rs%d" % j], writes=[dk])
        kb.barrier()
        if sQ is None:
            sU.close()
            return
        sQ.close()
        if stage < 2.3:
            return
        vT = kb.sb("vT", [128, 6, S], BF16, stack=s2)
        for f in range(16, 22):
            wi = f % 2
            tile = f - 16
            g = tile // 2
            r = (1, 4, 16)[g]
            kb.dma("pool", lambda f=f, wi=wi: ncg.dma_start(out=win[wi][:], in_=w_in[:, f * 128:(f + 1) * 128].rearrange("(k p) n -> p k n", p=128)),
                   writes=["win%d" % wi])
            for n in range(4):
                pb, pk = proj_tile(f, wi, n)
                m0 = n * 512 // r
                mn = 512 // r
                if r == 1:
                    dst = vT[:, tile, n * 512:(n + 1) * 512]
                    src = pb[:, :]
                else:
                    dst = vT[:, tile, :].rearrange("p (rr m) -> p m rr", rr=r)[:, m0:m0 + mn, :]
                    src = pb[:, :].rearrange("p (m rr) -> p m rr", rr=r)
                if n % 2 == 0:
                    kb.op("act", lambda dst=dst, src=src: nca.copy(out=dst, in_=src), reads=[pk], writes=["vT"])
                else:
                    kb.op("dve", lambda dst=dst, src=src: ncv.tensor_copy(out=dst, in_=src), reads=[pk], writes=["vT"])
        identb = cs["ident_bf"]
        for g in range(3):
            for ci in range(16):
                pb, pk = next_pb()
                pbb = pb[:, 0:128].bitcast(BF16)
                for hp in range(2):
                    kb.op("pe", lambda g=g, ci=ci, hp=hp, pbb=pbb: ncp.transpose(pbb[:, hp * 128:(hp + 1) * 128], vT[:, 2 * g + hp, ci * 128:(ci + 1) * 128], identb[:]),
                          reads=["vT", "c_ident_bf"], writes=[pk])
                src = pbb[:, :].rearrange("p (h d) -> p h d", d=64)
                if ci % 2 == 0:
                    kb.op("act", lambda g=g, ci=ci, src=src: nca.copy(out=Vg[g][:, ci, :, 0:64], in_=src), reads=[pk], writes=["Vg%d" % g])
                else:
                    kb.op("dve", lambda g=g, ci=ci, src=src: ncv.tensor_copy(out=Vg[g][:, ci, :, 0:64], in_=src), reads=[pk], writes=["Vg%d" % g])


def phase_B2b(nc, kb, b, cs, pA, pB, qT, kT, Vg, AOd, dbg_t):
    ncv, nca, ncp, ncg, ncs = nc.vector, nc.scalar, nc.tensor, nc.gpsimd, nc.sync
    with contextlib.ExitStack() as s3:
        acc = kb.sb("acc", [64, 4, S], stack=s3)
        accd = kb.sb("accd", [64, 4, S], stack=s3)
        kb.op("pool", lambda: ncg.memset(accd[:], 0.0), writes=["accd"])
        PT = [kb.sb("PT%d" % i, [128, 2, 256], BF16, stack=s3) for i in range(3)]
        aot = [kb.sb("aot%d" % i, [64, 512], stack=s3) for i in range(2)]
        kb.op("pool", lambda: ncg.memset(acc[:], 0.0), writes=["acc"])
        it = 0
        for g in range(3):
            r = (1, 4, 16)[g]
            L = S // r
            nch = L // 128
            for rr in range(r):
                for c in range(nch):
                    ci = rr * nch + c
                    j0 = max(0, 128 * c - 64)
                    j1 = min(L, 128 * c + 192)
                    nq = j1 - j0
                    mo = j0 - (128 * c - 64)
                    for hp in range(2):
                        tile = 2 * g + hp
                        pi = it % 4
                        ps_, psk = pB[pi], "pB%d" % pi
                        pt, ptk = PT[it % 3], "PT%d" % (it % 3)
                        po, pok = pA[it % 2], "pA%d" % (it % 2)
                        it += 1
                        for hl in range(2):
                            o = 64 * hl
                            kb.op("pe", lambda o=o, hl=hl, tile=tile, rr=rr, c=c, j0=j0, j1=j1, nq=nq, L=L, ps_=ps_: ncp.matmul(
                                ps_[:, hl * 256:hl * 256 + nq], kT[:, tile, hl, rr * L + 128 * c:rr * L + 128 * c + 128],
                                qT[:, tile, rr * L + j0:rr * L + j1], start=True, stop=True),
                                reads=["qT", "kT"], writes=[psk])
                        kb.op("act", lambda ps_=ps_, pt=pt, nq=nq: nca.activation(
                            out=pt[:, :, 0:nq], in_=ps_[:, :].rearrange("p (h q) -> p h q", h=2)[:, :, 0:nq], func=AF.Exp, scale=0.125),
                            reads=[psk], writes=[ptk])
                        for hl in range(2):
                            kb.op("pool" if hl == 0 else "dve",
                                  lambda hl=hl, pt=pt, nq=nq, mo=mo: (ncg if hl == 0 else ncv).tensor_tensor(
                                      out=pt[:, hl, 0:nq], in0=pt[:, hl, 0:nq], in1=cs["band"][:, mo:mo + nq], op=ALU.mult),
                                  reads=[ptk, "c_band"], writes=[ptk])
                        for hl in range(2):
                            h = 2 * hp + hl
                            kb.op("pe", lambda hl=hl, h=h, g=g, ci=ci, pt=pt, po=po, nq=nq: ncp.matmul(
                                po[0:64, hl * 256:hl * 256 + nq], Vg[g][:, ci, h, 0:64], pt[:, hl, 0:nq], start=True, stop=True),
                                reads=["Vg%d" % g, ptk], writes=[pok])
                            kb.op("pe", lambda hl=hl, pt=pt, po=po, nq=nq: ncp.matmul(
                                po[0:64, 512 + hl * 256:512 + hl * 256 + nq], cs["ones_bf"][:, 0:64], pt[:, hl, 0:nq], start=True, stop=True),
                                reads=["c_ones_bf", ptk], writes=[pok])
                        for hl in range(2):
                            h = 2 * hp + hl
                            ts0 = j0 * r + rr
                            dst = acc[0:64, h, ts0:ts0 + (nq - 1) * r + 1:r]
                            dstd = accd[0:64, h, ts0:ts0 + (nq - 1) * r + 1:r]
                            kb.op("dve", lambda dst=dst, po=po, hl=hl, nq=nq: ncv.tensor_tensor(
                                out=dst, in0=dst, in1=po[0:64, hl * 256:hl * 256 + nq], op=ALU.add),
                                reads=[pok, "acc"], writes=["acc"])
                            kb.op("dve", lambda dstd=dstd, po=po, hl=hl, nq=nq: ncv.tensor_tensor(
                                out=dstd, in0=dstd, in1=po[0:64, 512 + hl * 256:512 + hl * 256 + nq], op=ALU.add),
                                reads=[pok, "accd"], writes=["accd"])
        for h in range(4):
            for n in range(4):
                j = (h * 4 + n) % 2
                kb.op("dve", lambda n=n, h=h, j=j: ncv.reciprocal(out=aot[j][:], in_=accd[0:64, h, n * 512:(n + 1) * 512]), reads=["accd"], writes=["aot%d" % j])
                kb.op("dve", lambda n=n, h=h, j=j: ncv.tensor_tensor(out=aot[j][:], in0=aot[j][:], in1=acc[0:64, h, n * 512:(n + 1) * 512], op=ALU.mult),
                      reads=["aot%d" % j, "acc"], writes=["aot%d" % j])
                kb.dma("sp", lambda h=h, n=n, j=j: ncs.dma_start(out=AOd[b, h * 64:(h + 1) * 64, n * 512:(n + 1) * 512], in_=aot[j][:]),
                       reads=["aot%d" % j], writes=["AOd"])
                if dbg_t:
                    kb.dma("sp", lambda h=h, n=n, j=j: ncs.dma_start(out=dbg_t["ao"][b, h * 64:(h + 1) * 64, n * 512:(n + 1) * 512], in_=aot[j][:]),
                           reads=["aot%d" % j], writes=["dbg_ao"])


def phase_B2c(nc, kb, b, dram, cs, pv, pA, pB, HTd, PMd, AOd, out):
    ncv, nca, ncp, ncg, ncs = nc.vector, nc.scalar, nc.tensor, nc.gpsimd, nc.sync
    w_in = dram["w_in"]
    with contextlib.ExitStack() as s4:
        gate1_b = kb.sb("gate1_b", [128, NB, D], stack=s4)
        kb.dma("sp", lambda: ncs.dma_start(out=gate1_b[:].rearrange("p b d -> p (b d)"), in_=pv["BCd"][3]), reads=["BCd"], writes=["gate1_b"])
        wout = kb.sb("wout", [128, 8, D], F32R, stack=s4)
        wpu = kb.sb("wpu", [128, 4, D], F32R, stack=s4)
        wau = kb.sb("wau", [128, 2, D], F32R, stack=s4)
        hTt = kb.sb("hTt", [128, 8, 512], F32R, stack=s4)
        pmt = kb.sb("pmt_c", [128, 4, 512], F32R, stack=s4)
        aot = kb.sb("aot_c", [128, 2, 512], F32R, stack=s4)
        wgp = [kb.sb("wgp%d" % i, [128, 8, 128], F32R, stack=s4) for i in range(2)]
        wga = [kb.sb("wga%d" % i, [128, 8, 128], F32R, stack=s4) for i in range(2)]
        merged = kb.sb("merged", [128, 8, 512], F32R, stack=s4)
        sgp = kb.sb("sgp", [128, 512], stack=s4)
        sga = kb.sb("sga", [128, 512], stack=s4)
        m1 = kb.sb("m1", [128, 512], stack=s4)
        xt = [kb.sb("xtc%d" % i, [128, D], stack=s4) for i in range(2)]
        x1 = [kb.sb("x1c%d" % i, [128, D], stack=s4) for i in range(2)]
        kb.dma("pool", lambda: ncg.dma_start(out=wout[:], in_=dram["w_out"].rearrange("(k p) n -> p k n", p=128)), writes=["wout"])
        kb.dma("pool", lambda: ncg.dma_start(out=wpu[:], in_=dram["w_pool_up"].rearrange("(k p) n -> p k n", p=128)), writes=["wpu"])
        kb.dma("pool", lambda: ncg.dma_start(out=wau[:], in_=dram["w_attn_up"].rearrange("(k p) n -> p k n", p=128)), writes=["wau"])
        for n in range(4):
            kb.dma("pool", lambda n=n: ncg.dma_start(out=hTt[:], in_=HTd[b, :, n * 512:(n + 1) * 512].rearrange("(k p) t -> p k t", p=128)),
                   reads=["HTd"], writes=["hTt"])
            kb.dma("pool", lambda n=n: ncg.dma_start(out=pmt[:], in_=PMd[b, :, n * 512:(n + 1) * 512].rearrange("(k p) t -> p k t", p=128)),
                   reads=["PMd"], writes=["pmt_c"])
            kb.dma("pool", lambda n=n: ncg.dma_start(out=aot[:], in_=AOd[b, :, n * 512:(n + 1) * 512].rearrange("(k p) t -> p k t", p=128)),
                   reads=["AOd"], writes=["aot_c"])
            for j in range(8):
                wi = j % 2
                kb.dma("pool", lambda j=j, wi=wi: ncg.dma_start(out=wgp[wi][:], in_=w_in[:, 2816 + j * 128:2816 + (j + 1) * 128].rearrange("(k p) n -> p k n", p=128)),
                       writes=["wgp%d" % wi])
                kb.dma("pool", lambda j=j, wi=wi: ncg.dma_start(out=wga[wi][:], in_=w_in[:, 3840 + j * 128:3840 + (j + 1) * 128].rearrange("(k p) n -> p k n", p=128)),
                       writes=["wga%d" % wi])
                for kc in range(8):
                    kb.op("pe", lambda kc=kc, wi=wi: ncp.matmul(pB[0][:, :], wgp[wi][:, kc, :], hTt[:, kc, :], start=(kc == 0), stop=(kc == 7)),
                          reads=["wgp%d" % wi, "hTt"], writes=["pB0"])
                for kc in range(8):
                    kb.op("pe", lambda kc=kc, wi=wi: ncp.matmul(pB[1][:, :], wga[wi][:, kc, :], hTt[:, kc, :], start=(kc == 0), stop=(kc == 7)),
                          reads=["wga%d" % wi, "hTt"], writes=["pB1"])
                for g in range(4):
                    kb.op("pe", lambda g=g, j=j: ncp.matmul(pB[2][:, :], wpu[:, g, j * 128:(j + 1) * 128], pmt[:, g, :], start=(g == 0), stop=(g == 3)),
                          reads=["wpu", "pmt_c"], writes=["pB2"])
                for g in range(2):
                    kb.op("pe", lambda g=g, j=j: ncp.matmul(pB[3][:, :], wau[:, g, j * 128:(j + 1) * 128], aot[:, g, :], start=(g == 0), stop=(g == 1)),
                          reads=["wau", "aot_c"], writes=["pB3"])
                kb.op("act", lambda: nca.activation(out=sgp[:], in_=pB[0][:, :], func=AF.Sigmoid), reads=["pB0"], writes=["sgp"])
                kb.op("act", lambda: nca.activation(out=sga[:], in_=pB[1][:, :], func=AF.Sigmoid), reads=["pB1"], writes=["sga"])
                kb.op("dve", lambda: ncv.tensor_tensor(out=m1[:], in0=sgp[:], in1=pB[2][:, :], op=ALU.mult), reads=["sgp", "pB2"], writes=["m1"])
                kb.op("dve", lambda: ncv.tensor_tensor(out=sga[:], in0=sga[:], in1=pB[3][:, :], op=ALU.mult), reads=["sga", "pB3"], writes=["sga"])
                kb.op("pool", lambda j=j: ncg.tensor_tensor(out=merged[:, j, :], in0=m1[:], in1=sga[:], op=ALU.add), reads=["m1", "sga"], writes=["merged"])
            for s in range(4):
                i = s % 2
                r0 = b * S + n * 512 + s * 128
                kb.dma("sp", lambda i=i, r0=r0: ncs.dma_start(out=xt[i][:], in_=dram["x"][r0:r0 + 128, :]), writes=["xtc%d" % i])
                pa, pak = pA[i], "pA%d" % i
                for hf in range(2):
                    for j in range(8):
                        kb.op("pe", lambda j=j, hf=hf, s=s, pa=pa: ncp.matmul(pa[:, hf * 512:(hf + 1) * 512], merged[:, j, s * 128:(s + 1) * 128],
                                                                             wout[:, j, hf * 512:(hf + 1) * 512], start=(j == 0), stop=(j == 7)),
                              reads=["merged", "wout"], writes=[pak])
                kb.op("dve", lambda i=i, pa=pa: ncv.tensor_tensor(out=x1[i][:], in0=pa[:, :], in1=gate1_b[:, b, :], op=ALU.mult),
                      reads=[pak, "gate1_b"], writes=["x1c%d" % i])
                kb.op("pool", lambda i=i: ncg.tensor_tensor(out=x1[i][:], in0=x1[i][:], in1=xt[i][:], op=ALU.add),
                      reads=["x1c%d" % i, "xtc%d" % i], writes=["x1c%d" % i])
                kb.dma("sp", lambda i=i, r0=r0: ncs.dma_start(out=out[r0:r0 + 128, :], in_=x1[i][:]), reads=["x1c%d" % i], writes=["out"])


_NC_CACHE = {}


def _get_nc(stage=99, dbg=False):
    key = (stage, dbg)
    if key not in _NC_CACHE:
        _NC_CACHE[key] = build_nc(stage, dbg)
    return _NC_CACHE[key]


def _in_maps(inputs, cores, stage=99):
    consts = _consts()
    maps = []
    w = {}
    for name, shape in W_SPECS:
        if stage < 6 and name.startswith("w_exp"):
            continue
        w[name] = np.ascontiguousarray(np.asarray(inputs[name], dtype=np.float32).reshape(shape))
    x = np.asarray(inputs["x"], dtype=np.float32)
    c = np.asarray(inputs["c"], dtype=np.float32)
    pos = np.asarray(inputs["positions"], dtype=np.int32)
    for i in cores:
        m = {"x": np.ascontiguousarray(x[NB * i:NB * (i + 1)].reshape(T, D)),
             "c": np.ascontiguousarray(c[NB * i:NB * (i + 1)]),
             "positions": np.ascontiguousarray(pos[NB * i:NB * (i + 1)])}
        m.update(w)
        for k, v in consts.items():
            m["k_" + k] = v
        maps.append(m)
    return maps


def kernel(**inputs):
    nc = _get_nc(stage=6)
    maps = _in_maps(inputs, list(range(NCORES)), stage=6)
    res = run_bass_kernel_spmd(nc, maps, core_ids=list(range(NCORES)))
    outs = [np.asarray(r["out"]).reshape(NB, S, D) for r in res.results]
    return np.concatenate(outs, axis=0).astype(np.float32)
```

```python
import contextlib
import math
import numpy as np
import ml_dtypes
import concourse.bass as bass
import concourse.mybir as mybir
from concourse.bass_utils import run_bass_kernel_spmd

F32 = mybir.dt.float32
F32R = mybir.dt.float32r
BF16 = mybir.dt.bfloat16
I32 = mybir.dt.int32
AF = mybir.ActivationFunctionType
ALU = mybir.AluOpType
AX = mybir.AxisListType

D = 1024
S = 2048
NB = 2
T = NB * S
NCORES = 8
EPS = 1e-6
IN_WIDTH = 4864
NE = 256
NBLK = T * 8 // 128 + NE
TWO_PI = 2.0 * math.pi
DENSE_TB = 1024


class KB:
    N_DMA_SEMS = 48

    def __init__(self, nc, stack):
        self.nc = nc
        self.stack = stack
        self.eng = dict(pe=nc.tensor, act=nc.scalar, dve=nc.vector, pool=nc.gpsimd, sp=nc.sync)
        self.csem = {}
        self.ccnt = {}
        for e in ("pe", "act", "dve", "pool"):
            self.csem[e] = stack.enter_context(nc.semaphore("c_" + e))
            self.ccnt[e] = 0
        self.dsem = [stack.enter_context(nc.semaphore("d_%d" % i)) for i in range(self.N_DMA_SEMS)]
        self.dcnt = [0] * self.N_DMA_SEMS
        self.drr = 0
        self.drr_sw = 0
        self.waited = {e: {} for e in self.eng}
        self.res = {}
        self.n_inst = 0

    def sb(self, name, shape, dt=F32, stack=None):
        self.n_inst += 1
        return (stack or self.stack).enter_context(self.nc.sbuf_tensor("%s_%d" % (name, self.n_inst), list(shape), dt))

    def ps(self, name, shape, dt=F32, stack=None):
        return (stack or self.stack).enter_context(self.nc.psum_tensor(name, list(shape), dt))

    def _st(self, key):
        s = self.res.get(key)
        if s is None:
            s = {"w": None, "r": []}
            self.res[key] = s
        return s

    def _need(self, engine, deps):
        e = self.eng[engine]
        for (sem, name, val, src) in deps:
            if src == engine and engine == "pe":
                continue
            if engine == "pool" and name.startswith("ix_"):
                continue
            if self.waited[engine].get(name, 0) >= val:
                continue
            e.wait_ge(sem, val)
            self.waited[engine][name] = val

    def _deps(self, reads, writes):
        deps = []
        for k in reads:
            s = self._st(k)
            if s["w"] is not None:
                deps.append(s["w"])
        for k in writes:
            s = self._st(k)
            if s["w"] is not None:
                deps.append(s["w"])
            deps.extend(s["r"])
        return deps

    def _record(self, tok, reads, writes):
        for k in reads:
            s = self._st(k)
            s["r"] = [r for r in s["r"] if r[1] != tok[1]] + [tok]
        for k in writes:
            s = self._st(k)
            s["w"] = tok
            s["r"] = []

    def op(self, engine, fn, reads=(), writes=(), inc=True):
        self._need(engine, self._deps(reads, writes))
        inst = fn()
        if inc:
            self.ccnt[engine] += 1
            inst.then_inc(self.csem[engine], 1)
            tok = (self.csem[engine], "c_" + engine, self.ccnt[engine], engine)
        else:
            tok = (self.csem[engine], "c_" + engine, self.ccnt[engine] + 1, engine)
        self._record(tok, reads, writes)
        self.n_inst += 1
        return inst

    def dma(self, queue, fn, reads=(), writes=()):
        half = self.N_DMA_SEMS // 2
        if queue == "pool":
            i = half + self.drr_sw
            self.drr_sw = (self.drr_sw + 1) % half
        else:
            i = self.drr
            self.drr = (self.drr + 1) % half
        deps = self._deps(reads, writes)
        if self.dcnt[i] > 0:
            deps.append((self.dsem[i], "d_%d" % i, self.dcnt[i], "dma"))
        self._need(queue, deps)
        inst = fn()
        self.dcnt[i] += 16
        inst.then_inc(self.dsem[i], 16)
        tok = (self.dsem[i], "d_%d" % i, self.dcnt[i], "dma")
        self._record(tok, reads, writes)
        self.n_inst += 1
        return inst

    def _all(self):
        deps = []
        for i in range(self.N_DMA_SEMS):
            if self.dcnt[i] > 0:
                deps.append((self.dsem[i], "d_%d" % i, self.dcnt[i], "dma"))
        for e in ("pe", "act", "dve", "pool"):
            if self.ccnt[e] > 0:
                deps.append((self.csem[e], "c_" + e, self.ccnt[e], "x"))
        return deps

    def drain(self, engine="sp"):
        self._need(engine, self._all())

    def barrier(self):
        deps = self._all()
        for e in ("sp", "act", "dve", "pool", "pe"):
            self._need(e, [d for d in deps])
        self.res = {}


def _consts():
    c = {}
    c["ident"] = np.eye(128, dtype=np.float32)
    c["ident_bf"] = np.eye(128, dtype=np.float32).astype(ml_dtypes.bfloat16)
    bo = np.zeros((128, 128), np.float32)
    bo[:64, :64] = 1.0
    bo[64:, 64:] = 1.0
    c["blockones"] = bo.astype(ml_dtypes.bfloat16)
    rr = np.zeros((128, 128), np.float32)
    for o in (0, 64):
        for i in range(8):
            rr[o + i + 8, o + i] = -1.0
            rr[o + i, o + i + 8] = 1.0
    c["ropeR"] = rr.astype(ml_dtypes.bfloat16)
    invf = np.zeros((128, 1), np.float32)
    half = 8
    inv_freq = (500000.0 ** (-np.arange(half, dtype=np.float32) / half)).astype(np.float32)
    for p in range(128):
        d = p % 64
        if d < 16:
            invf[p, 0] = inv_freq[d % 8]
    c["invf"] = invf
    kk = np.arange(128)[:, None]
    jj = np.arange(256)[None, :]
    c["band"] = ((jj >= kk) & (jj <= kk + 128)).astype(np.float32).astype(ml_dtypes.bfloat16)
    pe = np.ones((128, 4, 16), np.float32)
    for g, R in enumerate((1, 2, 4, 8)):
        for t in range(R):
            pe[:, g, t] = 1.0 / (t + R + 1)
            pe[:, g, 8 + t] = 1.0 / (R + 1 + t)
    c["pooledge"] = pe
    tri = (np.arange(128)[:, None] < np.arange(128)[None, :]).astype(np.float32)
    c["tri"] = tri.astype(ml_dtypes.bfloat16)
    c["ones_bf"] = np.ones((128, 128), ml_dtypes.bfloat16)
    c["ones_f"] = np.ones((128, 128), np.float32)
    ec = np.zeros((128, 2), np.float32)
    ec[:, 0] = EPS
    ec[:, 1] = 64.0 * EPS
    c["epsc"] = ec
    thr = np.zeros((128, 4), np.float32)
    for j in range(4):
        thr[:, j] = 128.0 * (128 * j + np.arange(128))
    c["thr4"] = thr
    return c


CONST_SPECS = [("ident", [128, 128], F32), ("ident_bf", [128, 128], BF16), ("blockones", [128, 128], BF16), ("ropeR", [128, 128], BF16),
               ("invf", [128, 1], F32), ("band", [128, 256], BF16), ("pooledge", [128, 4, 16], F32),
               ("tri", [128, 128], BF16), ("ones_bf", [128, 128], BF16), ("ones_f", [128, 128], F32), ("epsc", [128, 2], F32), ("thr4", [128, 4], F32)]

W_SPECS = [("w_ada", [D, 6 * D]), ("b_ada", [1, 6 * D]), ("norm1_g", [1, D]), ("w_in", [D, IN_WIDTH]),
           ("pool_w_grp", [512, 128]), ("pool_scale", [1, 512]), ("q_norm_g", [1, 64]), ("k_norm_g", [1, 64]),
           ("w_pool_up", [512, D]), ("w_attn_up", [256, D]), ("w_out", [D, D]), ("norm2_g", [1, D]),
           ("w_router", [D, NE]), ("router_bias", [1, NE]), ("w_shared_gate", [D, 256]), ("w_shared_up", [D, 256]),
           ("w_shared_down", [256, D]), ("w_exp_gate", [NE, D, 256]), ("w_exp_up", [NE, D, 256]),
           ("w_exp_down", [NE, 256, D])]


def build_nc(stage=99, dbg=False):
    nc = bass.Bass("TRN2", target_bir_lowering=False)
    dram = {}
    dram["x"] = nc.dram_tensor("x", [T, D], F32, kind="ExternalInput").ap()
    dram["c"] = nc.dram_tensor("c", [NB, D], F32, kind="ExternalInput").ap()
    dram["positions"] = nc.dram_tensor("positions", [NB, S], I32, kind="ExternalInput").ap()
    for name, shape in W_SPECS:
        if stage < 6 and name.startswith("w_exp"):
            continue
        dram[name] = nc.dram_tensor(name, shape, F32, kind="ExternalInput").ap()
    for name, shape, dt in CONST_SPECS:
        dram[name] = nc.dram_tensor("k_" + name, shape, dt, kind="ExternalInput").ap()
    out = nc.dram_tensor("out", [T, D], F32, kind="ExternalOutput").ap()
    HTd = nc.dram_tensor("HTd", [NB, D, S], F32, kind="Internal").ap()
    PMd = nc.dram_tensor("PMd", [NB, 512, S], F32, kind="Internal").ap()
    AOd = nc.dram_tensor("AOd", [NB, 256, S], F32, kind="Internal").ap()
    BCd = nc.dram_tensor("BCd", [4, 128, NB * D], F32, kind="Internal").ap()
    H2d = nc.dram_tensor("H2d", [T, D], F32, kind="Internal").ap()
    Gd = nc.dram_tensor("Gd", [128, T // 128, NE], F32, kind="Internal").ap()
    XGd = Yd = BEXd = None
    if stage >= 7:
        XGd = nc.dram_tensor("XGd", [NBLK * 128, D], F32, kind="Internal").ap()
        Yd = nc.dram_tensor("Yd", [NBLK * 128, D], F32, kind="Internal").ap()
        BEXd = nc.dram_tensor("BEXd", [NBLK], I32, kind="Internal").ap()
    dbg_t = {}
    if dbg:
        dbg_t["hT"] = nc.dram_tensor("dbg_hT", [NB, D, S], F32, kind="ExternalOutput").ap()
        dbg_t["pm"] = nc.dram_tensor("dbg_pm", [NB, 512, S], F32, kind="ExternalOutput").ap()
        dbg_t["ao"] = nc.dram_tensor("dbg_ao", [NB, 256, S], F32, kind="ExternalOutput").ap()
        dbg_t["mod"] = nc.dram_tensor("dbg_mod", [128, 96], F32, kind="ExternalOutput").ap()
        dbg_t["G"] = nc.dram_tensor("dbg_G", [128, T // 128, NE], F32, kind="ExternalOutput").ap()
        if stage >= 7:
            dbg_t["dest"] = nc.dram_tensor("dbg_dest", [128, T // 128 * 8], I32, kind="ExternalOutput").ap()
            dbg_t["gk"] = nc.dram_tensor("dbg_gk", [128, T // 128 * 8], F32, kind="ExternalOutput").ap()
            dbg_t["bex"] = nc.dram_tensor("dbg_bex", [1, NBLK], I32, kind="ExternalOutput").ap()

    with contextlib.ExitStack() as st:
        kb = KB(nc, st)
        ncv, nca, ncp, ncg, ncs = nc.vector, nc.scalar, nc.tensor, nc.gpsimd, nc.sync

        cs = {}
        for name, shape, dt in CONST_SPECS:
            cs[name] = kb.sb("c_" + name, shape, dt)
            kb.dma("sp", lambda n=name: ncs.dma_start(out=cs[n][:], in_=dram[n]), writes=["c_" + name])
        ident = cs["ident"]

        modT = kb.sb("modT", [128, 48, NB])
        gs1 = kb.sb("gs1", [128, 8, NB])
        gs2 = kb.sb("gs2", [128, 8, NB])
        g1T = kb.sb("g1T", [128, 8])
        g2T = kb.sb("g2T", [128, 8])
        lsT = kb.sb("lsT", [128, 4])
        gq = kb.sb("gq", [128, 1])
        gk = kb.sb("gk", [128, 1])

        pA = [kb.ps("pA%d" % i, [128, 1024]) for i in range(2)]
        pB = [kb.ps("pB%d" % i, [128, 512]) for i in range(4)]

        with contextlib.ExitStack() as sa, nc.allow_non_contiguous_dma(reason="tiny transposed vector loads"):
            gate1_b = kb.sb("gate1_b", [128, NB, D], stack=sa)
            gate2_b = kb.sb("gate2_b", [128, NB, D], stack=sa)
            gs2_b = kb.sb("gs2_b", [128, NB, D], stack=sa)
            sh2_b = kb.sb("sh2_b", [128, NB, D], stack=sa)
            cact = kb.sb("cact", [128, 8, NB], stack=sa)
            crep = kb.sb("crep", [128, 8, NB, 128], stack=sa)
            badaT = kb.sb("badaT", [128, 48], stack=sa)
            bada_row = kb.sb("bada_row", [1, 6 * D], stack=sa)
            g2row_b = kb.sb("g2row_b", [128, D], stack=sa)
            wa = [kb.sb("wa%d" % i, [128, 8, 512], stack=sa) for i in range(2)]
            gtmp = kb.sb("gtmp", [128, 64], stack=sa)
            for b_ in range(NB):
                kb.dma("sp", lambda b_=b_: ncs.dma_start(out=cact[:, :, b_], in_=dram["c"][b_, :].rearrange("(k p) -> p k", p=128)), writes=["cact"])
            kb.dma("sp", lambda: ncs.dma_start(out=badaT[:], in_=dram["b_ada"].rearrange("o (j p) -> p (o j)", p=128)), writes=["badaT"])
            kb.dma("sp", lambda: ncs.dma_start(out=bada_row[:], in_=dram["b_ada"]), writes=["bada_row"])
            kb.dma("sp", lambda: ncs.dma_start(out=g1T[:], in_=dram["norm1_g"].rearrange("o (k p) -> p (o k)", p=128)), writes=["g1T"])
            kb.dma("sp", lambda: ncs.dma_start(out=g2T[:], in_=dram["norm2_g"].rearrange("o (k p) -> p (o k)", p=128)), writes=["g2T"])
            kb.dma("sp", lambda: ncs.dma_start(out=lsT[:], in_=dram["pool_scale"].rearrange("o (g p) -> p (o g)", p=128)), writes=["lsT"])
            kb.dma("sp", lambda: ncs.dma_start(out=g2row_b[:], in_=dram["norm2_g"].rearrange("o d -> (o d)").partition_broadcast(128)), writes=["g2row_b"])
            for h2_, (gt, nm) in enumerate(((gq, "q_norm_g"), (gk, "k_norm_g"))):
                for o in (0, 64):
                    kb.dma("sp", lambda gt=gt, nm=nm, o=o: ncs.dma_start(out=gt[o:o + 64, :], in_=dram[nm].rearrange("o d -> d o")),
                           writes=["gq" if gt is gq else "gk"])
            kb.op("dve", lambda: ncv.tensor_scalar(out=gq[:], in0=gq[:], scalar1=8.0, scalar2=None, op0=ALU.mult), reads=["gq"], writes=["gq"])
            kb.op("dve", lambda: ncv.tensor_scalar(out=gk[:], in0=gk[:], scalar1=8.0, scalar2=None, op0=ALU.mult), reads=["gk"], writes=["gk"])
            kb.op("act", lambda: nca.activation(out=cact[:], in_=cact[:], func=AF.Silu), reads=["cact"], writes=["cact"])
            for kc in range(8):
                for b in range(NB):
                    kb.op("dve", lambda kc=kc, b=b: ncv.tensor_copy(out=crep[:, kc, b, :], in_=cact[:, kc, b:b + 1].to_broadcast([128, 128])),
                          reads=["cact"], writes=["crep"])
            pm = pB[0]
            for t in range(12):
                w = wa[t % 2]
                wk = "wa%d" % (t % 2)
                kb.dma("sp" if t % 2 == 0 else "act",
                       lambda t=t, w=w: (ncs if t % 2 == 0 else nca).dma_start(
                           out=w[:], in_=dram["w_ada"][:, t * 512:(t + 1) * 512].rearrange("(k p) n -> p k n", p=128)),
                       writes=[wk])
                for jj in range(4):
                    j = 4 * t + jj
                    for kc in range(8):
                        kb.op("pe", lambda j=j, jj=jj, kc=kc, w=w: ncp.matmul(pm[:, 2 * j:2 * j + 2], w[:, kc, jj * 128:(jj + 1) * 128], cact[:, kc, :],
                                                                            start=(kc == 0), stop=(kc == 7)),
                              reads=[wk, "cact"], writes=["pB0"])
                if t in (4, 5, 10, 11):
                    dst = gate1_b if t in (4, 5) else gate2_b
                    dk = "gate1_b" if t in (4, 5) else "gate2_b"
                    half = t % 2 if t in (4, 5) else (t - 10)
                    for b in range(NB):
                        pg = pB[1 + b]
                        for kc in range(8):
                            kb.op("pe", lambda kc=kc, b=b, w=w, pg=pg: ncp.matmul(pg[:, :], crep[:, kc, b, :], w[:, kc, :], start=(kc == 0), stop=False),
                                  reads=[wk, "crep"], writes=["pB%d" % (1 + b)])
                        kb.op("pe", lambda t=t, pg=pg: ncp.matmul(pg[:, :], cs["ones_f"][0:1, :], bada_row[0:1, t * 512:(t + 1) * 512], start=False, stop=True),
                              reads=["bada_row", "c_ones_f"], writes=["pB%d" % (1 + b)])
                        kb.op("act", lambda b=b, pg=pg, dst=dst, half=half: nca.copy(out=dst[:, b, half * 512:(half + 1) * 512], in_=pg[:, :]),
                              reads=["pB%d" % (1 + b)], writes=[dk])
                if t in (6, 7, 8, 9):
                    dst = sh2_b if t in (6, 7) else gs2_b
                    dk = "sh2_b" if t in (6, 7) else "gs2_b"
                    half = t % 2
                    for b in range(NB):
                        pg = pB[1 + b]
                        for kc in range(8):
                            kb.op("pe", lambda kc=kc, b=b, w=w, pg=pg: ncp.matmul(pg[:, :], crep[:, kc, b, :], w[:, kc, :], start=(kc == 0), stop=False),
                                  reads=[wk, "crep"], writes=["pB%d" % (1 + b)])
                        kb.op("pe", lambda t=t, pg=pg: ncp.matmul(pg[:, :], cs["ones_f"][0:1, :], bada_row[0:1, t * 512:(t + 1) * 512], start=False, stop=True),
                              reads=["bada_row", "c_ones_f"], writes=["pB%d" % (1 + b)])
                        if t in (6, 7):
                            kb.op("act", lambda b=b, pg=pg, dst=dst, half=half: nca.copy(out=dst[:, b, half * 512:(half + 1) * 512], in_=pg[:, :]),
                                  reads=["pB%d" % (1 + b)], writes=[dk])
                        else:
                            kb.op("dve", lambda b=b, pg=pg, half=half: ncv.scalar_tensor_tensor(
                                out=gs2_b[:, b, half * 512:(half + 1) * 512], in0=pg[:, :], scalar=1.0,
                                in1=g2row_b[:, half * 512:(half + 1) * 512], op0=ALU.add, op1=ALU.mult),
                                reads=["pB%d" % (1 + b), "g2row_b"], writes=[dk])
            for b in range(NB):
                kb.op("dve", lambda b=b: ncv.tensor_tensor(out=modT[:, :, b], in0=pm[:, b:96:2], in1=badaT[:, :], op=ALU.add),
                      reads=["pB0", "badaT"], writes=["modT"])
                kb.op("dve", lambda b=b: ncv.scalar_tensor_tensor(out=gs1[:, :, b], in0=modT[:, 8:16, b], scalar=1.0, in1=g1T[:, :], op0=ALU.add, op1=ALU.mult),
                      reads=["modT", "g1T"], writes=["gs1"])
                kb.op("dve", lambda b=b: ncv.scalar_tensor_tensor(out=gs2[:, :, b], in0=modT[:, 32:40, b], scalar=1.0, in1=g2T[:, :], op0=ALU.add, op1=ALU.mult),
                      reads=["modT", "g2T"], writes=["gs2"])
            for i_, (t_, k_) in enumerate(((gate2_b, "gate2_b"), (gs2_b, "gs2_b"), (sh2_b, "sh2_b"), (gate1_b, "gate1_b"))):
                kb.dma("sp", lambda i_=i_, t_=t_: ncs.dma_start(out=BCd[i_], in_=t_[:].rearrange("p b d -> p (b d)")), reads=[k_], writes=["BCd"])
            if dbg:
                kb.dma("sp", lambda: ncs.dma_start(out=dbg_t["mod"], in_=modT[:].rearrange("p j b -> p (j b)")), reads=["modT"], writes=["dbg_mod"])
            kb.barrier()

        if stage >= 1:
            for b in range(NB):
                phase_B(nc, kb, b, dram, cs, dict(modT=modT, gs1=gs1, BCd=BCd, lsT=lsT, gq=gq, gk=gk),
                        pA, pB, HTd, PMd, AOd, out, dbg_t, stage)
        if stage >= 5:
            phase_C(nc, kb, dram, cs, dict(BCd=BCd, H2d=H2d, XGd=XGd, Yd=Yd, BEXd=BEXd, Gd=Gd), pA, pB, out, dbg_t, stage)
        kb.drain("sp")
    return nc


def phase_C(nc, kb, dram, cs, pv, pA, pB, out, dbg_t, stage):
    ncv, nca, ncp, ncg, ncs = nc.vector, nc.scalar, nc.tensor, nc.gpsimd, nc.sync
    ident = cs["ident"]
    BCd = pv["BCd"]
    with contextlib.ExitStack() as s5:
        s5a = contextlib.ExitStack()
        gate2_b = kb.sb("gate2_bc", [128, NB, D], stack=s5)
        NT = T // 128
        dense = stage < 7
        Gall = kb.sb("Gall", [128, NT, NE], stack=(s5a if dense else s5))
        Mall = None if dense else kb.sb("Mall", [128, NT, NE], BF16, stack=s5)
        gs2_b = kb.sb("gs2_bc", [128, NB, D], stack=s5a)
        sh2_b = kb.sb("sh2_bc", [128, NB, D], stack=s5a)
        for i_, (t_, k_) in enumerate(((gate2_b, "gate2_b"), (gs2_b, "gs2_b"), (sh2_b, "sh2_b"))):
            kb.dma("sp", lambda i_=i_, t_=t_: ncs.dma_start(out=t_[:].rearrange("p b d -> p (b d)"), in_=BCd[i_]), reads=["BCd"], writes=[k_])
        wsgu = kb.sb("wsgu", [128, 8, 512], F32R, stack=s5a)
        wsd = kb.sb("wsd", [128, 2, D], F32R, stack=s5a)
        kb.dma("pool", lambda: ncg.dma_start(out=wsgu[:, :, 0:256], in_=dram["w_shared_gate"].rearrange("(k p) n -> p k n", p=128)), writes=["wsgu"])
        kb.dma("pool", lambda: ncg.dma_start(out=wsgu[:, :, 256:512], in_=dram["w_shared_up"].rearrange("(k p) n -> p k n", p=128)), writes=["wsgu"])
        kb.dma("pool", lambda: ncg.dma_start(out=wsd[:], in_=dram["w_shared_down"].rearrange("(k p) n -> p k n", p=128)), writes=["wsd"])
        NT = T // 128
        H2d = pv["H2d"]
        wr = kb.sb("wr", [128, 8, NE], F32R, stack=s5a)
        kb.dma("pool", lambda: ncg.dma_start(out=wr[:], in_=dram["w_router"].rearrange("(k p) n -> p k n", p=128)), writes=["wr"])
        rbias = kb.sb("rbias", [128, NE], stack=s5a)
        kb.dma("sp", lambda: ncs.dma_start(out=rbias[:], in_=dram["router_bias"].rearrange("o d -> (o d)").partition_broadcast(128)), writes=["rbias"])
        sc = kb.sb("sc", [128, NE], stack=s5a)
        sel = kb.sb("sel", [128, NE], stack=s5a)
        msk = kb.sb("msk", [128, NE], stack=s5a)
        selm = kb.sb("selm", [128, NE], stack=s5a)
        wtmp = kb.sb("wtmp", [128, NE], stack=s5a)
        m8g = kb.sb("m8g", [128, 8, 8], stack=s5a)
        gsc = kb.sb("gsc", [128, 8], stack=s5a)
        m8 = kb.sb("m8", [128, 8], stack=s5a)
        gm = kb.sb("gm", [128, 8], stack=s5a)
        pen = kb.sb("pen", [128, 8], stack=s5a)
        wsum = kb.sb("wsum", [128, 1], stack=s5a)
        x1 = [kb.sb("x1t%d" % i, [128, D], stack=s5a) for i in range(2)]
        xn = [kb.sb("xn2%d" % i, [128, D], stack=s5a) for i in range(2)]
        h2 = [kb.sb("h2t%d" % i, [128, D], stack=s5a) for i in range(2)]
        h2T = [kb.sb("h2T%d" % i, [128, 8, 128], F32R, stack=s5a) for i in range(2)]
        junk = kb.sb("junk2", [128, D], stack=s5a)
        ss = kb.sb("ss2", [128, 2], stack=s5a)
        rstd = kb.sb("rstd2", [128, 2], stack=s5a)
        sg = [kb.sb("sg%d" % i, [128, 256], stack=s5a) for i in range(2)]
        act = [kb.sb("actt%d" % i, [128, 256], stack=s5a) for i in range(2)]
        actT = [kb.sb("actT%d" % i, [128, 2, 128], F32R, stack=s5a) for i in range(2)]
        ot = [kb.sb("ot%d" % i, [128, D], stack=s5a) for i in range(2)]
        for tt in range(T // 128):
            i = tt % 2
            b = tt // (S // 128)
            r0 = tt * 128
            kb.dma("sp", lambda i=i, r0=r0: ncs.dma_start(out=x1[i][:], in_=out[r0:r0 + 128, :]), reads=["out"], writes=["x1t%d" % i])
            kb.op("act", lambda i=i: nca.activation(out=junk[:], in_=x1[i][:], func=AF.Square, accum_out=ss[:, i:i + 1]),
                  reads=["x1t%d" % i], writes=["junk2", "ss2%d" % i])
            kb.op("act", lambda i=i: nca.activation(out=rstd[:, i:i + 1], in_=ss[:, i:i + 1], func=AF.Sqrt, scale=1.0 / D, bias=cs["epsc"][:, 0:1]),
                  reads=["ss2%d" % i, "c_epsc"], writes=["rstd2%d" % i])
            kb.op("dve", lambda i=i: ncv.reciprocal(out=rstd[:, i:i + 1], in_=rstd[:, i:i + 1]), reads=["rstd2%d" % i], writes=["rstd2%d" % i])
            kb.op("act", lambda i=i: nca.activation(out=xn[i][:], in_=x1[i][:], func=AF.Identity, scale=rstd[:, i:i + 1]),
                  reads=["x1t%d" % i, "rstd2%d" % i], writes=["xn2%d" % i])
            kb.op("dve", lambda i=i, b=b: ncv.tensor_tensor(out=h2[i][:], in0=xn[i][:], in1=gs2_b[:, b, :], op=ALU.mult),
                  reads=["xn2%d" % i, "gs2_b"], writes=["h2t%d" % i])
            kb.op("pool", lambda i=i, b=b: ncg.tensor_tensor(out=h2[i][:], in0=h2[i][:], in1=sh2_b[:, b, :], op=ALU.add),
                  reads=["h2t%d" % i, "sh2_b"], writes=["h2t%d" % i])
            pa, pak = pA[i], "pA%d" % i
            for kc in range(8):
                kb.op("pe", lambda kc=kc, i=i, pa=pa: ncp.transpose(pa[:, kc * 128:(kc + 1) * 128], h2[i][:, kc * 128:(kc + 1) * 128], ident[:]),
                      reads=["h2t%d" % i, "c_ident"], writes=[pak])
            kb.op("act", lambda i=i, pa=pa: nca.copy(out=h2T[i][:, 0:4, :], in_=pa[:, 0:512].rearrange("p (k t) -> p k t", k=4)), reads=[pak], writes=["h2T%d" % i])
            kb.op("dve", lambda i=i, pa=pa: ncv.tensor_copy(out=h2T[i][:, 4:8, :], in_=pa[:, 512:1024].rearrange("p (k t) -> p k t", k=4)), reads=[pak], writes=["h2T%d" % i])
            kb.dma("sp", lambda i=i, r0=r0: ncs.dma_start(out=H2d[r0:r0 + 128, :], in_=h2[i][:]), reads=["h2t%d" % i], writes=["H2d"])
            pr, prk = pB[2 + i], "pB%d" % (2 + i)
            for kc in range(8):
                kb.op("pe", lambda kc=kc, i=i, pr=pr: ncp.matmul(pr[:, 0:NE], h2T[i][:, kc, :], wr[:, kc, :], start=(kc == 0), stop=(kc == 7)),
                      reads=["h2T%d" % i, "wr"], writes=[prk])
            kb.op("act", lambda pr=pr: nca.activation(out=sc[:], in_=pr[:, 0:NE], func=AF.Sigmoid), reads=[prk], writes=["sc"])
            kb.op("dve", lambda: ncv.tensor_tensor(out=sel[:], in0=sc[:], in1=rbias[:], op=ALU.add), reads=["sc", "rbias"], writes=["sel"])
            for g in range(8):
                kb.op("dve", lambda g=g: ncv.max(out=m8g[:, g, :], in_=sel[:, g * 32:(g + 1) * 32]), reads=["sel"], writes=["m8g"])
            kb.op("dve", lambda: ncv.tensor_tensor(out=gsc[:], in0=m8g[:, :, 0], in1=m8g[:, :, 1], op=ALU.add), reads=["m8g"], writes=["gsc"])
            kb.op("dve", lambda: ncv.max(out=m8[:], in_=gsc[:]), reads=["gsc"], writes=["m8"])
            kb.op("dve", lambda: ncv.tensor_scalar(out=gm[:], in0=gsc[:], scalar1=m8[:, 3:4], scalar2=None, op0=ALU.is_ge), reads=["gsc", "m8"], writes=["gm"])
            kb.op("dve", lambda: ncv.tensor_scalar(out=pen[:], in0=gm[:], scalar1=-1.0, scalar2=1.0e4, op0=ALU.add, op1=ALU.mult), reads=["gm"], writes=["pen"])
            for g in range(8):
                kb.op("dve", lambda g=g: ncv.tensor_scalar(out=msk[:, g * 32:(g + 1) * 32], in0=sel[:, g * 32:(g + 1) * 32], scalar1=gm[:, g:g + 1],
                                                          scalar2=pen[:, g:g + 1], op0=ALU.mult, op1=ALU.add),
                      reads=["sel", "gm", "pen"], writes=["msk"])
            kb.op("dve", lambda: ncv.max(out=m8[:], in_=msk[:]), reads=["msk"], writes=["m8"])
            kb.op("dve", lambda: ncv.tensor_scalar(out=selm[:], in0=msk[:], scalar1=m8[:, 7:8], scalar2=None, op0=ALU.is_ge), reads=["msk", "m8"], writes=["selm"])
            kb.op("dve", lambda: ncv.scalar_tensor_tensor(out=wtmp[:], in0=sc[:], scalar=1.0, in1=selm[:], op0=ALU.mult, op1=ALU.mult, accum_out=wsum[:, 0:1]),
                  reads=["sc", "selm"], writes=["wtmp", "wsum"])
            kb.op("dve", lambda: ncv.reciprocal(out=wsum[:], in_=wsum[:]), reads=["wsum"], writes=["wsum"])
            kb.op("dve", lambda tt=tt: ncv.tensor_scalar(out=Gall[:, tt, :], in0=wtmp[:], scalar1=wsum[:, 0:1], scalar2=2.5, op0=ALU.mult, op1=ALU.mult),
                  reads=["wtmp", "wsum"], writes=["Gall"])
            if Mall is not None:
                kb.op("pool", lambda tt=tt: ncg.tensor_copy(out=Mall[:, tt, :], in_=selm[:]), reads=["selm"], writes=["Mall"])
            pg, pgk = pB[i], "pB%d" % i
            for kc in range(8):
                kb.op("pe", lambda kc=kc, i=i, pg=pg: ncp.matmul(pg[:, :], h2T[i][:, kc, :], wsgu[:, kc, :], start=(kc == 0), stop=(kc == 7)),
                      reads=["h2T%d" % i, "wsgu"], writes=[pgk])
            kb.op("act", lambda i=i, pg=pg: nca.activation(out=sg[i][:], in_=pg[:, 0:256], func=AF.Silu), reads=[pgk], writes=["sg%d" % i])
            kb.op("dve", lambda i=i, pg=pg: ncv.tensor_tensor(out=act[i][:], in0=sg[i][:], in1=pg[:, 256:512], op=ALU.mult), reads=[pgk, "sg%d" % i], writes=["actt%d" % i])
            pt, ptk = pB[2 + i], "pB%d" % (2 + i)
            for j in range(2):
                kb.op("pe", lambda j=j, i=i, pt=pt: ncp.transpose(pt[:, j * 128:(j + 1) * 128], act[i][:, j * 128:(j + 1) * 128], ident[:]),
                      reads=["actt%d" % i, "c_ident"], writes=[ptk])
            kb.op("act", lambda i=i, pt=pt: nca.copy(out=actT[i][:, :, :], in_=pt[:, 0:256].rearrange("p (k t) -> p k t", k=2)), reads=[ptk], writes=["actT%d" % i])
            for hf in range(2):
                for j in range(2):
                    kb.op("pe", lambda j=j, hf=hf, i=i, pa=pa: ncp.matmul(pa[:, hf * 512:(hf + 1) * 512], actT[i][:, j, :], wsd[:, j, hf * 512:(hf + 1) * 512],
                                                                         start=(j == 0), stop=(j == 1)),
                          reads=["actT%d" % i, "wsd"], writes=[pak])
            kb.op("dve", lambda i=i, b=b, pa=pa: ncv.tensor_tensor(out=ot[i][:], in0=pa[:, :], in1=gate2_b[:, b, :], op=ALU.mult),
                  reads=[pak, "gate2_b"], writes=["ot%d" % i])
            kb.op("pool", lambda i=i: ncg.tensor_tensor(out=ot[i][:], in0=ot[i][:], in1=x1[i][:], op=ALU.add),
                  reads=["ot%d" % i, "x1t%d" % i], writes=["ot%d" % i])
            kb.dma("sp", lambda i=i, r0=r0: ncs.dma_start(out=out[r0:r0 + 128, :], in_=ot[i][:]), reads=["ot%d" % i], writes=["out"])
        if dbg_t:
            kb.dma("sp", lambda: ncs.dma_start(out=dbg_t["G"], in_=Gall[:]), reads=["Gall"], writes=["dbg_G"])
        if dense:
            kb.dma("sp", lambda: ncs.dma_start(out=pv["Gd"], in_=Gall[:]), reads=["Gall"], writes=["Gd"])
        kb.barrier()
        s5a.close()
        if stage >= 6:
            if stage >= 7:
                phase_R(nc, kb, dram, cs, pv, pA, pB, out, gate2_b, Gall, Mall, dbg_t)
            else:
                phase_R_dense(nc, kb, dram, cs, pv, pA, pB, out, gate2_b)


def phase_R_dense(nc, kb, dram, cs, pv, pA, pB, out, gate2_b):
    ncv, nca, ncp, ncg, ncs = nc.vector, nc.scalar, nc.tensor, nc.gpsimd, nc.sync
    ident = cs["ident"]
    H2d, Gd = pv["H2d"], pv["Gd"]
    NS = DENSE_TB // 128
    NW = 3
    with contextlib.ExitStack() as s4:
        wgu = [kb.sb("wgu%d" % i, [128, 8, 512], F32R, stack=s4) for i in range(NW)]
        wdn = [kb.sb("wdn%d" % i, [128, 2, D], F32R, stack=s4) for i in range(NW)]
        Gb = kb.sb("Gb", [128, NS, NE], stack=s4)
        h2T = kb.sb("h2Tb", [128, 8, DENSE_TB], F32R, stack=s4)
        acc = [kb.sb("accd%d" % i, [128, D], stack=s4) for i in range(NS)]
        sg = [kb.sb("sgr%d" % i, [128, 256], stack=s4) for i in range(3)]
        act = [kb.sb("actr%d" % i, [128, 256], stack=s4) for i in range(3)]
        actT = [kb.sb("actTr%d" % i, [128, 2, 128], F32R, stack=s4) for i in range(3)]
        ot = [kb.sb("otr%d" % i, [128, D], stack=s4) for i in range(2)]

        def load_w(e):
            w = e % NW
            kb.dma("pool", lambda: ncg.dma_start(out=wgu[w][:, :, 0:256], in_=dram["w_exp_gate"][e].rearrange("(k p) n -> p k n", p=128)), writes=["wgu%d" % w])
            kb.dma("pool", lambda: ncg.dma_start(out=wgu[w][:, :, 256:512], in_=dram["w_exp_up"][e].rearrange("(k p) n -> p k n", p=128)), writes=["wgu%d" % w])
            kb.dma("pool", lambda: ncg.dma_start(out=wdn[w][:], in_=dram["w_exp_down"][e].rearrange("(k p) n -> p k n", p=128)), writes=["wdn%d" % w])

        units = [(e, sidx) for e in range(NE) for sidx in range(NS)]
        U = len(units)

        def gu(n):
            e, sidx = units[n]
            w = e % NW
            pg, pgk = pB[n % 2], "pB%d" % (n % 2)
            for kc in range(8):
                kb.op("pe", lambda kc=kc: ncp.matmul(pg[:, :], h2T[:, kc, sidx * 128:(sidx + 1) * 128], wgu[w][:, kc, :], start=(kc == 0), stop=(kc == 7)),
                      reads=["h2Tb", "wgu%d" % w], writes=[pgk], inc=(kc == 7))

        def mid1(n):
            pg, pgk = pB[n % 2], "pB%d" % (n % 2)
            q = n % 3
            kb.op("act", lambda: nca.activation(out=sg[q][:], in_=pg[:, 0:256], func=AF.Silu), reads=[pgk], writes=["sgr%d" % q])
            kb.op("dve", lambda: ncv.tensor_tensor(out=act[q][:], in0=sg[q][:], in1=pg[:, 256:512], op=ALU.mult), reads=[pgk, "sgr%d" % q], writes=["actr%d" % q])

        def tr(n):
            pt, ptk = pB[2 + n % 2], "pB%d" % (2 + n % 2)
            q = n % 3
            for j in range(2):
                kb.op("pe", lambda j=j: ncp.transpose(pt[:, j * 128:(j + 1) * 128], act[q][:, j * 128:(j + 1) * 128], ident[:]),
                      reads=["actr%d" % q, "c_ident"], writes=[ptk], inc=(j == 1))

        def mid2(n):
            pt, ptk = pB[2 + n % 2], "pB%d" % (2 + n % 2)
            q = n % 3
            kb.op("act", lambda: nca.copy(out=actT[q][:, :, :], in_=pt[:, 0:256].rearrange("p (k t) -> p k t", k=2)), reads=[ptk], writes=["actTr%d" % q])

        def dn(n):
            e, sidx = units[n]
            w = e % NW
            pa, pak = pA[n % 2], "pA%d" % (n % 2)
            q = n % 3
            for hf in range(2):
                for j in range(2):
                    kb.op("pe", lambda j=j, hf=hf: ncp.matmul(pa[:, hf * 512:(hf + 1) * 512], actT[q][:, j, :], wdn[w][:, j, hf * 512:(hf + 1) * 512],
                                                              start=(j == 0), stop=(j == 1)),
                          reads=["actTr%d" % q, "wdn%d" % w], writes=[pak], inc=(hf == 1 and j == 1))

        def fin(n):
            e, sidx = units[n]
            pa, pak = pA[n % 2], "pA%d" % (n % 2)
            kb.op("dve", lambda: ncv.scalar_tensor_tensor(out=acc[sidx][:], in0=pa[:, :], scalar=Gb[:, sidx, e:e + 1], in1=acc[sidx][:], op0=ALU.mult, op1=ALU.add),
                  reads=[pak, "Gb", "accd%d" % sidx], writes=["accd%d" % sidx])

        for tb in range(T // DENSE_TB):
            kb.dma("sp", lambda tb=tb: ncs.dma_start(out=Gb[:], in_=Gd[:, tb * NS:(tb + 1) * NS, :]), reads=["Gd"], writes=["Gb"])
            load_w(0)
            load_w(1)
            for sidx in range(NS):
                i = tb * NS + sidx
                j = sidx % 2
                kb.dma("sp", lambda i=i, j=j: ncs.dma_start(out=ot[j][:], in_=H2d[i * 128:(i + 1) * 128, :]), reads=["H2d"], writes=["otr%d" % j])
                pa, pak = pA[j], "pA%d" % j
                for kc in range(8):
                    kb.op("pe", lambda kc=kc, j=j, pa=pa: ncp.transpose(pa[:, kc * 128:(kc + 1) * 128], ot[j][:, kc * 128:(kc + 1) * 128], ident[:]),
                          reads=["otr%d" % j, "c_ident"], writes=[pak], inc=(kc == 7))
                kb.op("act", lambda sidx=sidx, pa=pa: nca.copy(out=h2T[:, 0:4, sidx * 128:(sidx + 1) * 128], in_=pa[:, 0:512].rearrange("p (k t) -> p k t", k=4)),
                      reads=[pak], writes=["h2Tb"])
                kb.op("dve", lambda sidx=sidx, pa=pa: ncv.tensor_copy(out=h2T[:, 4:8, sidx * 128:(sidx + 1) * 128], in_=pa[:, 512:1024].rearrange("p (k t) -> p k t", k=4)),
                      reads=[pak], writes=["h2Tb"])
                kb.op("pool", lambda sidx=sidx: ncg.memset(acc[sidx][:], 0.0), writes=["accd%d" % sidx])
            gu(0)
            mid1(0)
            for n in range(U):
                if n + 1 < U:
                    gu(n + 1)
                    mid1(n + 1)
                tr(n)
                mid2(n)
                if n >= 1:
                    dn(n - 1)
                    fin(n - 1)
                e, sidx = units[n]
                if sidx == 1 and e + 2 < NE:
                    load_w(e + 2)
            dn(U - 1)
            fin(U - 1)
            for sidx in range(NS):
                i = tb * NS + sidx
                j = sidx % 2
                b = i // (S // 128)
                kb.dma("sp", lambda i=i, j=j: ncs.dma_start(out=ot[j][:], in_=out[i * 128:(i + 1) * 128, :]), reads=["out"], writes=["otr%d" % j])
                kb.op("pool", lambda sidx=sidx, b=b: ncg.tensor_tensor(out=acc[sidx][:], in0=acc[sidx][:], in1=gate2_b[:, b, :], op=ALU.mult),
                      reads=["accd%d" % sidx, "gate2_b"], writes=["accd%d" % sidx])
                kb.op("pool", lambda sidx=sidx, j=j: ncg.tensor_tensor(out=ot[j][:], in0=ot[j][:], in1=acc[sidx][:], op=ALU.add),
                      reads=["accd%d" % sidx, "otr%d" % j], writes=["otr%d" % j])
                kb.dma("sp", lambda i=i, j=j: ncs.dma_start(out=out[i * 128:(i + 1) * 128, :], in_=ot[j][:]), reads=["otr%d" % j], writes=["out"])
        kb.barrier()


def phase_R(nc, kb, dram, cs, pv, pA, pB, out, gate2_b, Gall, Mall, dbg_t):
    ncv, nca, ncp, ncg, ncs = nc.vector, nc.scalar, nc.tensor, nc.gpsimd, nc.sync
    ident = cs["ident"]
    NT = T // 128
    R = NBLK * 128
    BIGK = 70000.0
    H2d, XGd, Yd, BEXd = pv["H2d"], pv["XGd"], pv["Yd"], pv["BEXd"]
    with contextlib.ExitStack() as sr:
        DESTi = kb.sb("DESTi", [128, NT * 8], I32, stack=sr)
        GK = kb.sb("GK", [128, NT * 8], stack=sr)
        bexrow = kb.sb("bexrow", [1, NBLK], I32, stack=sr)
        with contextlib.ExitStack() as s2:
            RANK = kb.sb("RANK", [128, NT, NE], stack=s2)
            base = kb.sb("base", [128, NE], stack=s2)
            nbk = kb.sb("nbk", [128, NE], stack=s2)
            padded = kb.sb("padded", [128, NE], stack=s2)
            cA = kb.sb("cA", [128, NE], stack=s2)
            cB = kb.sb("cB", [128, NE], stack=s2)
            key = kb.sb("key", [128, NE], stack=s2)
            jk = kb.sb("jk", [128, NE], stack=s2)
            ones256 = kb.sb("ones256", [128, NE], stack=s2)
            m8 = kb.sb("m8r", [128, 8], stack=s2)
            destf = kb.sb("destf", [128, NT * 8], stack=s2)
            bx = kb.sb("bx", [128, 4], stack=s2)
            bxi = kb.sb("bxi", [128, 4], I32, stack=s2)
            kb.op("pool", lambda: ncg.memset(base[:], 0.0), writes=["base"])
            kb.op("pool", lambda: ncg.memset(nbk[:], 0.0), writes=["nbk"])
            kb.op("pool", lambda: ncg.memset(ones256[:], 1.0), writes=["ones256"])
            for i in range(NT):
                pt, ptk = pB[i % 2], "pB%d" % (i % 2)
                kb.op("pe", lambda i=i, pt=pt: ncp.matmul(pt[:, 0:NE], cs["tri"][:], Mall[:, i, :], start=True, stop=True), reads=["Mall", "c_tri"], writes=[ptk])
                kb.op("pe", lambda i=i, pt=pt: ncp.matmul(pt[:, NE:2 * NE], cs["ones_bf"][:], Mall[:, i, :], start=True, stop=True), reads=["Mall", "c_ones_bf"], writes=[ptk])
                kb.op("dve", lambda i=i, pt=pt: ncv.tensor_tensor(out=RANK[:, i, :], in0=pt[:, 0:NE], in1=base[:], op=ALU.add), reads=[ptk, "base"], writes=["RANK"])
                kb.op("dve", lambda pt=pt: ncv.tensor_tensor(out=base[:], in0=base[:], in1=pt[:, NE:2 * NE], op=ALU.add), reads=[ptk, "base"], writes=["base"])
            for k in range(T // 128):
                kb.op("dve", lambda k=k: ncv.scalar_tensor_tensor(out=nbk[:], in0=base[:], scalar=128.0 * k, in1=nbk[:], op0=ALU.is_gt, op1=ALU.add),
                      reads=["base", "nbk"], writes=["nbk"])
            kb.op("dve", lambda: ncv.tensor_scalar(out=padded[:], in0=nbk[:], scalar1=128.0, scalar2=None, op0=ALU.mult), reads=["nbk"], writes=["padded"])
            kb.op("dve", lambda: ncv.tensor_copy(out=cA[:], in_=padded[:]), reads=["padded"], writes=["cA"])
            cur, curk, nxt, nxtk = cA, "cA", cB, "cB"
            sft = 1
            while sft < NE:
                kb.op("dve", lambda cur=cur, nxt=nxt, sft=sft: ncv.tensor_copy(out=nxt[:, 0:sft], in_=cur[:, 0:sft]), reads=[curk], writes=[nxtk])
                kb.op("dve", lambda cur=cur, nxt=nxt, sft=sft: ncv.tensor_tensor(out=nxt[:, sft:NE], in0=cur[:, sft:NE], in1=cur[:, 0:NE - sft], op=ALU.add),
                      reads=[curk], writes=[nxtk])
                cur, curk, nxt, nxtk = nxt, nxtk, cur, curk
                sft *= 2
            pend, pendk = cur, curk
            pstart, pstartk = nxt, nxtk
            kb.op("dve", lambda: ncv.tensor_tensor(out=pstart[:], in0=pend[:], in1=padded[:], op=ALU.subtract), reads=[pendk, "padded"], writes=[pstartk])
            for i in range(NT):
                kb.op("dve", lambda i=i: ncv.tensor_tensor(out=key[:], in0=RANK[:, i, :], in1=pstart[:], op=ALU.add), reads=["RANK", pstartk], writes=["key"])
                kb.op("dve", lambda: ncv.tensor_scalar(out=key[:], in0=key[:], scalar1=-1.0, scalar2=BIGK + 1.0, op0=ALU.mult, op1=ALU.add), reads=["key"], writes=["key"])
                kb.op("dve", lambda i=i: ncv.tensor_tensor(out=key[:], in0=key[:], in1=Mall[:, i, :], op=ALU.mult), reads=["key", "Mall"], writes=["key"])
                kb.op("dve", lambda: ncv.max(out=m8[:], in_=key[:]), reads=["key"], writes=["m8r"])
                kb.op("dve", lambda i=i: ncv.tensor_scalar(out=destf[:, i * 8:(i + 1) * 8], in0=m8[:], scalar1=-1.0, scalar2=BIGK + 1.0, op0=ALU.mult, op1=ALU.add),
                      reads=["m8r"], writes=["destf"])
                for k in range(8):
                    kb.op("dve", lambda i=i, k=k: ncv.scalar_tensor_tensor(out=jk[:], in0=key[:], scalar=m8[:, k:k + 1], in1=Gall[:, i, :], op0=ALU.is_equal, op1=ALU.mult,
                                                                             accum_out=GK[:, i * 8 + k:i * 8 + k + 1]),
                          reads=["key", "m8r", "Gall"], writes=["jk", "GK"])
            kb.op("dve", lambda: ncv.tensor_copy(out=DESTi[:], in_=destf[:]), reads=["destf"], writes=["DESTi"])
            for j in range(4):
                kb.op("dve", lambda j=j: ncv.scalar_tensor_tensor(out=jk[:], in0=pend[:], scalar=cs["thr4"][:, j:j + 1], in1=ones256[:], op0=ALU.is_le, op1=ALU.mult,
                                                                  accum_out=bx[:, j:j + 1]),
                      reads=[pendk, "c_thr4", "ones256"], writes=["jk", "bx"])
            kb.op("dve", lambda: ncv.tensor_scalar(out=bx[:], in0=bx[:], scalar1=float(NE - 1), scalar2=None, op0=ALU.min), reads=["bx"], writes=["bx"])
            kb.op("dve", lambda: ncv.tensor_copy(out=bxi[:], in_=bx[:]), reads=["bx"], writes=["bxi"])
            with nc.allow_non_contiguous_dma(reason="tiny block->expert table transpose"):
                kb.dma("sp", lambda: ncs.dma_start(out=BEXd.rearrange("(j p) -> p j", p=128), in_=bxi[:]), reads=["bxi"], writes=["BEXd"])
            kb.dma("sp", lambda: ncs.dma_start(out=bexrow[:], in_=BEXd.rearrange("(o n) -> o n", o=1)), reads=["BEXd"], writes=["bexrow"])
            if dbg_t:
                kb.dma("sp", lambda: ncs.dma_start(out=dbg_t["dest"], in_=DESTi[:]), reads=["DESTi"], writes=["dbg_dest"])
                kb.dma("sp", lambda: ncs.dma_start(out=dbg_t["gk"], in_=GK[:]), reads=["GK"], writes=["dbg_gk"])
                kb.dma("sp", lambda: ncs.dma_start(out=dbg_t["bex"], in_=bexrow[:]), reads=["bexrow"], writes=["dbg_bex"])
            kb.barrier()
        ssem = kb.stack.enter_context(nc.semaphore("ix_scatter"))
        with contextlib.ExitStack() as s3:
            h2all = kb.sb("h2all", [128, NT, D], stack=s3)
            for i in range(NT):
                kb.dma("sp" if i % 2 == 0 else "act", lambda i=i: (ncs if i % 2 == 0 else nca).dma_start(out=h2all[:, i, :], in_=H2d[i * 128:(i + 1) * 128, :]),
                       reads=["H2d"], writes=["h2all%d" % i])
            nsc = 0
            for i in range(NT):
                kb._need("pool", kb._deps(["h2all%d" % i, "DESTi"], []))
                for k in range(8):
                    ncg.indirect_dma_start(
                        out=XGd[:, :], out_offset=bass.IndirectOffsetOnAxis(ap=DESTi[:, i * 8 + k:i * 8 + k + 1], axis=0),
                        in_=h2all[:, i, :], in_offset=None, bounds_check=R - 1, oob_is_err=False).then_inc(ssem, 16)
                    nsc += 1
            tok = (ssem, "ix_scatter", 16 * nsc, "dma")
            kb._record(tok, ["h2all%d" % i for i in range(NT)] + ["DESTi"], ["XGd"])
            kb.dma("sp", lambda: ncs.dma_start(out=BEXd[0:1], in_=BEXd[0:1]), reads=["XGd"], writes=["relay"])
            kb.barrier()
            kb._record(tok, [], ["XGd"])
        with contextlib.ExitStack() as s4:
            wgu = [kb.sb("wgu%d" % i, [128, 8, 512], F32R, stack=s4) for i in range(2)]
            wdn = [kb.sb("wdn%d" % i, [128, 2, D], F32R, stack=s4) for i in range(2)]
            xg = [kb.sb("xg%d" % i, [128, D], stack=s4) for i in range(2)]
            xgT = [kb.sb("xgT%d" % i, [128, 8, 128], F32R, stack=s4) for i in range(2)]
            sg = [kb.sb("sgr%d" % i, [128, 256], stack=s4) for i in range(2)]
            act = [kb.sb("actr%d" % i, [128, 256], stack=s4) for i in range(2)]
            actT = [kb.sb("actTr%d" % i, [128, 2, 128], F32R, stack=s4) for i in range(2)]
            yt = [kb.sb("yt%d" % i, [128, D], stack=s4) for i in range(2)]
            for blk in range(NBLK):
                i = blk % 2
                kb._need("pool", kb._deps(["bexrow"], ["wgu%d" % i, "wdn%d" % i]))
                e = ncg.value_load(bexrow[0:1, blk:blk + 1], min_val=0, max_val=NE - 1)
                kb.dma("pool", lambda i=i, e=e: ncg.dma_start(out=wgu[i][:, :, 0:256], in_=dram["w_exp_gate"][bass.ds(e, 1), :, :].rearrange("o (k p) n -> p (o k) n", p=128)),
                       reads=["bexrow"], writes=["wgu%d" % i])
                kb.dma("pool", lambda i=i, e=e: ncg.dma_start(out=wgu[i][:, :, 256:512], in_=dram["w_exp_up"][bass.ds(e, 1), :, :].rearrange("o (k p) n -> p (o k) n", p=128)),
                       reads=["bexrow"], writes=["wgu%d" % i])
                kb.dma("pool", lambda i=i, e=e: ncg.dma_start(out=wdn[i][:], in_=dram["w_exp_down"][bass.ds(e, 1), :, :].rearrange("o (k p) n -> p (o k) n", p=128)),
                       reads=["bexrow"], writes=["wdn%d" % i])
                kb.dma("sp", lambda i=i, blk=blk: ncs.dma_start(out=xg[i][:], in_=XGd[blk * 128:(blk + 1) * 128, :]), reads=["XGd"], writes=["xg%d" % i])
                pa, pak = pA[i], "pA%d" % i
                for kc in range(8):
                    kb.op("pe", lambda kc=kc, i=i, pa=pa: ncp.transpose(pa[:, kc * 128:(kc + 1) * 128], xg[i][:, kc * 128:(kc + 1) * 128], ident[:]),
                          reads=["xg%d" % i, "c_ident"], writes=[pak])
                kb.op("act", lambda i=i, pa=pa: nca.copy(out=xgT[i][:, 0:4, :], in_=pa[:, 0:512].rearrange("p (k t) -> p k t", k=4)), reads=[pak], writes=["xgT%d" % i])
                kb.op("dve", lambda i=i, pa=pa: ncv.tensor_copy(out=xgT[i][:, 4:8, :], in_=pa[:, 512:1024].rearrange("p (k t) -> p k t", k=4)), reads=[pak], writes=["xgT%d" % i])
                pg, pgk = pB[i], "pB%d" % i
                for kc in range(8):
                    kb.op("pe", lambda kc=kc, i=i, pg=pg: ncp.matmul(pg[:, :], xgT[i][:, kc, :], wgu[i][:, kc, :], start=(kc == 0), stop=(kc == 7)),
                          reads=["xgT%d" % i, "wgu%d" % i], writes=[pgk])
                kb.op("act", lambda i=i, pg=pg: nca.activation(out=sg[i][:], in_=pg[:, 0:256], func=AF.Silu), reads=[pgk], writes=["sgr%d" % i])
                kb.op("dve", lambda i=i, pg=pg: ncv.tensor_tensor(out=act[i][:], in0=sg[i][:], in1=pg[:, 256:512], op=ALU.mult), reads=[pgk, "sgr%d" % i], writes=["actr%d" % i])
                pt, ptk = pB[2 + i], "pB%d" % (2 + i)
                for j in range(2):
                    kb.op("pe", lambda j=j, i=i, pt=pt: ncp.transpose(pt[:, j * 128:(j + 1) * 128], act[i][:, j * 128:(j + 1) * 128], ident[:]),
                          reads=["actr%d" % i, "c_ident"], writes=[ptk])
                kb.op("act", lambda i=i, pt=pt: nca.copy(out=actT[i][:, :, :], in_=pt[:, 0:256].rearrange("p (k t) -> p k t", k=2)), reads=[ptk], writes=["actTr%d" % i])
                for hf in range(2):
                    for j in range(2):
                        kb.op("pe", lambda j=j, hf=hf, i=i, pa=pa: ncp.matmul(pa[:, hf * 512:(hf + 1) * 512], actT[i][:, j, :], wdn[i][:, j, hf * 512:(hf + 1) * 512],
                                                                             start=(j == 0), stop=(j == 1)),
                              reads=["actTr%d" % i, "wdn%d" % i], writes=[pak])
                kb.op("act", lambda i=i, pa=pa: nca.copy(out=yt[i][:, 0:512], in_=pa[:, 0:512]), reads=[pak], writes=["yt%d" % i])
                kb.op("dve", lambda i=i, pa=pa: ncv.tensor_copy(out=yt[i][:, 512:1024], in_=pa[:, 512:1024]), reads=[pak], writes=["yt%d" % i])
                kb.dma("sp", lambda i=i, blk=blk: ncs.dma_start(out=Yd[blk * 128:(blk + 1) * 128, :], in_=yt[i][:]), reads=["yt%d" % i], writes=["Yd"])
            kb.barrier()
        gsem = [kb.stack.enter_context(nc.semaphore("ix_g%d" % k)) for k in range(8)]
        with contextlib.ExitStack() as s6:
            yk = [kb.sb("yk%d" % i, [128, D], stack=s6) for i in range(8)]
            acc = [kb.sb("accr%d" % i, [128, D], stack=s6) for i in range(2)]
            ot = [kb.sb("otr%d" % i, [128, D], stack=s6) for i in range(2)]
            for i in range(NT):
                j = i % 2
                b = i // (S // 128)
                kb.dma("sp", lambda i=i, j=j: ncs.dma_start(out=ot[j][:], in_=out[i * 128:(i + 1) * 128, :]), reads=["out"], writes=["otr%d" % j])
                for k in range(8):
                    kb._need("pool", kb._deps(["Yd", "DESTi"], ["yk%d" % k]))
                    ncg.indirect_dma_start(
                        out=yk[k][:, :], out_offset=None, in_=Yd[:, :],
                        in_offset=bass.IndirectOffsetOnAxis(ap=DESTi[:, i * 8 + k:i * 8 + k + 1], axis=0), bounds_check=R - 1, oob_is_err=False).then_inc(gsem[k], 16)
                    kb._record((gsem[k], "ix_g%d" % k, 16 * (i + 1), "dma"), ["Yd", "DESTi"], ["yk%d" % k])
                    gcol = GK[:, i * 8 + k:i * 8 + k + 1]
                    if k == 0:
                        kb.op("dve", lambda j=j, k=k, gcol=gcol: ncv.tensor_scalar(out=acc[j][:], in0=yk[k][:], scalar1=gcol, scalar2=None, op0=ALU.mult),
                              reads=["yk%d" % k, "GK"], writes=["accr%d" % j])
                    else:
                        kb.op("dve", lambda j=j, k=k, gcol=gcol: ncv.scalar_tensor_tensor(out=acc[j][:], in0=yk[k][:], scalar=gcol, in1=acc[j][:], op0=ALU.mult, op1=ALU.add),
                              reads=["yk%d" % k, "GK", "accr%d" % j], writes=["accr%d" % j])
                kb.op("dve", lambda j=j, b=b: ncv.tensor_tensor(out=acc[j][:], in0=acc[j][:], in1=gate2_b[:, b, :], op=ALU.mult), reads=["accr%d" % j, "gate2_b"], writes=["accr%d" % j])
                kb.op("dve", lambda j=j: ncv.tensor_tensor(out=ot[j][:], in0=ot[j][:], in1=acc[j][:], op=ALU.add), reads=["accr%d" % j, "otr%d" % j], writes=["otr%d" % j])
                kb.dma("sp", lambda i=i, j=j: ncs.dma_start(out=out[i * 128:(i + 1) * 128, :], in_=ot[j][:]), reads=["otr%d" % j], writes=["out"])
            kb.barrier()


def phase_B(nc, kb, b, dram, cs, pv, pA, pB, HTd, PMd, AOd, out, dbg_t, stage):
    ncv, nca, ncp, ncg, ncs = nc.vector, nc.scalar, nc.tensor, nc.gpsimd, nc.sync
    modT, gs1, lsT, gq, gk = pv["modT"], pv["gs1"], pv["lsT"], pv["gq"], pv["gk"]
    ident = cs["ident"]
    w_in = dram["w_in"]
    GROUPS = ((128, 1), (512, 4), (2048, 16))

    with contextlib.ExitStack() as sq:
        qT = kb.sb("qT", [128, 6, S], BF16, stack=sq)
        kT = kb.sb("kT", [128, 6, 2, S], BF16, stack=sq)
        kb.op("pool", lambda: ncg.memset(kT[:], 0.0), writes=["kT"])
        Vg = [kb.sb("Vg%d" % g, [128, 16, 4, 65], BF16, stack=sq) for g in range(3)]
        for g in range(3):
            kb.op("pool", lambda g=g: ncg.memset(Vg[g][:], 1.0), writes=["Vg%d" % g])
        with contextlib.ExitStack() as s1:
            hT = kb.sb("hT", [128, 8, S], F32R, stack=s1)
            s1b = contextlib.ExitStack()
            xt = [kb.sb("xt%d" % i, [128, D], stack=s1b) for i in range(2)]
            xn = [kb.sb("xn%d" % i, [128, D], stack=s1b) for i in range(2)]
            junk = kb.sb("junk", [128, D], stack=s1b)
            ss = kb.sb("ss", [128, 2], stack=s1b)
            rstd = kb.sb("rstd", [128, 2], stack=s1b)
            for tt in range(16):
                i = tt % 2
                r0 = b * S + tt * 128
                kb.dma("sp", lambda i=i, r0=r0: ncs.dma_start(out=xt[i][:], in_=dram["x"][r0:r0 + 128, :]), writes=["xt%d" % i])
                kb.op("act", lambda i=i: nca.activation(out=junk[:], in_=xt[i][:], func=AF.Square, accum_out=ss[:, i:i + 1]),
                      reads=["xt%d" % i], writes=["junk", "ss%d" % i])
                kb.op("act", lambda i=i: nca.activation(out=rstd[:, i:i + 1], in_=ss[:, i:i + 1], func=AF.Sqrt, scale=1.0 / D, bias=cs["epsc"][:, 0:1]),
                      reads=["ss%d" % i, "c_epsc"], writes=["rstd%d" % i])
                kb.op("dve", lambda i=i: ncv.reciprocal(out=rstd[:, i:i + 1], in_=rstd[:, i:i + 1]),
                      reads=["rstd%d" % i], writes=["rstd%d" % i])
                kb.op("act", lambda i=i: nca.activation(out=xn[i][:], in_=xt[i][:], func=AF.Identity, scale=rstd[:, i:i + 1]),
                      reads=["xt%d" % i, "rstd%d" % i], writes=["xn%d" % i])
                pa = pA[i]
                for kc in range(8):
                    kb.op("pe", lambda kc=kc, i=i, pa=pa: ncp.transpose(pa[:, kc * 128:(kc + 1) * 128], xn[i][:, kc * 128:(kc + 1) * 128], ident[:]),
                          reads=["xn%d" % i, "c_ident"], writes=["pA%d" % i])
                for kc in range(8):
                    dst = hT[:, kc, tt * 128:(tt + 1) * 128]
                    if kc % 2 == 0:
                        kb.op("dve", lambda kc=kc, pa=pa, dst=dst: ncv.tensor_scalar(out=dst, in0=pa[:, kc * 128:(kc + 1) * 128], scalar1=gs1[:, kc, b:b + 1],
                                                                                     scalar2=modT[:, kc, b:b + 1], op0=ALU.mult, op1=ALU.add),
                              reads=["pA%d" % i, "gs1", "modT"], writes=["hT"])
                    else:
                        kb.op("act", lambda kc=kc, pa=pa, dst=dst: nca.activation(out=dst, in_=pa[:, kc * 128:(kc + 1) * 128], func=AF.Identity,
                                                                                  scale=gs1[:, kc, b:b + 1], bias=modT[:, kc, b:b + 1]),
                              reads=["pA%d" % i, "gs1", "modT"], writes=["hT"])
            for kc in range(8):
                kb.dma("pool", lambda kc=kc: ncg.dma_start(out=HTd[b, kc * 128:(kc + 1) * 128, :], in_=hT[:, kc, :]),
                       reads=["hT"], writes=["HTd"])
                if dbg_t:
                    kb.dma("pool", lambda kc=kc: ncg.dma_start(out=dbg_t["hT"][b, kc * 128:(kc + 1) * 128, :], in_=hT[:, kc, :]),
                           reads=["hT"], writes=["dbg_hT"])
            kb.barrier()
            s1b.close()
            if stage >= 2:
                phase_B2a(nc, kb, b, dram, cs, pv, pA, pB, hT, qT, kT, Vg, PMd, dbg_t, stage)
            kb.barrier()
        if stage >= 3:
            phase_B2b(nc, kb, b, cs, pA, pB, qT, kT, Vg, AOd, dbg_t)
        kb.barrier()
    if stage >= 4:
        phase_B2c(nc, kb, b, dram, cs, pv, pA, pB, HTd, PMd, AOd, out)
        kb.barrier()


def phase_B2a(nc, kb, b, dram, cs, pv, pA, pB, hT, qT, kT, Vg, PMd, dbg_t, stage):
    ncv, nca, ncp, ncg, ncs = nc.vector, nc.scalar, nc.tensor, nc.gpsimd, nc.sync
    lsT, gq, gk = pv["lsT"], pv["gq"], pv["gk"]
    w_in = dram["w_in"]
    with contextlib.ExitStack() as s2:
        win = [kb.sb("win%d" % i, [128, 8, 128], F32R, stack=s2) for i in range(2)]
        PADW = 16
        sU = contextlib.ExitStack()
        wgrp = kb.sb("wgrp", [128, 128], F32R, stack=sU)
        ub = kb.sb("ub", [128, S + 2 * PADW], stack=sU)
        a1 = kb.sb("a1", [128, S + 2 * PADW], stack=sU)
        a2 = kb.sb("a2", [128, S + 2 * PADW], stack=sU)
        pooled = kb.sb("pooled", [128, S], F32R, stack=sU)
        pmt = [kb.sb("pmt0", [128, 512], stack=sU)] * 2
        kb.op("pool", lambda: ncg.memset(ub[:], 0.0), writes=["ub"])
        kb.op("pool", lambda: ncg.memset(a1[:], 0.0), writes=["a1"])
        kb.op("pool", lambda: ncg.memset(a2[:], 0.0), writes=["a2"])
        sQ = None

        def open_qk():
            sQ_ = contextlib.ExitStack()
            Ct_ = kb.sb("Ct", [128, S], stack=sQ_)
            St_ = kb.sb("St", [128, S], stack=sQ_)
            with contextlib.ExitStack() as sp_:
                posi = kb.sb("posi", [128, S], I32, stack=sp_)
                kb.dma("sp", lambda: ncs.dma_start(out=posi[:], in_=dram["positions"][b:b + 1, :].rearrange("o d -> (o d)").partition_broadcast(128)), writes=["posi"])
                H = S // 8
                posf = kb.sb("posf", [128, S], stack=sp_)
                kf = kb.sb("kf", [128, H], stack=sp_)
                ki = kb.sb("ki", [128, H], I32, stack=sp_)
                kb.op("dve", lambda: ncv.tensor_copy(out=posf[:], in_=posi[:]), reads=["posi"], writes=["posf"])
                C1 = 6.28125
                C2 = TWO_PI - C1
                for tab0, tk_, off in ((St_, "St", 0.0), (Ct_, "Ct", 0.5 * math.pi)):
                    for hh in range(8):
                        tab = tab0[:, hh * H:(hh + 1) * H]
                        pf = posf[:, hh * H:(hh + 1) * H]
                        kb.op("dve", lambda tab=tab, pf=pf, off=off: ncv.tensor_scalar(out=tab, in0=pf, scalar1=cs["invf"][:, 0:1], scalar2=off, op0=ALU.mult, op1=ALU.add),
                              reads=["posf", "c_invf"], writes=[tk_])
                        kb.op("dve", lambda tab=tab: ncv.tensor_scalar(out=kf[:], in0=tab, scalar1=1.0 / TWO_PI, scalar2=None, op0=ALU.mult),
                              reads=[tk_], writes=["kf"])
                        kb.op("dve", lambda: ncv.tensor_copy(out=ki[:], in_=kf[:]), reads=["kf"], writes=["ki"])
                        kb.op("dve", lambda: ncv.tensor_copy(out=kf[:], in_=ki[:]), reads=["ki"], writes=["kf"])
                        kb.op("dve", lambda tab=tab: ncv.scalar_tensor_tensor(out=tab, in0=kf[:], scalar=-C1, in1=tab, op0=ALU.mult, op1=ALU.add),
                              reads=["kf", tk_], writes=[tk_])
                        kb.op("dve", lambda tab=tab: ncv.scalar_tensor_tensor(out=tab, in0=kf[:], scalar=-C2, in1=tab, op0=ALU.mult, op1=ALU.add),
                              reads=["kf", tk_], writes=[tk_])
                        kb.op("dve", lambda tab=tab: ncv.tensor_scalar(out=kf[:], in0=tab, scalar1=math.pi, scalar2=-TWO_PI, op0=ALU.is_gt, op1=ALU.mult),
                              reads=[tk_], writes=["kf"])
                        kb.op("dve", lambda tab=tab: ncv.tensor_tensor(out=tab, in0=tab, in1=kf[:], op=ALU.add), reads=["kf", tk_], writes=[tk_])
                        kb.op("dve", lambda tab=tab: ncv.tensor_scalar(out=kf[:], in0=tab, scalar1=-math.pi, scalar2=TWO_PI, op0=ALU.is_lt, op1=ALU.mult),
                              reads=[tk_], writes=["kf"])
                        kb.op("dve", lambda tab=tab: ncv.tensor_tensor(out=tab, in0=tab, in1=kf[:], op=ALU.add), reads=["kf", tk_], writes=[tk_])
                        kb.op("act", lambda tab=tab: nca.activation(out=tab, in_=tab, func=AF.Sin), reads=[tk_], writes=[tk_])
                kb.barrier()
            sqt_ = [kb.sb("sqt%d" % i, [128, 512], BF16, stack=sQ_) for i in range(2)]
            qg_ = [kb.sb("qg%d" % i, [128, 512], BF16, stack=sQ_) for i in range(2)]
            rs_ = [kb.sb("rs%d" % i, [128, 512], stack=sQ_) for i in range(2)]
            ta_ = [kb.sb("ta%d" % i, [128, 512], stack=sQ_) for i in range(2)]
            tb_ = [kb.sb("tb%d" % i, [128, 512], stack=sQ_) for i in range(2)]
            return sQ_, Ct_, St_, sqt_, qg_, rs_, ta_, tb_

        pcnt = [0]

        def next_pb():
            p = pcnt[0] % 4
            pcnt[0] += 1
            return pB[p], "pB%d" % p

        def proj_tile(f, wi, n):
            pb, pk = next_pb()
            for kc in range(8):
                kb.op("pe", lambda kc=kc: ncp.matmul(pb[:, :], win[wi][:, kc, :], hT[:, kc, n * 512:(n + 1) * 512], start=(kc == 0), stop=(kc == 7)),
                      reads=["win%d" % wi, "hT"], writes=[pk])
            return pb, pk

        for f in range(16):
            wi = f % 2
            if f == 4 and stage < 2.2:
                break
            if f == 4:
                kb.barrier()
                sU.close()
                sQ, Ct, St, sqt, qg, rs, ta, tb = open_qk()
            kb.dma("pool", lambda f=f, wi=wi: ncg.dma_start(out=win[wi][:], in_=w_in[:, f * 128:(f + 1) * 128].rearrange("(k p) n -> p k n", p=128)),
                   writes=["win%d" % wi])
            if f < 4:
                g = f
                R = (1, 2, 4, 8)[g]
                kb.dma("pool", lambda g=g: ncg.dma_start(out=wgrp[:], in_=dram["pool_w_grp"][g * 128:(g + 1) * 128, :]), writes=["wgrp"])
                for n in range(4):
                    pb, pk = proj_tile(f, wi, n)
                    kb.op("act", lambda n=n, pb=pb: nca.copy(out=ub[:, PADW + n * 512:PADW + (n + 1) * 512], in_=pb[:, :]), reads=[pk], writes=["ub"])
                lo, hi = 0, S + 2 * PADW
                kb.op("dve", lambda: ncv.tensor_tensor(out=a1[:, 0:hi - 1], in0=ub[:, 0:hi - 1], in1=ub[:, 1:hi], op=ALU.add), reads=["ub"], writes=["a1"])
                cur, curk, width = a1, "a1", 2
                other, otherk = a2, "a2"
                while width < R * 2:
                    kb.op("dve", lambda cur=cur, other=other, width=width: ncv.tensor_tensor(
                        out=other[:, 0:hi - 2 * width + 1], in0=cur[:, 0:hi - 2 * width + 1], in1=cur[:, width:hi - width + 1], op=ALU.add),
                        reads=[curk], writes=[otherk])
                    cur, other = other, cur
                    curk, otherk = otherk, curk
                    width *= 2
                kb.op("dve", lambda cur=cur, other=other, R=R: ncv.tensor_tensor(
                    out=other[:, PADW:PADW + S], in0=cur[:, PADW - R:PADW - R + S], in1=ub[:, PADW + R:PADW + R + S], op=ALU.add),
                    reads=[curk, "ub"], writes=[otherk])
                kb.op("dve", lambda other=other, R=R: ncv.scalar_tensor_tensor(
                    out=pooled[:, :], in0=other[:, PADW:PADW + S], scalar=1.0 / (2 * R + 1), in1=ub[:, PADW:PADW + S], op0=ALU.mult, op1=ALU.subtract),
                    reads=[otherk, "ub"], writes=["pooled"])
                kb.op("dve", lambda other=other, R=R, g=g: ncv.tensor_tensor(
                    out=cur[:, PADW:PADW + R], in0=other[:, PADW:PADW + R], in1=cs["pooledge"][:, g, 0:R], op=ALU.mult),
                    reads=[otherk, "c_pooledge"], writes=[curk])
                kb.op("dve", lambda cur=cur, R=R: ncv.tensor_tensor(
                    out=pooled[:, 0:R], in0=cur[:, PADW:PADW + R], in1=ub[:, PADW:PADW + R], op=ALU.subtract),
                    reads=[curk, "ub"], writes=["pooled"])
                for t in range(R):
                    pos = S - 1 - t
                    kb.op("dve", lambda other=other, g=g, t=t, pos=pos: ncv.scalar_tensor_tensor(
                        out=pooled[:, pos:pos + 1], in0=other[:, PADW + pos:PADW + pos + 1], scalar=cs["pooledge"][:, g, 8 + t:9 + t],
                        in1=ub[:, PADW + pos:PADW + pos + 1], op0=ALU.mult, op1=ALU.subtract),
                        reads=[otherk, "ub", "c_pooledge"], writes=["pooled"])
                for n in range(4):
                    pb, pk = next_pb()
                    kb.op("pe", lambda g=g, n=n, pb=pb: ncp.matmul(pb[:, :], wgrp[:, :], pooled[:, n * 512:(n + 1) * 512], start=True, stop=True),
                          reads=["wgrp", "pooled"], writes=[pk])
                    j = 0
                    kb.op("act", lambda g=g, pb=pb, j=j: nca.activation(out=pmt[j][:], in_=pb[:, :], func=AF.Identity, scale=lsT[:, g:g + 1]),
                          reads=[pk, "lsT"], writes=["pmt%d" % j])
                    kb.dma("sp", lambda g=g, n=n, j=j: ncs.dma_start(out=PMd[b, g * 128:(g + 1) * 128, n * 512:(n + 1) * 512], in_=pmt[j][:]),
                           reads=["pmt%d" % j], writes=["PMd"])
                    if dbg_t:
                        kb.dma("sp", lambda g=g, n=n, j=j: ncs.dma_start(out=dbg_t["pm"][b, g * 128:(g + 1) * 128, n * 512:(n + 1) * 512], in_=pmt[j][:]),
                               reads=["pmt%d" % j], writes=["dbg_pm"])
            else:
                isq = f < 10
                tile = (f - 4) if isq else (f - 10)
                g = tile // 2
                r = (1, 4, 16)[g]
                L = S // r
                dstT = qT if isq else kT
                dk = "qT" if isq else "kT"
                gain = gq if isq else gk
                gaink = "gq" if isq else "gk"
                for n in range(4):
                    j = n % 2
                    pb, pk = proj_tile(f, wi, n)
                    kb.op("act", lambda pb=pb, j=j: nca.activation(out=sqt[j][:], in_=pb[:, :], func=AF.Square), reads=[pk], writes=["sqt%d" % j])
                    kb.op("act", lambda pb=pb, j=j, gain=gain: nca.activation(out=qg[j][:], in_=pb[:, :], func=AF.Identity, scale=gain[:, 0:1]),
                          reads=[pk, gaink], writes=["qg%d" % j])
                    ps_, psk = next_pb()
                    kb.op("pe", lambda ps_=ps_, j=j: ncp.matmul(ps_[:, :], cs["blockones"][:], sqt[j][:], start=True, stop=True),
                          reads=["sqt%d" % j, "c_blockones"], writes=[psk])
                    pr_, prk = next_pb()
                    kb.op("pe", lambda pr_=pr_, j=j: ncp.matmul(pr_[:, :], cs["ropeR"][:], qg[j][:], start=True, stop=True),
                          reads=["qg%d" % j, "c_ropeR"], writes=[prk])
                    kb.op("act", lambda ps_=ps_, j=j: nca.activation(out=rs[j][:], in_=ps_[:, :], func=AF.Sqrt, bias=cs["epsc"][:, 1:2]),
                          reads=[psk, "c_epsc"], writes=["rs%d" % j])
                    kb.op("dve", lambda j=j: ncv.reciprocal(out=rs[j][:], in_=rs[j][:]), reads=["rs%d" % j], writes=["rs%d" % j])
                    kb.op("pool", lambda j=j, n=n: ncg.tensor_tensor(out=ta[j][:], in0=qg[j][:], in1=Ct[:, n * 512:(n + 1) * 512], op=ALU.mult),
                          reads=["qg%d" % j, "Ct"], writes=["ta%d" % j])
                    kb.op("dve", lambda pr_=pr_, j=j, n=n: ncv.tensor_tensor(out=tb[j][:], in0=pr_[:, :], in1=St[:, n * 512:(n + 1) * 512], op=ALU.mult),
                          reads=[prk, "St"], writes=["tb%d" % j])
                    kb.op("pool", lambda j=j: ncg.tensor_tensor(out=ta[j][:], in0=ta[j][:], in1=tb[j][:], op=ALU.add),
                          reads=["ta%d" % j, "tb%d" % j], writes=["ta%d" % j])
                    m0 = n * 512 // r
                    mn = 512 // r
                    if not isq:
                        dst = src0 = src1 = None
                    elif r == 1:
                        dst = dstT[:, tile, n * 512:(n + 1) * 512]
                        src0 = ta[j][:, :]
                        src1 = rs[j][:, :]
                    else:
                        dst = dstT[:, tile, :].rearrange("p (rr m) -> p m rr", rr=r)[:, m0:m0 + mn, :]
                        src0 = ta[j][:, :].rearrange("p (m rr) -> p m rr", rr=r)
                        src1 = rs[j][:, :].rearrange("p (m rr) -> p m rr", rr=r)
                    if isq:
                        kb.op("dve", lambda dst=dst, src0=src0, src1=src1: ncv.tensor_tensor(out=dst, in0=src0, in1=src1, op=ALU.mult),
                              reads=["ta%d" % j, "rs%d" % j], writes=[dk])
                    else:
                        for hl in range(2):
                            o = 64 * hl
                            if r == 1:
                                dsth = kT[o:o + 64, tile, hl, n * 512:(n + 1) * 512]
                                s0h, s1h = ta[j][o:o + 64, :], rs[j][o:o + 64, :]
                            else:
                                dsth = kT[o:o + 64, tile, hl, :].rearrange("p (rr m) -> p m rr", rr=r)[:, m0:m0 + mn, :]
                                s0h = ta[j][o:o + 64, :].rearrange("p (m rr) -> p m rr", rr=r)
                                s1h = rs[j][o:o + 64, :].rearrange("p (m rr) -> p m rr", rr=r)
                            kb.op("dve", lambda dsth=dsth, s0h=s0h, s1h=s1h: ncv.tensor_tensor(out=dsth, in0=s0h, in1=s1h, op=ALU.mult),
                                  reads=["ta%d" % j, "rs%d" % j], writes=[dk])
        kb.barrier()
        if sQ is None:
            sU.close()
            return
        sQ.close()
        if stage < 2.3:
            return
        vT = kb.sb("vT", [128, 6, S], BF16, stack=s2)
        for f in range(16, 22):
            wi = f % 2
            tile = f - 16
            g = tile // 2
            r = (1, 4, 16)[g]
            kb.dma("pool", lambda f=f, wi=wi: ncg.dma_start(out=win[wi][:], in_=w_in[:, f * 128:(f + 1) * 128].rearrange("(k p) n -> p k n", p=128)),
                   writes=["win%d" % wi])
            for n in range(4):
                pb, pk = proj_tile(f, wi, n)
                m0 = n * 512 // r
                mn = 512 // r
                if r == 1:
                    dst = vT[:, tile, n * 512:(n + 1) * 512]
                    src = pb[:, :]
                else:
                    dst = vT[:, tile, :].rearrange("p (rr m) -> p m rr", rr=r)[:, m0:m0 + mn, :]
                    src = pb[:, :].rearrange("p (m rr) -> p m rr", rr=r)
                if n % 2 == 0:
                    kb.op("act", lambda dst=dst, src=src: nca.copy(out=dst, in_=src), reads=[pk], writes=["vT"])
                else:
                    kb.op("dve", lambda dst=dst, src=src: ncv.tensor_copy(out=dst, in_=src), reads=[pk], writes=["vT"])
        identb = cs["ident_bf"]
        for g in range(3):
            for ci in range(16):
                pb, pk = next_pb()
                pbb = pb[:, 0:128].bitcast(BF16)
                for hp in range(2):
                    kb.op("pe", lambda g=g, ci=ci, hp=hp, pbb=pbb: ncp.transpose(pbb[:, hp * 128:(hp + 1) * 128], vT[:, 2 * g + hp, ci * 128:(ci + 1) * 128], identb[:]),
                          reads=["vT", "c_ident_bf"], writes=[pk])
                src = pbb[:, :].rearrange("p (h d) -> p h d", d=64)
                if ci % 2 == 0:
                    kb.op("act", lambda g=g, ci=ci, src=src: nca.copy(out=Vg[g][:, ci, :, 0:64], in_=src), reads=[pk], writes=["Vg%d" % g])
                else:
                    kb.op("dve", lambda g=g, ci=ci, src=src: ncv.tensor_copy(out=Vg[g][:, ci, :, 0:64], in_=src), reads=[pk], writes=["Vg%d" % g])


def phase_B2b(nc, kb, b, cs, pA, pB, qT, kT, Vg, AOd, dbg_t):
    ncv, nca, ncp, ncg, ncs = nc.vector, nc.scalar, nc.tensor, nc.gpsimd, nc.sync
    with contextlib.ExitStack() as s3:
        acc = kb.sb("acc", [64, 4, S], stack=s3)
        accd = kb.sb("accd", [64, 4, S], stack=s3)
        kb.op("pool", lambda: ncg.memset(accd[:], 0.0), writes=["accd"])
        PT = [kb.sb("PT%d" % i, [128, 2, 256], BF16, stack=s3) for i in range(3)]
        aot = [kb.sb("aot%d" % i, [64, 512], stack=s3) for i in range(2)]
        kb.op("pool", lambda: ncg.memset(acc[:], 0.0), writes=["acc"])
        it = 0
        for g in range(3):
            r = (1, 4, 16)[g]
            L = S // r
            nch = L // 128
            for rr in range(r):
                for c in range(nch):
                    ci = rr * nch + c
                    j0 = max(0, 128 * c - 64)
                    j1 = min(L, 128 * c + 192)
                    nq = j1 - j0
                    mo = j0 - (128 * c - 64)
                    for hp in range(2):
                        tile = 2 * g + hp
                        pi = it % 4
                        ps_, psk = pB[pi], "pB%d" % pi
                        pt, ptk = PT[it % 3], "PT%d" % (it % 3)
                        po, pok = pA[it % 2], "pA%d" % (it % 2)
                        it += 1
                        for hl in range(2):
                            o = 64 * hl
                            kb.op("pe", lambda o=o, hl=hl, tile=tile, rr=rr, c=c, j0=j0, j1=j1, nq=nq, L=L, ps_=ps_: ncp.matmul(
                                ps_[:, hl * 256:hl * 256 + nq], kT[:, tile, hl, rr * L + 128 * c:rr * L + 128 * c + 128],
                                qT[:, tile, rr * L + j0:rr * L + j1], start=True, stop=True),
                                reads=["qT", "kT"], writes=[psk])
                        kb.op("act", lambda ps_=ps_, pt=pt, nq=nq: nca.activation(
                            out=pt[:, :, 0:nq], in_=ps_[:, :].rearrange("p (h q) -> p h q", h=2)[:, :, 0:nq], func=AF.Exp, scale=0.125),
                            reads=[psk], writes=[ptk])
                        for hl in range(2):
                            kb.op("pool" if hl == 0 else "dve",
                                  lambda hl=hl, pt=pt, nq=nq, mo=mo: (ncg if hl == 0 else ncv).tensor_tensor(
                                      out=pt[:, hl, 0:nq], in0=pt[:, hl, 0:nq], in1=cs["band"][:, mo:mo + nq], op=ALU.mult),
                                  reads=[ptk, "c_band"], writes=[ptk])
                        for hl in range(2):
                            h = 2 * hp + hl
                            kb.op("pe", lambda hl=hl, h=h, g=g, ci=ci, pt=pt, po=po, nq=nq: ncp.matmul(
                                po[0:64, hl * 256:hl * 256 + nq], Vg[g][:, ci, h, 0:64], pt[:, hl, 0:nq], start=True, stop=True),
                                reads=["Vg%d" % g, ptk], writes=[pok])
                            kb.op("pe", lambda hl=hl, pt=pt, po=po, nq=nq: ncp.matmul(
                                po[0:64, 512 + hl * 256:512 + hl * 256 + nq], cs["ones_bf"][:, 0:64], pt[:, hl, 0:nq], start=True, stop=True),
                                reads=["c_ones_bf", ptk], writes=[pok])
                        for hl in range(2):
                            h = 2 * hp + hl
                            ts0 = j0 * r + rr
                            dst = acc[0:64, h, ts0:ts0 + (nq - 1) * r + 1:r]
                            dstd = accd[0:64, h, ts0:ts0 + (nq - 1) * r + 1:r]
                            kb.op("dve", lambda dst=dst, po=po, hl=hl, nq=nq: ncv.tensor_tensor(
                                out=dst, in0=dst, in1=po[0:64, hl * 256:hl * 256 + nq], op=ALU.add),
                                reads=[pok, "acc"], writes=["acc"])
                            kb.op("dve", lambda dstd=dstd, po=po, hl=hl, nq=nq: ncv.tensor_tensor(
                                out=dstd, in0=dstd, in1=po[0:64, 512 + hl * 256:512 + hl * 256 + nq], op=ALU.add),
                                reads=[pok, "accd"], writes=["accd"])
        for h in range(4):
            for n in range(4):
                j = (h * 4 + n) % 2
                kb.op("dve", lambda n=n, h=h, j=j: ncv.reciprocal(out=aot[j][:], in_=accd[0:64, h, n * 512:(n + 1) * 512]), reads=["accd"], writes=["aot%d" % j])
                kb.op("dve", lambda n=n, h=h, j=j: ncv.tensor_tensor(out=aot[j][:], in0=aot[j][:], in1=acc[0:64, h, n * 512:(n + 1) * 512], op=ALU.mult),
                      reads=["aot%d" % j, "acc"], writes=["aot%d" % j])
                kb.dma("sp", lambda h=h, n=n, j=j: ncs.dma_start(out=AOd[b, h * 64:(h + 1) * 64, n * 512:(n + 1) * 512], in_=aot[j][:]),
                       reads=["aot%d" % j], writes=["AOd"])
                if dbg_t:
                    kb.dma("sp", lambda h=h, n=n, j=j: ncs.dma_start(out=dbg_t["ao"][b, h * 64:(h + 1) * 64, n * 512:(n + 1) * 512], in_=aot[j][:]),
                           reads=["aot%d" % j], writes=["dbg_ao"])


def phase_B2c(nc, kb, b, dram, cs, pv, pA, pB, HTd, PMd, AOd, out):
    ncv, nca, ncp, ncg, ncs = nc.vector, nc.scalar, nc.tensor, nc.gpsimd, nc.sync
    w_in = dram["w_in"]
    with contextlib.ExitStack() as s4:
        gate1_b = kb.sb("gate1_b", [128, NB, D], stack=s4)
        kb.dma("sp", lambda: ncs.dma_start(out=gate1_b[:].rearrange("p b d -> p (b d)"), in_=pv["BCd"][3]), reads=["BCd"], writes=["gate1_b"])
        wout = kb.sb("wout", [128, 8, D], F32R, stack=s4)
        wpu = kb.sb("wpu", [128, 4, D], F32R, stack=s4)
        wau = kb.sb("wau", [128, 2, D], F32R, stack=s4)
        hTt = kb.sb("hTt", [128, 8, 512], F32R, stack=s4)
        pmt = kb.sb("pmt_c", [128, 4, 512], F32R, stack=s4)
        aot = kb.sb("aot_c", [128, 2, 512], F32R, stack=s4)
        wgp = [kb.sb("wgp%d" % i, [128, 8, 128], F32R, stack=s4) for i in range(2)]
        wga = [kb.sb("wga%d" % i, [128, 8, 128], F32R, stack=s4) for i in range(2)]
        merged = kb.sb("merged", [128, 8, 512], F32R, stack=s4)
        sgp = kb.sb("sgp", [128, 512], stack=s4)
        sga = kb.sb("sga", [128, 512], stack=s4)
        m1 = kb.sb("m1", [128, 512], stack=s4)
        xt = [kb.sb("xtc%d" % i, [128, D], stack=s4) for i in range(2)]
        x1 = [kb.sb("x1c%d" % i, [128, D], stack=s4) for i in range(2)]
        kb.dma("pool", lambda: ncg.dma_start(out=wout[:], in_=dram["w_out"].rearrange("(k p) n -> p k n", p=128)), writes=["wout"])
        kb.dma("pool", lambda: ncg.dma_start(out=wpu[:], in_=dram["w_pool_up"].rearrange("(k p) n -> p k n", p=128)), writes=["wpu"])
        kb.dma("pool", lambda: ncg.dma_start(out=wau[:], in_=dram["w_attn_up"].rearrange("(k p) n -> p k n", p=128)), writes=["wau"])
        for n in range(4):
            kb.dma("pool", lambda n=n: ncg.dma_start(out=hTt[:], in_=HTd[b, :, n * 512:(n + 1) * 512].rearrange("(k p) t -> p k t", p=128)),
                   reads=["HTd"], writes=["hTt"])
            kb.dma("pool", lambda n=n: ncg.dma_start(out=pmt[:], in_=PMd[b, :, n * 512:(n + 1) * 512].rearrange("(k p) t -> p k t", p=128)),
                   reads=["PMd"], writes=["pmt_c"])
            kb.dma("pool", lambda n=n: ncg.dma_start(out=aot[:], in_=AOd[b, :, n * 512:(n + 1) * 512].rearrange("(k p) t -> p k t", p=128)),
                   reads=["AOd"], writes=["aot_c"])
            for j in range(8):
                wi = j % 2
                kb.dma("pool", lambda j=j, wi=wi: ncg.dma_start(out=wgp[wi][:], in_=w_in[:, 2816 + j * 128:2816 + (j + 1) * 128].rearrange("(k p) n -> p k n", p=128)),
                       writes=["wgp%d" % wi])
                kb.dma("pool", lambda j=j, wi=wi: ncg.dma_start(out=wga[wi][:], in_=w_in[:, 3840 + j * 128:3840 + (j + 1) * 128].rearrange("(k p) n -> p k n", p=128)),
                       writes=["wga%d" % wi])
                for kc in range(8):
                    kb.op("pe", lambda kc=kc, wi=wi: ncp.matmul(pB[0][:, :], wgp[wi][:, kc, :], hTt[:, kc, :], start=(kc == 0), stop=(kc == 7)),
                          reads=["wgp%d" % wi, "hTt"], writes=["pB0"])
                for kc in range(8):
                    kb.op("pe", lambda kc=kc, wi=wi: ncp.matmul(pB[1][:, :], wga[wi][:, kc, :], hTt[:, kc, :], start=(kc == 0), stop=(kc == 7)),
                          reads=["wga%d" % wi, "hTt"], writes=["pB1"])
                for g in range(4):
                    kb.op("pe", lambda g=g, j=j: ncp.matmul(pB[2][:, :], wpu[:, g, j * 128:(j + 1) * 128], pmt[:, g, :], start=(g == 0), stop=(g == 3)),
                          reads=["wpu", "pmt_c"], writes=["pB2"])
                for g in range(2):
                    kb.op("pe", lambda g=g, j=j: ncp.matmul(pB[3][:, :], wau[:, g, j * 128:(j + 1) * 128], aot[:, g, :], start=(g == 0), stop=(g == 1)),
                          reads=["wau", "aot_c"], writes=["pB3"])
                kb.op("act", lambda: nca.activation(out=sgp[:], in_=pB[0][:, :], func=AF.Sigmoid), reads=["pB0"], writes=["sgp"])
                kb.op("act", lambda: nca.activation(out=sga[:], in_=pB[1][:, :], func=AF.Sigmoid), reads=["pB1"], writes=["sga"])
                kb.op("dve", lambda: ncv.tensor_tensor(out=m1[:], in0=sgp[:], in1=pB[2][:, :], op=ALU.mult), reads=["sgp", "pB2"], writes=["m1"])
                kb.op("dve", lambda: ncv.tensor_tensor(out=sga[:], in0=sga[:], in1=pB[3][:, :], op=ALU.mult), reads=["sga", "pB3"], writes=["sga"])
                kb.op("pool", lambda j=j: ncg.tensor_tensor(out=merged[:, j, :], in0=m1[:], in1=sga[:], op=ALU.add), reads=["m1", "sga"], writes=["merged"])
            for s in range(4):
                i = s % 2
                r0 = b * S + n * 512 + s * 128
                kb.dma("sp", lambda i=i, r0=r0: ncs.dma_start(out=xt[i][:], in_=dram["x"][r0:r0 + 128, :]), writes=["xtc%d" % i])
                pa, pak = pA[i], "pA%d" % i
                for hf in range(2):
                    for j in range(8):
                        kb.op("pe", lambda j=j, hf=hf, s=s, pa=pa: ncp.matmul(pa[:, hf * 512:(hf + 1) * 512], merged[:, j, s * 128:(s + 1) * 128],
                                                                             wout[:, j, hf * 512:(hf + 1) * 512], start=(j == 0), stop=(j == 7)),
                              reads=["merged", "wout"], writes=[pak])
                kb.op("dve", lambda i=i, pa=pa: ncv.tensor_tensor(out=x1[i][:], in0=pa[:, :], in1=gate1_b[:, b, :], op=ALU.mult),
                      reads=[pak, "gate1_b"], writes=["x1c%d" % i])
                kb.op("pool", lambda i=i: ncg.tensor_tensor(out=x1[i][:], in0=x1[i][:], in1=xt[i][:], op=ALU.add),
                      reads=["x1c%d" % i, "xtc%d" % i], writes=["x1c%d" % i])
                kb.dma("sp", lambda i=i, r0=r0: ncs.dma_start(out=out[r0:r0 + 128, :], in_=x1[i][:]), reads=["x1c%d" % i], writes=["out"])


_NC_CACHE = {}


def _get_nc(stage=99, dbg=False):
    key = (stage, dbg)
    if key not in _NC_CACHE:
        _NC_CACHE[key] = build_nc(stage, dbg)
    return _NC_CACHE[key]


def _in_maps(inputs, cores, stage=99):
    consts = _consts()
    maps = []
    w = {}
    for name, shape in W_SPECS:
        if stage < 6 and name.startswith("w_exp"):
            continue
        w[name] = np.ascontiguousarray(np.asarray(inputs[name], dtype=np.float32).reshape(shape))
    x = np.asarray(inputs["x"], dtype=np.float32)
    c = np.asarray(inputs["c"], dtype=np.float32)
    pos = np.asarray(inputs["positions"], dtype=np.int32)
    for i in cores:
        m = {"x": np.ascontiguousarray(x[NB * i:NB * (i + 1)].reshape(T, D)),
             "c": np.ascontiguousarray(c[NB * i:NB * (i + 1)]),
             "positions": np.ascontiguousarray(pos[NB * i:NB * (i + 1)])}
        m.update(w)
        for k, v in consts.items():
            m["k_" + k] = v
        maps.append(m)
    return maps


def kernel(**inputs):
    nc = _get_nc(stage=6)
    maps = _in_maps(inputs, list(range(NCORES)), stage=6)
    res = run_bass_kernel_spmd(nc, maps, core_ids=list(range(NCORES)))
    outs = [np.asarray(r["out"]).reshape(NB, S, D) for r in res.results]
    return np.concatenate(outs, axis=0).astype(np.float32)
```
